# Optimizing a Trainium2 kernel written in Bass

```python
import jax, jax.numpy as jnp
from jax import lax
import numpy as np

D_MODEL = 1024
BATCH = 4
SEQ = 8192
DEPTH = 2

CHUNK = 64
N_BRANCH = 3
POOL_GROUPS = 4
POOL_GW = 128
POOL_W = POOL_GROUPS * POOL_GW
POOL_WINDOWS = (2, 4, 8, 16)
ML_HEADS = 4
ML_DH = 128
ML_W = ML_HEADS * ML_DH
CONV_W = 4
FOX_HEADS = 8
FOX_DH = 64
FOX_W = FOX_HEADS * FOX_DH
Q_BLOCK = 128
D_FF = 2816
N_EXPERTS = 8
TOP_K = 2
D_FF_EXPERT = 3584
MOE_BLOCK = 128
NORM_EPS = 1e-6
IN_SIZES = (POOL_W, ML_W, ML_W, ML_W, ML_W, ML_HEADS, ML_HEADS, FOX_W, FOX_W, FOX_W, FOX_HEADS, N_BRANCH * D_MODEL)
IN_W = sum(IN_SIZES)
N_DENSE = (DEPTH + 1) // 2
N_MOE = DEPTH // 2

kernel_name = 'hybrid_pool_mlstm_fox_moe_trunk'


def rmsnorm(x, g):
    xf = x.astype(jnp.float32)
    y = xf * lax.rsqrt(jnp.mean(xf * xf, axis=-1, keepdims=True) + NORM_EPS)
    return (y * g.astype(jnp.float32)).astype(x.dtype)


def causal_depthwise_conv(u, w):
    return lax.conv_general_dilated(u, w[:, None, :].astype(u.dtype), window_strides=(1,),
                                    padding=[(CONV_W - 1, 0)], dimension_numbers=('NWC', 'WIO', 'NWC'),
                                    feature_group_count=u.shape[-1])


def pool_mixer(u, w_grp, scale):
    bsz, s_len, _ = u.shape
    uf = u.astype(jnp.float32).reshape(bsz, s_len, POOL_GROUPS, POOL_GW)
    cs = jnp.concatenate([jnp.zeros_like(uf[:, :1]), jnp.cumsum(uf, axis=1)], axis=1)
    pos = jnp.arange(s_len)
    outs = []
    for g, w in enumerate(POOL_WINDOWS):
        csg = cs[:, :, g]
        lower = jnp.concatenate([jnp.zeros_like(csg[:, :w - 1]), csg[:, :s_len - w + 1]], axis=1)
        cnt = jnp.minimum(pos + 1, w).astype(jnp.float32)[None, :, None]
        outs.append((csg[:, 1:] - lower) / cnt - uf[:, :, g])
    d = jnp.stack(outs, axis=2)
    y = jnp.einsum('bsgc,gcd->bsgd', d, w_grp.astype(jnp.float32)).reshape(bsz, s_len, POOL_W)
    return (y * scale.astype(jnp.float32)).astype(u.dtype)


def mlstm_mixer(q, k, v, o_pre, i_pre, f_pre, g_norm):
    bsz, s_len, _ = q.shape
    nc = s_len // CHUNK

    def heads(t):
        return t.astype(jnp.float32).reshape(bsz, nc, CHUNK, ML_HEADS, ML_DH).transpose(1, 0, 3, 2, 4)

    def gates(t):
        return t.astype(jnp.float32).reshape(bsz, nc, CHUNK, ML_HEADS).transpose(1, 0, 3, 2)

    qc, kc, vc = heads(q), heads(k) * (ML_DH ** -0.5), heads(v)
    lic = gates(i_pre)
    lfc = gates(jax.nn.log_sigmoid(f_pre.astype(jnp.float32)))
    tri = jnp.tril(jnp.ones((CHUNK, CHUNK), dtype=bool))

    def step(carry, inp):
        c_mat, n_vec, m = carry
        qb, kb, vb, li, lf = inp
        b = jnp.cumsum(lf, axis=-1)
        dmat = jnp.where(tri, b[..., :, None] - b[..., None, :] + li[..., None, :], -jnp.inf)
        m_inter = b + m[..., None]
        m_t = jnp.maximum(m_inter, jnp.max(dmat, axis=-1))
        w_inter = jnp.exp(m_inter - m_t)
        p = jnp.exp(dmat - m_t[..., None]) * jnp.einsum('bhtd,bhsd->bhts', qb, kb)
        num = w_inter[..., None] * jnp.einsum('bhvk,bhtk->bhtv', c_mat, qb) + jnp.einsum('bhts,bhsv->bhtv', p, vb)
        den = w_inter * jnp.einsum('bhk,bhtk->bht', n_vec, qb) + jnp.sum(p, axis=-1)
        h = num / jnp.maximum(jnp.abs(den), jnp.exp(-m_t))[..., None]
        b_last = b[..., -1]
        g = b_last[..., None] - b + li
        m_new = jnp.maximum(b_last + m, jnp.max(g, axis=-1))
        decay = jnp.exp(b_last + m - m_new)
        wk = jnp.exp(g - m_new[..., None])
        c_mat = decay[..., None, None] * c_mat + jnp.einsum('bhs,bhsv,bhsk->bhvk', wk, vb, kb)
        n_vec = decay[..., None] * n_vec + jnp.einsum('bhs,bhsk->bhk', wk, kb)
        return (c_mat, n_vec, m_new), h

    init = (jnp.zeros((bsz, ML_HEADS, ML_DH, ML_DH), jnp.float32),
            jnp.zeros((bsz, ML_HEADS, ML_DH), jnp.float32),
            jnp.zeros((bsz, ML_HEADS), jnp.float32))
    _, h = lax.scan(step, init, (qc, kc, vc, lic, lfc))
    h = h.transpose(1, 0, 3, 2, 4)
    h = h * lax.rsqrt(jnp.mean(h * h, axis=-1, keepdims=True) + NORM_EPS) * g_norm.astype(jnp.float32).reshape(ML_HEADS, ML_DH)
    h = h.reshape(bsz, s_len, ML_W) * jax.nn.sigmoid(o_pre.astype(jnp.float32))
    return h.astype(q.dtype)


def fox_mixer(q, k, v, f_pre):
    bsz, s_len, _ = q.shape
    nb = s_len // Q_BLOCK
    qh = q.reshape(bsz, s_len, FOX_HEADS, FOX_DH).transpose(0, 2, 1, 3)
    kh = k.reshape(bsz, s_len, FOX_HEADS, FOX_DH).transpose(0, 2, 1, 3)
    vh = v.reshape(bsz, s_len, FOX_HEADS, FOX_DH).transpose(0, 2, 1, 3)
    f_cum = jnp.cumsum(jax.nn.log_sigmoid(f_pre.astype(jnp.float32)), axis=1).transpose(0, 2, 1)
    q_blocks = qh.reshape(bsz, FOX_HEADS, nb, Q_BLOCK, FOX_DH).transpose(2, 0, 1, 3, 4)
    f_blocks = f_cum.reshape(bsz, FOX_HEADS, nb, Q_BLOCK).transpose(2, 0, 1, 3)
    kpos = jnp.arange(s_len)
    scale = FOX_DH ** -0.5

    def block(args):
        qb, fq, i = args
        qpos = i * Q_BLOCK + jnp.arange(Q_BLOCK)
        s = jnp.einsum('bhtd,bhsd->bhts', qb, kh).astype(jnp.float32) * scale + fq[..., :, None] - f_cum[:, :, None, :]
        s = jnp.where(kpos[None, :] <= qpos[:, None], s, -jnp.inf)
        p = jax.nn.softmax(s, axis=-1)
        return jnp.einsum('bhts,bhsd->bhtd', p.astype(vh.dtype), vh)

    o = lax.map(block, (q_blocks, f_blocks, jnp.arange(nb)))
    return o.transpose(1, 0, 3, 2, 4).reshape(bsz, s_len, FOX_W)


def token_mixing(x, norm_g, w_in, pool_w_grp, pool_scale, ml_conv_w, ml_b_i, ml_b_f, ml_norm_g,
                 fox_b_f, w_br_pool, w_br_ml, w_br_fox, w_out):
    h = rmsnorm(x, norm_g)
    proj = h @ w_in
    splits = np.cumsum(IN_SIZES)[:-1].tolist()
    (u_pool, ml_q, ml_k, ml_v, ml_o, ml_i, ml_f, fx_q, fx_k, fx_v, fx_f, gate_pre) = jnp.split(proj, splits, axis=-1)
    qk = jax.nn.silu(causal_depthwise_conv(jnp.concatenate([ml_q, ml_k], axis=-1), ml_conv_w))
    ml_q, ml_k = qk[..., :ML_W], qk[..., ML_W:]
    y_pool = pool_mixer(u_pool, pool_w_grp, pool_scale) @ w_br_pool
    y_ml = mlstm_mixer(ml_q, ml_k, ml_v, ml_o, ml_i + ml_b_i, ml_f + ml_b_f, ml_norm_g) @ w_br_ml
    y_fox = fox_mixer(fx_q, fx_k, fx_v, fx_f + fox_b_f) @ w_br_fox
    gts = jax.nn.sigmoid(gate_pre).reshape(gate_pre.shape[:-1] + (N_BRANCH, D_MODEL))
    merged = gts[..., 0, :] * y_pool + gts[..., 1, :] * y_ml + gts[..., 2, :] * y_fox
    return merged @ w_out


def swiglu(h, w_gate, w_up, w_down):
    return (jax.nn.silu(h @ w_gate) * (h @ w_up)) @ w_down


def moe_swiglu(h, w_router, b_router, w_gate, w_up, w_down):
    bsz, s_len, d = h.shape
    n_tok = bsz * s_len
    t = h.reshape(n_tok, d)
    logits = (t @ w_router).astype(jnp.float32) + b_router.astype(jnp.float32)
    top_v, top_e = lax.top_k(logits, TOP_K)
    gate = jax.nn.softmax(top_v, axis=-1)
    n_asg = n_tok * TOP_K
    e_flat = top_e.reshape(n_asg)
    tok_flat = jnp.repeat(jnp.arange(n_tok, dtype=jnp.int32), TOP_K)
    g_flat = gate.reshape(n_asg)
    order = jnp.argsort(e_flat)
    e_s, tok_s, g_s = e_flat[order], tok_flat[order], g_flat[order]
    counts = jnp.bincount(e_flat, length=N_EXPERTS).astype(jnp.int32)
    start = jnp.cumsum(counts) - counts
    padded = ((counts + MOE_BLOCK - 1) // MOE_BLOCK) * MOE_BLOCK
    pend = jnp.cumsum(padded)
    pstart = pend - padded
    dest = pstart[e_s] + (jnp.arange(n_asg, dtype=jnp.int32) - start[e_s])
    n_blk = (n_asg + N_EXPERTS * (MOE_BLOCK - 1) + MOE_BLOCK - 1) // MOE_BLOCK
    n_rows = n_blk * MOE_BLOCK
    row_tok = jnp.full((n_rows,), n_tok, dtype=jnp.int32).at[dest].set(tok_s)
    row_g = jnp.zeros((n_rows,), jnp.float32).at[dest].set(g_s)
    blk_e = jnp.minimum(jnp.searchsorted(pend, jnp.arange(n_blk, dtype=jnp.int32) * MOE_BLOCK, side='right'), N_EXPERTS - 1)
    t_pad = jnp.concatenate([t, jnp.zeros((1, d), t.dtype)], axis=0)
    xr = t_pad[row_tok].reshape(n_blk, MOE_BLOCK, d)

    def one(args):
        xb, e = args
        return (jax.nn.silu(xb @ w_gate[e]) * (xb @ w_up[e])) @ w_down[e]

    out = lax.map(one, (xr, blk_e)).reshape(n_rows, d)
    y = jnp.zeros((n_tok + 1, d), out.dtype).at[row_tok].add(out * row_g[:, None].astype(out.dtype))[:n_tok]
    return y.reshape(bsz, s_len, d)


def setup_inputs(seed: int = 0) -> dict:
    key = jax.random.key(seed)
    ks = iter(jax.random.split(key, 32))

    def nrm(shape, scale):
        return jax.random.normal(next(ks), shape, jnp.float32) * scale

    return {
        'x': nrm((BATCH, SEQ, D_MODEL), 1.0),
        'mix_norm_g': 1.0 + nrm((DEPTH, D_MODEL), 0.05),
        'w_in': nrm((DEPTH, D_MODEL, IN_W), D_MODEL ** -0.5),
        'pool_w_grp': nrm((DEPTH, POOL_GROUPS, POOL_GW, POOL_GW), POOL_GW ** -0.5),
        'pool_scale': 1.0 + nrm((DEPTH, POOL_W), 0.05),
        'ml_conv_w': nrm((DEPTH, CONV_W, 2 * ML_W), CONV_W ** -0.5),
        'ml_b_i': nrm((DEPTH, ML_HEADS), 0.1),
        'ml_b_f': 3.0 + nrm((DEPTH, ML_HEADS), 0.5),
        'ml_norm_g': 1.0 + nrm((DEPTH, ML_W), 0.05),
        'fox_b_f': 3.0 + nrm((DEPTH, FOX_HEADS), 0.5),
        'w_br_pool': nrm((DEPTH, POOL_W, D_MODEL), POOL_W ** -0.5),
        'w_br_ml': nrm((DEPTH, ML_W, D_MODEL), ML_W ** -0.5),
        'w_br_fox': nrm((DEPTH, FOX_W, D_MODEL), FOX_W ** -0.5),
        'w_out': nrm((DEPTH, D_MODEL, D_MODEL), D_MODEL ** -0.5),
        'ffn_norm_g': 1.0 + nrm((DEPTH, D_MODEL), 0.05),
        'ff_w_gate': nrm((N_DENSE, D_MODEL, D_FF), D_MODEL ** -0.5),
        'ff_w_up': nrm((N_DENSE, D_MODEL, D_FF), D_MODEL ** -0.5),
        'ff_w_down': nrm((N_DENSE, D_FF, D_MODEL), D_FF ** -0.5),
        'moe_w_router': nrm((N_MOE, D_MODEL, N_EXPERTS), D_MODEL ** -0.5),
        'moe_b_router': nrm((N_MOE, N_EXPERTS), 0.01),
        'moe_w_gate': nrm((N_MOE, N_EXPERTS, D_MODEL, D_FF_EXPERT), D_MODEL ** -0.5),
        'moe_w_up': nrm((N_MOE, N_EXPERTS, D_MODEL, D_FF_EXPERT), D_MODEL ** -0.5),
        'moe_w_down': nrm((N_MOE, N_EXPERTS, D_FF_EXPERT, D_MODEL), D_FF_EXPERT ** -0.5),
        'final_norm_g': 1.0 + nrm((D_MODEL,), 0.05),
    }


def reference(x, mix_norm_g, w_in, pool_w_grp, pool_scale, ml_conv_w, ml_b_i, ml_b_f, ml_norm_g, fox_b_f,
              w_br_pool, w_br_ml, w_br_fox, w_out, ffn_norm_g, ff_w_gate, ff_w_up, ff_w_down,
              moe_w_router, moe_b_router, moe_w_gate, moe_w_up, moe_w_down, final_norm_g):
    for l in range(DEPTH):
        x = x + token_mixing(x, mix_norm_g[l], w_in[l], pool_w_grp[l], pool_scale[l], ml_conv_w[l],
                             ml_b_i[l], ml_b_f[l], ml_norm_g[l], fox_b_f[l], w_br_pool[l], w_br_ml[l],
                             w_br_fox[l], w_out[l])
        h = rmsnorm(x, ffn_norm_g[l])
        if l % 2 == 0:
            x = x + swiglu(h, ff_w_gate[l // 2], ff_w_up[l // 2], ff_w_down[l // 2])
        else:
            x = x + moe_swiglu(h, moe_w_router[l // 2], moe_b_router[l // 2], moe_w_gate[l // 2],
                               moe_w_up[l // 2], moe_w_down[l // 2])
    return rmsnorm(x, final_norm_g)
```

```python
import contextlib
import os
import numpy as np
import concourse.bass as bass
import concourse.mybir as mybir
from concourse.bass_utils import run_bass_kernel_spmd

F32 = mybir.dt.float32
BF16 = mybir.dt.bfloat16
AF = mybir.ActivationFunctionType
ALU = mybir.AluOpType
AX = mybir.AxisListType

D = 1024
SEQ = 8192
BATCH = 4
NCORES = 8
IN_W = 7184
D_FF = 2816
D_FFE = 3584
NEXP = 8
EPS = 1e-6


ALL_BUFS = []


class Buf:
    __slots__ = ("name", "w", "r")

    def __init__(self, name=""):
        self.name = name
        self.w = None
        self.r = {}
        ALL_BUFS.append(self)


class Lane:
    def __init__(self, name, sem, step):
        self.name = name
        self.sem = sem
        self.step = step
        self.count = 0
        self.seen = {}
        self.snaps = {}


SYNC_SAME = {"tensor": False, "vector": True, "scalar": True, "gpsimd": True, "sync": False}


class FW:
    def __init__(self, nc, n_dma_lanes=24):
        self.nc = nc
        self.ops = {k: [] for k in ("tensor", "vector", "scalar", "gpsimd", "sync")}
        self.eng = {}
        for k in self.ops:
            self.eng[k] = Lane(k, nc.alloc_semaphore(name=f"prog_{k}"), 1)
        self.dma_lanes = [Lane(f"dma{i}", nc.alloc_semaphore(name=f"dma_{i}"), 16)
                          for i in range(n_dma_lanes)]
        self.dma_rr = 0
        self.cc = Lane("cc", nc.alloc_semaphore(name="cc_sem"), 1)

    def cc_op(self, fn, reads=()):
        E = self.eng["gpsimd"]
        self._emit_waits(E, self._needs(reads, ()))
        self.cc.count += 1
        self.ops["gpsimd"].append(("cc", fn, self.cc.sem))

    def cc_wait(self):
        for E in self.eng.values():
            self._emit_waits(E, {self.cc: self.cc.count})

    def _needs(self, reads, writes):
        needs = {}
        for b in reads:
            if b.w is not None:
                l, i = b.w
                if needs.get(l, 0) < i:
                    needs[l] = i
        for b in writes:
            if b.w is not None:
                l, i = b.w
                if needs.get(l, 0) < i:
                    needs[l] = i
            for l, i in b.r.items():
                if needs.get(l, 0) < i:
                    needs[l] = i
        return needs

    def _emit_waits(self, E, needs):
        for l, i in needs.items():
            if l is E and not SYNC_SAME[E.name]:
                continue
            if E.seen.get(l, 0) >= i:
                continue
            self.ops[E.name].append(("wait", l.sem, i * l.step))
            E.seen[l] = i
            snap = l.snaps.get(i)
            if snap:
                for l2, i2 in snap.items():
                    if E.seen.get(l2, 0) < i2:
                        E.seen[l2] = i2

    def op(self, engine, fn, reads=(), writes=()):
        E = self.eng[engine]
        self._emit_waits(E, self._needs(reads, writes))
        E.count += 1
        idx = E.count
        E.snaps[idx] = dict(E.seen)
        self.ops[engine].append(("op", fn, E.sem))
        for b in reads:
            b.r[E] = idx
        for b in writes:
            b.w = (E, idx)
            b.r = {}

    def dma(self, queue, out, in_, reads=(), writes=(), dyn=0, **kw):
        E = self.eng[queue]
        L = self.dma_lanes[self.dma_rr]
        self.dma_rr = (self.dma_rr + 1) % len(self.dma_lanes)
        needs = self._needs(reads, writes)
        if L.count > 0:
            needs[L] = max(needs.get(L, 0), L.count)
        self._emit_waits(E, needs)
        L.count += 1
        idx = L.count
        L.snaps[idx] = dict(E.seen)
        if dyn:
            self.ops[queue].append(("dyndma", out, in_, dyn, L.sem))
        else:
            self.ops[queue].append(("dma", out, in_, kw, L.sem))
        for b in reads:
            b.r[L] = idx
        for b in writes:
            b.w = (L, idx)
            b.r = {}

    def _lanes(self):
        return list(self.eng.values()) + self.dma_lanes + [self.cc]

    def begin_if(self, flag_ap):
        self.barrier()
        self._snap = ({l: l.count for l in self._lanes()}, {l: dict(l.seen) for l in self._lanes()},
                      [(b, b.w, dict(b.r)) for b in ALL_BUFS], self.dma_rr)
        for name in self.ops:
            self.ops[name].append(("if", flag_ap))

    def _restore(self):
        counts, seens, bufs, rr = self._snap
        for l, c_ in counts.items():
            l.count = c_
            l.seen = dict(seens[l])
        for b, w, r in bufs:
            b.w = w
            b.r = dict(r)
        self.dma_rr = rr

    def begin_else(self):
        self._endA = {l: l.count for l in self._lanes()}
        self._padA = {name: [] for name in self.ops}
        for name in self.ops:
            self.ops[name].append(("else", self._padA[name]))
        self._restore()

    def end_if(self):
        endB = {l: l.count for l in self._lanes()}
        padB = {name: [] for name in self.ops}
        for l in self._lanes():
            fin = max(self._endA[l], endB[l])
            owner = l.name if l.name in self.ops else "sync"
            for end, pads in ((self._endA[l], self._padA), (endB[l], padB)):
                if fin > end:
                    pads[owner].append((l.sem, end * l.step, (fin - end) * l.step))
            l.count = fin
        for name in self.ops:
            self.ops[name].append(("endif", padB[name]))
        counts, seens, bufs, rr = self._snap
        for l in self._lanes():
            l.seen = dict(seens[l])
        self.barrier()

    def barrier(self):
        lanes = list(self.eng.values()) + self.dma_lanes
        counts = {l: l.count for l in lanes if l.count > 0}
        for E in self.eng.values():
            self._emit_waits(E, {l: i for l, i in counts.items() if l is not E})

    def wait_all(self, engine):
        E = self.eng[engine]
        needs = {}
        for l in list(self.eng.values()) + self.dma_lanes:
            if l.count > 0 and l is not E:
                needs[l] = l.count
        self._emit_waits(E, needs)

    def emit(self):
        with self.nc.Block() as block:
            for name in self.ops:
                ops = self.ops[name]
                if not ops:
                    continue

                def body(eng, ops=ops):
                    me = None
                    stack = []

                    def pad(pads):
                        for sem, cur, amt in pads:
                            eng.wait_ge(sem, cur)
                            eng.sem_inc(sem, amt)
                    for o in ops:
                        if o[0] == "if":
                            reg = eng.alloc_register()
                            eng.reg_load(reg, o[1])
                            cm = eng.If_eq(reg, 1)
                            cm.__enter__()
                            stack.append(cm)
                        elif o[0] == "else":
                            pad(o[1])
                            stack.pop().__exit__(None, None, None)
                            cm = eng.Else()
                            cm.__enter__()
                            stack.append(cm)
                        elif o[0] == "endif":
                            pad(o[1])
                            stack.pop().__exit__(None, None, None)
                        elif o[0] == "wait":
                            eng.wait_ge(o[1], o[2])
                        elif o[0] == "op":
                            o[1](eng).then_inc(o[2], 1)
                        elif o[0] == "cc":
                            o[1](eng).then_inc(o[2], 1)
                        elif o[0] == "dyndma":
                            _, out, in_, mult, sem = o
                            if me is None:
                                me = eng.partition_id() % 2
                            off = me * mult
                            eng.dma_start(out=out, in_=bass.AP(in_.tensor, in_.offset + off, in_.ap)).then_inc(sem, 16)
                        else:
                            _, out, in_, kw, sem = o
                            eng.dma_start(out=out, in_=in_, **kw).then_inc(sem, 16)

                getattr(block, name)(body)


class T:
    __slots__ = ("t", "b")

    def __init__(self, t, b):
        self.t = t
        self.b = b


class Rot:
    def __init__(self, items):
        self.items = items
        self.i = 0

    def get(self):
        x = self.items[self.i]
        self.i = (self.i + 1) % len(self.items)
        return x


class Cx:
    def __init__(self, nc):
        self.nc = nc
        self.fw = FW(nc)
        self.st = contextlib.ExitStack()
        self.n = 0

    def sb(self, shape, dtype, name=None):
        self.n += 1
        nm = f"{name or 'sb'}_{self.n}"
        t = self.st.enter_context(self.nc.sbuf_tensor(nm, list(shape), dtype))
        return T(t, Buf(nm))

    def sbpool(self, n, shape, dtype, name=None):
        return Rot([self.sb(shape, dtype, (name or "p") + f"_{self.n}_{i}") for i in range(n)])

    def psum_banks(self, n=8, dtype=F32):
        out = []
        for i in range(n):
            self.n += 1
            cols = 512 if dtype == F32 else 1024
            t = self.st.enter_context(self.nc.psum_tensor(f"ps{self.n}", [128, cols], dtype))
            out.append(T(t, Buf(f"ps{self.n}")))
        return Rot(out)

    def mm(self, out, lhsT, rhs, start, stop, reads, writes):
        self.fw.op("tensor", lambda e: e.matmul(out, lhsT=lhsT, rhs=rhs, start=start, stop=stop),
                   reads, writes)

    def transpose(self, out, in_, ident, reads, writes):
        self.fw.op("tensor", lambda e: e.transpose(out, in_, ident), reads, writes)

    def act(self, out, in_, func, reads, writes, bias=None, scale=None):
        kw = {}
        if bias is not None:
            kw["bias"] = bias
        if scale is not None:
            kw["scale"] = scale
        self.fw.op("scalar", lambda e: e.activation(out=out, in_=in_, func=func, **kw), reads, writes)

    def tt(self, eng, out, in0, in1, op, reads, writes):
        self.fw.op(eng, lambda e: e.tensor_tensor(out=out, in0=in0, in1=in1, op=op), reads, writes)

    def ts(self, eng, out, in0, s1, op0, reads, writes, s2=None, op1=None):
        if op1 is None:
            self.fw.op(eng, lambda e: e.tensor_scalar(out=out, in0=in0, scalar1=s1, scalar2=None, op0=op0),
                       reads, writes)
        else:
            self.fw.op(eng, lambda e: e.tensor_scalar(out=out, in0=in0, scalar1=s1, scalar2=s2, op0=op0, op1=op1),
                       reads, writes)

    def stt(self, out, in0, scalar, in1, op0, op1, reads, writes):
        self.fw.op("vector", lambda e: e.scalar_tensor_tensor(out=out, in0=in0, scalar=scalar, in1=in1,
                                                              op0=op0, op1=op1), reads, writes)

    def copy(self, eng, out, in_, reads, writes):
        if eng == "scalar":
            self.fw.op("scalar", lambda e: e.activation(out=out, in_=in_, func=AF.Copy), reads, writes)
        else:
            self.fw.op(eng, lambda e: e.tensor_copy(out=out, in_=in_), reads, writes)

    def memset(self, eng, ap, val, writes):
        self.fw.op(eng, lambda e: e.memset(ap, val), (), writes)

    def dma(self, q, out, in_, reads=(), writes=(), dyn=0, **kw):
        self.fw.dma(q, out, in_, reads, writes, dyn=dyn, **kw)

    def finish(self):
        self.fw.wait_all("sync")
        self.fw.emit()
        self.st.close()


def dram(nc, name, shape, dtype, kind):
    return nc.dram_tensor(name, list(shape), dtype, kind=kind).ap()


def make_consts(cx):
    c = {}
    c["ones_f"] = cx.sb([128, 128], F32, "ones_f")
    c["ones_b"] = cx.sb([128, 128], BF16, "ones_b")
    c["id_f"] = cx.sb([128, 128], F32, "id_f")
    c["id_b"] = cx.sb([128, 128], BF16, "id_b")
    cx.memset("gpsimd", c["ones_f"].t[:], 1.0, [c["ones_f"].b])
    cx.memset("gpsimd", c["ones_b"].t[:], 1.0, [c["ones_b"].b])
    cx.memset("gpsimd", c["id_f"].t[:], 0.0, [c["id_f"].b])
    idf = c["id_f"]
    cx.fw.op("gpsimd", lambda e: e.affine_select(out=idf.t[:], in_=idf.t[:], compare_op=ALU.not_equal,
                                                 fill=1.0, base=0, pattern=[[-1, 128]], channel_multiplier=1),
             [idf.b], [idf.b])
    cx.copy("gpsimd", c["id_b"].t[:], idf.t[:], [idf.b], [c["id_b"].b])
    return c


def rmsnorm_stats(cx, c, xt, xb, ncols, psum, sqpool, tmp_pool, nfeat_chunks=8, nfeat=1024.0):
    ps = psum.get()
    for kc in range(nfeat_chunks):
        sq = sqpool.get()
        cx.tt("gpsimd", sq.t[:, :ncols], xt(kc), xt(kc), ALU.mult, [xb], [sq.b])
        cx.mm(ps.t[:, :ncols], c["ones_f"].t[:, :], sq.t[:, :ncols], kc == 0, kc == nfeat_chunks - 1,
              [sq.b, c["ones_f"].b], [ps.b])
    ln = tmp_pool.get()
    cx.act(ln.t[:, :ncols], ps.t[:, :ncols], AF.Ln, [ps.b], [ln.b], bias=c["eps"].t[:, 0:1], scale=1.0 / nfeat)
    rstd = tmp_pool.get()
    cx.act(rstd.t[:, :ncols], ln.t[:, :ncols], AF.Exp, [ln.b], [rstd.b], scale=-0.5)
    return rstd


def add_eps(cx, c):
    c["eps"] = cx.sb([128, 1], F32, "eps")
    cx.memset("gpsimd", c["eps"].t[:], EPS, [c["eps"].b])


FM_BASES = [0, 512, 1024, 2048, 2568, 3080]


def emit_phase_a(cx, c, Tc, xT, g_in, w_in, PFM, GATES, VTOK, GTOK, FFM, psum, xch=None):
    NB = Tc // 512
    xTv = xT.rearrange("(kc p) t -> p kc t", p=128)
    wv = w_in.rearrange("(kc p) n -> p kc n", p=128)
    with contextlib.ExitStack() as st:
        old, cx.st = cx.st, st
        gt = cx.sb([128, 8], F32, "a_g")
        cx.dma("sync", gt.t[:], g_in[:, :], (), [gt.b])
        hT = cx.sb([128, 8, Tc], BF16, "a_hT")
        bh = [Buf(f"hT{b}") for b in range(NB)]
        xpool = cx.sbpool(2, [128, 8, 512], F32, "a_x")
        sqpool = cx.sbpool(3, [128, 512], F32, "a_sq")
        tmp = cx.sbpool(4, [128, 512], F32, "a_tmp")
        wpool = cx.sbpool(3, [128, 8, 512], BF16, "a_w")
        stpool = cx.sbpool(3, [128, Tc], BF16, "a_st")
        for blk in range(NB):
            x = xpool.get()
            cx.dma("sync", x.t[:], xTv[:, :, blk * 512:(blk + 1) * 512], (), [x.b])
            rstd = rmsnorm_stats(cx, c, lambda kc: x.t[:, kc, :], x.b, 512, psum, sqpool, tmp)
            for kc in range(8):
                cx.stt(hT.t[:, kc, blk * 512:(blk + 1) * 512], x.t[:, kc, :], gt.t[:, kc:kc + 1], rstd.t[:, :],
                       ALU.mult, ALU.mult, [x.b, gt.b, rstd.b], [bh[blk]])
        jobs = []
        for g in range(2):
            for j, base in enumerate(FM_BASES):
                jobs.append((base + g * 256, 256, "pfm", (g, j * 256)))
        gate_jobs = [(4112 + n * 512, 512, "gate", n * 512) for n in range(6)]
        ev = 0

        def flush_all():
            if xch:
                for x_ in xch.values():
                    x_.flush()

        def run_jobs(jobs):
          nonlocal ev
          for (c0, ncols, kind, dest) in jobs:
            w = wpool.get()
            cx.dma("gpsimd", w.t[:, :, :ncols], wv[:, :, c0:c0 + ncols], (), [w.b])
            flush_all()
            for sub in range(ncols // 128):
                stg = stpool.get()
                for blk in range(NB):
                    ps = psum.get()
                    for kc in range(8):
                        cx.mm(ps.t[:, :], w.t[:, kc, sub * 128:(sub + 1) * 128], hT.t[:, kc, blk * 512:(blk + 1) * 512],
                              kc == 0, kc == 7, [w.b, bh[blk]], [ps.b])
                    o = stg.t[:, blk * 512:(blk + 1) * 512]
                    if kind == "gate":
                        cx.act(o, ps.t[:, :], AF.Sigmoid, [ps.b], [stg.b])
                    else:
                        cx.copy("vector" if ev % 2 == 0 else "scalar", o, ps.t[:, :], [ps.b], [stg.b])
                        ev += 1
                if kind == "gate":
                    dst = GATES[dest + sub * 128:dest + (sub + 1) * 128, :]
                    cx.dma("sync", dst, stg.t[:, :], [stg.b], ())
                else:
                    dst = PFM[dest[0], dest[1] + sub * 128:dest[1] + (sub + 1) * 128, :]
                    db_ = Buf("pfmrow")
                    cx.dma("sync", dst, stg.t[:, :], [stg.b], [db_])
                    if xch:
                        r0_ = dest[0] * 1536 + dest[1] + sub * 128
                        xch["pfm"].wrote(r0_, r0_ + 128, Tc, db_)
        run_jobs(jobs)
        vst = cx.sbpool(2, [128, 4, 512], BF16, "a_vst")
        for g in range(2):
            w = wpool.get()
            cx.dma("gpsimd", w.t[:, :, 0:256], wv[:, :, 1536 + g * 256:1536 + (g + 1) * 256], (), [w.b])
            cx.dma("gpsimd", w.t[:, :, 256:512], wv[:, :, 3592 + g * 256:3592 + (g + 1) * 256], (), [w.b])
            vv = VTOK[g].rearrange("(n p) c -> p n c", p=128)
            for t4 in range(Tc // 512):
                stg = vst.get()
                for ti in range(4):
                    tt_ = t4 * 4 + ti
                    ps = psum.get()
                    for kc in range(8):
                        cx.mm(ps.t[:, :], hT.t[:, kc, tt_ * 128:(tt_ + 1) * 128], w.t[:, kc, :], kc == 0, kc == 7,
                              [w.b, bh[tt_ // 4]], [ps.b])
                    cx.copy("vector" if ti % 2 == 0 else "scalar", stg.t[:, ti, :], ps.t[:, :], [ps.b], [stg.b])
                db_ = Buf("vtokrow")
                cx.dma("sync", vv[:, t4 * 4:(t4 + 1) * 4, :], stg.t[:, :, :], [stg.b], [db_])
                if xch:
                    xch["vtok"].wrote(g * Tc + t4 * 512, g * Tc + (t4 + 1) * 512, 512, db_)
                    flush_all()
        w8 = cx.sb([128, 8, 16], BF16, "a_w8")
        for g in range(2):
            cx.dma("gpsimd", w8.t[:, :, g * 4:g * 4 + 2], wv[:, :, 2560 + 2 * g:2560 + 2 * g + 2], (), [w8.b])
            cx.dma("gpsimd", w8.t[:, :, g * 4 + 2:g * 4 + 4], wv[:, :, 2564 + 2 * g:2564 + 2 * g + 2], (), [w8.b])
        cx.dma("gpsimd", w8.t[:, :, 8:16], wv[:, :, 4104:4112], (), [w8.b])
        NT = Tc // 128
        gacc = cx.sb([128, NT, 8], F32, "a_gacc")
        ps = psum.get()
        for tt_ in range(NT):
            for kc in range(8):
                cx.mm(ps.t[:, tt_ * 8:(tt_ + 1) * 8], hT.t[:, kc, tt_ * 128:(tt_ + 1) * 128], w8.t[:, kc, 0:8],
                      kc == 0, kc == 7, [w8.b, bh[tt_ // 4]], [ps.b])
        cx.copy("vector", gacc.t[:, :, :], ps.t[:, 0:NT * 8].rearrange("p (n c) -> p n c", c=8), [ps.b], [gacc.b])
        for g in range(2):
            db_ = Buf("gtokrow")
            cx.dma("sync", GTOK[g], gacc.t[:, :, g * 4:(g + 1) * 4], [gacc.b], [db_])
            if xch:
                xch["gtok"].wrote(g * 128, (g + 1) * 128, (Tc // 128) * 4, db_)
        fst = cx.sb([8, Tc], F32, "a_fst")
        for blk in range(NB):
            ps = psum.get()
            for kc in range(8):
                cx.mm(ps.t[0:8, :], w8.t[:, kc, 8:16], hT.t[:, kc, blk * 512:(blk + 1) * 512], kc == 0, kc == 7,
                      [w8.b, bh[blk]], [ps.b])
            cx.copy("vector", fst.t[0:8, blk * 512:(blk + 1) * 512], ps.t[0:8, :], [ps.b], [fst.b])
        db_ = Buf("ffmrow")
        cx.dma("sync", FFM.rearrange("g h t -> (g h) t"), fst.t[:, :], [fst.b], [db_])
        if xch:
            xch["ffm"].wrote(0, 8, Tc, db_)
        flush_all()
        run_jobs(gate_jobs)
        cx.fw.barrier()
        cx.st = old


def build_phase_a(Tc):
    nc = bass.Bass("TRN2", target_bir_lowering=False)
    xT = dram(nc, "xT", [D, Tc], F32, "ExternalInput")
    g_in = dram(nc, "norm_g", [128, 8], F32, "ExternalInput")
    w_in = dram(nc, "w_in", [D, IN_W], F32, "ExternalInput")
    PFM = dram(nc, "pfm", [2, 1536, Tc], BF16, "ExternalOutput")
    GATES = dram(nc, "gates", [3072, Tc], BF16, "ExternalOutput")
    VTOK = dram(nc, "vtok", [2, Tc, 512], BF16, "ExternalOutput")
    GTOK = dram(nc, "gtok", [2, 128, Tc // 128, 4], F32, "ExternalOutput")
    FFM = dram(nc, "ffm", [2, 4, Tc], F32, "ExternalOutput")
    cx = Cx(nc)
    c = make_consts(cx)
    add_eps(cx, c)
    psum = cx.psum_banks(8)
    emit_phase_a(cx, c, Tc, xT, g_in, w_in, PFM, GATES, VTOK, GTOK, FFM, psum)
    cx.finish()
    return nc


TS = 1024


DBG = None


def emit_phase_c(cx, c, Tc, xT, MIN, GATES, w_br, w_out, ffn_g, ffw, outT, psum, moe=False, final_g=None, dyn_min=0,
                 wq="gpsimd", wbuf16=None):
    wrd = [wbuf16] if wbuf16 is not None else []
    F = D_FFE if moe else D_FF
    NFC = F // 128
    NSB = Tc // TS
    NBK = TS // 512
    xTv = xT.rearrange("(kc p) t -> p kc t", p=128)
    oTv = outT.rearrange("(kc p) t -> p kc t", p=128)
    gav = GATES.rearrange("(br oc p) t -> p br oc t", br=3, oc=8, p=128)
    with contextlib.ExitStack() as st:
        old, cx.st = cx.st, st
        gn = cx.sb([128, 8], F32, "c_gn")
        cx.dma("sync", gn.t[:], ffn_g[:, :], (), [gn.b])
        if final_g is not None:
            gf = cx.sb([128, 8], F32, "c_gf")
            cx.dma("sync", gf.t[:], final_g[:, :], (), [gf.b])
        xt = cx.sb([128, 8, TS], F32, "c_x")
        h2 = cx.sb([128, 8, TS], BF16, "c_h2")
        R = cx.sb([128, 28, TS], BF16, "c_R")
        bR = [Buf("R_mt"), Buf("R_mg"), Buf("R_rest")]

        def rbuf(ch):
            return bR[0] if ch < 12 else (bR[1] if ch < 20 else bR[2])
        W = cx.sb([128, 3 * 8192], BF16, "c_W")
        bW = [Buf("W0"), Buf("W1"), Buf("W2")]
        wbr = W.t[:, 0:12288].rearrange("p (q c) -> p q c", c=1024)
        wout = W.t[:, 12288:20480].rearrange("p (q c) -> p q c", c=1024)
        wrot = Rot([(W.t[:, i * 8192:(i + 1) * 8192], bW[i]) for i in range(3)])
        gpool = cx.sbpool(2, [128, 3, 512], BF16, "c_gt")
        sqpool = cx.sbpool(3, [128, 512], F32, "c_sq")
        tmp = cx.sbpool(5, [128, 512], F32, "c_tmp")
        if moe:
            w_router, b_router = ffw[0], ffw[1]
            wr = cx.sb([128, 8, 8], F32, "c_wr")
            cx.dma("sync", wr.t[:], w_router.rearrange("(kc p) e -> p kc e", p=128), (), [wr.b])
            for kc in range(8):
                cx.ts("vector", wr.t[:, kc, :], wr.t[:, kc, :], gn.t[:, kc:kc + 1], ALU.mult, [wr.b, gn.b], [wr.b])
            br_ = cx.sb([8, 1], F32, "c_br")
            cx.dma("sync", br_.t[:], b_router[:, :], (), [br_.b])
            SEL = cx.sb([8, 8, 128], F32, "c_sel")
            cx.memset("gpsimd", SEL.t[:], 0.0, [SEL.b])
            cx.fw.op("gpsimd", lambda e: e.affine_select(out=SEL.t[:], in_=SEL.t[:], compare_op=ALU.not_equal,
                                                         fill=1.0, base=0, pattern=[[-1, 8], [0, 128]],
                                                         channel_multiplier=1), [SEL.b], [SEL.b])
            LT = cx.sb([8, TS], F32, "c_LT")
            GT = cx.sb([8, TS], F32, "c_GT")
            gbpool = cx.sbpool(2, [128, TS], F32, "c_gb")
            small = cx.sbpool(12, [128, 8], F32, "c_small")
            CAP = int(os.environ.get("MOE_CAP", "384"))
            NST = CAP // 128
            CUM = cx.sb([8, TS], F32, "c_cum")
            posb = cx.sb([128, TS], F32, "c_posb")
            PM = cx.sb([128, 8, 16], F32, "c_pm")
            IOTA1 = cx.sb([128, CAP], F32, "c_iota")
            SLOT = cx.sb([128, NST], F32, "c_slot")
            cx.fw.op("gpsimd", lambda e: e.iota(IOTA1.t[:, :], pattern=[[1, CAP]], base=1, channel_multiplier=0,
                                                allow_small_or_imprecise_dtypes=True), (), [IOTA1.b])
            cx.fw.op("gpsimd", lambda e: e.iota(SLOT.t[:, :], pattern=[[128, NST]], base=1, channel_multiplier=1,
                                                allow_small_or_imprecise_dtypes=True), (), [SLOT.b])
            flg = cx.sb([1, 8], F32, "c_flg")
            flgi = cx.sb([1, 1], mybir.dt.int32, "c_flgi")
            cx.n += 1
            pTb = cx.st.enter_context(cx.nc.psum_tensor(f"c_psT{cx.n}", [128, 1024], BF16))
            bpT = Buf("c_pT")
            Rf = R.t[:, :, :].rearrange("p a b -> p (a b)")
            ACTE = Rf[:, 0:28 * CAP].rearrange("p (a b) -> p a b", b=CAP)
            H2T = Rf[:, 10752:10752 + 8192].rearrange("p (a b) -> p a b", b=1024)
            SELt = Rf[:, 18944:18944 + 8 * CAP].rearrange("p (a b) -> p a b", b=CAP)
            SELT = Rf[:, 22016:22016 + NST * 1024].rearrange("p (a b) -> p a b", b=1024)
            Xf = h2.t[:, :, :].rearrange("p a b -> p (a b)")
            H2E = Xf[:, 0:8 * CAP].rearrange("p (a b) -> p a b", b=CAP)
            YE = Xf[:, 3072:3072 + NST * 1024].rearrange("p (a b) -> p a b", b=1024)
        for sb in range(NSB):
            t0 = sb * TS
            cx.dma("sync", xt.t[:], xTv[:, :, t0:t0 + TS], (), [xt.b])
            for g_ in range(2):
                cx.dma("sync", R.t[:, g_ * 6:(g_ + 1) * 6, :], MIN[g_].rearrange("(j p) t -> p j t", p=128)[:, :, t0:t0 + TS],
                       (), [bR[0]], dyn=dyn_min)
            for br in range(3):
                cx.dma("gpsimd", wbr[:, br * 4:(br + 1) * 4, :], w_br[br].rearrange("(n p) c -> p n c", p=128), (),
                       [bW[0], bW[1]])
            cx.dma("gpsimd", wout, w_out.rearrange("(kc p) c -> p kc c", p=128), (), [bW[1], bW[2]])
            for oc in range(8):
                for blk in range(NBK):
                    bs = slice(blk * 512, (blk + 1) * 512)
                    gt = gpool.get()
                    cx.dma("sync", gt.t[:], gav[:, :, oc, t0 + blk * 512:t0 + (blk + 1) * 512], (), [gt.b])
                    pp = []
                    for br in range(3):
                        ps = psum.get()
                        for n in range(4):
                            ch = (n // 2) * 6 + 2 * br + (n % 2)
                            cx.mm(ps.t[:, :], wbr[:, br * 4 + n, oc * 128:(oc + 1) * 128], R.t[:, ch, bs],
                                  n == 0, n == 3, [bW[0], bW[1], bR[0]], [ps.b])
                        pp.append(ps)
                    t1, t2, t3 = tmp.get(), tmp.get(), tmp.get()
                    cx.tt("vector", t1.t[:, :], pp[0].t[:, :], gt.t[:, 0, :], ALU.mult, [pp[0].b, gt.b], [t1.b])
                    cx.tt("vector", t2.t[:, :], pp[1].t[:, :], gt.t[:, 1, :], ALU.mult, [pp[1].b, gt.b], [t2.b])
                    cx.tt("vector", t3.t[:, :], pp[2].t[:, :], gt.t[:, 2, :], ALU.mult, [pp[2].b, gt.b], [t3.b])
                    cx.tt("gpsimd", t1.t[:, :], t1.t[:, :], t2.t[:, :], ALU.add, [t1.b, t2.b], [t1.b])
                    cx.tt("gpsimd", R.t[:, 12 + oc, bs], t1.t[:, :], t3.t[:, :], ALU.add, [t1.b, t3.b], [bR[1]])
            for oc in range(8):
                for blk in range(NBK):
                    bs = slice(blk * 512, (blk + 1) * 512)
                    ps = psum.get()
                    for kc in range(8):
                        cx.mm(ps.t[:, :], wout[:, kc, oc * 128:(oc + 1) * 128], R.t[:, 12 + kc, bs], kc == 0, kc == 7,
                              [bW[1], bW[2], bR[1]], [ps.b])
                    cx.tt("vector", xt.t[:, oc, bs], xt.t[:, oc, bs], ps.t[:, :], ALU.add, [ps.b, xt.b], [xt.b])
            for blk in range(NBK):
                bs = slice(blk * 512, (blk + 1) * 512)
                rstd = rmsnorm_stats(cx, c, lambda kc: xt.t[:, kc, bs], xt.b, 512, psum, sqpool, tmp)
                for kc in range(8):
                    cx.stt(h2.t[:, kc, bs], xt.t[:, kc, bs], gn.t[:, kc:kc + 1], rstd.t[:, :], ALU.mult, ALU.mult,
                           [xt.b, gn.b, rstd.b], [h2.b])
                if moe:
                    ps = psum.get()
                    for kc in range(8):
                        cx.mm(ps.t[0:8, :], wr.t[:, kc, :], xt.t[:, kc, bs], kc == 0, kc == 7, [wr.b, xt.b], [ps.b])
                    cx.tt("vector", LT.t[0:8, bs], ps.t[0:8, :], rstd.t[0:8, :], ALU.mult, [ps.b, rstd.b], [LT.b])
                    cx.ts("vector", LT.t[0:8, bs], LT.t[0:8, bs], br_.t[0:8, 0:1], ALU.add, [LT.b, br_.b], [LT.b])
                    pg = psum.get()
                    for ti in range(4):
                        cs = slice(blk * 512 + ti * 128, blk * 512 + (ti + 1) * 128)
                        pl = psum.get()
                        cx.transpose(pl.t[:, 0:8], LT.t[0:8, cs], c["id_f"].t[0:8, 0:8], [LT.b, c["id_f"].b], [pl.b])
                        lg, m1, eq, lg2, m2, sel, nm1, ex, w_, den, G = [small.get() for _ in range(11)]
                        cx.copy("vector", lg.t[:, :], pl.t[:, 0:8], [pl.b], [lg.b])
                        cx.fw.op("vector", lambda e, m1=m1, lg=lg: e.tensor_reduce(out=m1.t[:, 0:1], in_=lg.t[:, :], axis=AX.X, op=ALU.max), [lg.b], [m1.b])
                        cx.ts("vector", eq.t[:, :], lg.t[:, :], m1.t[:, 0:1], ALU.is_equal, [lg.b, m1.b], [eq.b])
                        cx.stt(lg2.t[:, :], eq.t[:, :], -1e30, lg.t[:, :], ALU.mult, ALU.add, [eq.b, lg.b], [lg2.b])
                        cx.fw.op("vector", lambda e, m2=m2, lg2=lg2: e.tensor_reduce(out=m2.t[:, 0:1], in_=lg2.t[:, :], axis=AX.X, op=ALU.max), [lg2.b], [m2.b])
                        cx.ts("vector", sel.t[:, :], lg.t[:, :], m2.t[:, 0:1], ALU.is_ge, [lg.b, m2.b], [sel.b])
                        cx.ts("vector", nm1.t[:, 0:1], m1.t[:, 0:1], -1.0, ALU.mult, [m1.b], [nm1.b])
                        cx.act(ex.t[:, :], lg.t[:, :], AF.Exp, [lg.b, nm1.b], [ex.b], bias=nm1.t[:, 0:1])
                        cx.tt("vector", w_.t[:, :], ex.t[:, :], sel.t[:, :], ALU.mult, [ex.b, sel.b], [w_.b])
                        cx.fw.op("vector", lambda e, den=den, w_=w_: e.tensor_reduce(out=den.t[:, 0:1], in_=w_.t[:, :], axis=AX.X, op=ALU.add), [w_.b], [den.b])
                        cx.fw.op("vector", lambda e, den=den: e.reciprocal(out=den.t[:, 0:1], in_=den.t[:, 0:1]), [den.b], [den.b])
                        cx.ts("vector", G.t[:, :], w_.t[:, :], den.t[:, 0:1], ALU.mult, [w_.b, den.b], [G.b])
                        cx.transpose(pg.t[0:8, ti * 128:(ti + 1) * 128], G.t[:, 0:8], c["id_f"].t[:, :], [G.b, c["id_f"].b], [pg.b])
                    cx.copy("vector", GT.t[0:8, bs], pg.t[0:8, :], [pg.b], [GT.b])
                if moe and DBG is not None:
                    cx.dma("sync", DBG[0:8, t0:t0 + TS], GT.t[0:8, :], [GT.b], ())
                    cx.dma("sync", DBG[8:16, t0:t0 + TS], LT.t[0:8, :], [LT.b], ())
            def dense_ffn():
                for e_ in range(NEXP if moe else 1):
                    if moe:
                        wg_v = ffw[2][e_].rearrange("(kc p) n -> p kc n", p=128)
                        wu_v = ffw[3][e_].rearrange("(kc p) n -> p kc n", p=128)
                        wd_v = ffw[4][e_].rearrange("(kc p) c -> p kc c", p=128)
                        gb = gbpool.get()
                        for blk in range(NBK):
                            bs = slice(blk * 512, (blk + 1) * 512)
                            ps = psum.get()
                            cx.mm(ps.t[:, :], SEL.t[0:8, e_, :], GT.t[0:8, bs], True, True, [SEL.b, GT.b], [ps.b])
                            cx.copy("vector", gb.t[:, bs], ps.t[:, :], [ps.b], [gb.b])
                    else:
                        wg_v = ffw[0].rearrange("(kc p) n -> p kc n", p=128)
                        wu_v = ffw[1].rearrange("(kc p) n -> p kc n", p=128)
                        wd_v = ffw[2].rearrange("(kc p) c -> p kc c", p=128)
                    for s0 in range(0, F, 512):
                        ncol = min(512, F - s0)
                        wt, wb = wrot.get()
                        wgt = wt[:, 0:4096].rearrange("p (k c) -> p k c", c=512)
                        wut = wt[:, 4096:8192].rearrange("p (k c) -> p k c", c=512)
                        cx.dma(wq, wgt[:, :, :ncol], wg_v[:, :, s0:s0 + ncol], wrd, [wb])
                        cx.dma(wq, wut[:, :, :ncol], wu_v[:, :, s0:s0 + ncol], wrd, [wb])
                        for sub in range(ncol // 128):
                            ch = s0 // 128 + sub
                            for blk in range(NBK):
                                bs = slice(blk * 512, (blk + 1) * 512)
                                pg_, pu_ = psum.get(), psum.get()
                                for kc in range(8):
                                    cx.mm(pg_.t[:, :], wgt[:, kc, sub * 128:(sub + 1) * 128], h2.t[:, kc, bs], kc == 0, kc == 7,
                                          [wb, h2.b], [pg_.b])
                                for kc in range(8):
                                    cx.mm(pu_.t[:, :], wut[:, kc, sub * 128:(sub + 1) * 128], h2.t[:, kc, bs], kc == 0, kc == 7,
                                          [wb, h2.b], [pu_.b])
                                sg = tmp.get()
                                cx.act(sg.t[:, :], pg_.t[:, :], AF.Silu, [pg_.b], [sg.b])
                                cx.tt("vector", R.t[:, ch, bs], pu_.t[:, :], sg.t[:, :], ALU.mult, [pu_.b, sg.b], [rbuf(ch)])
                    KGS = 7 if NFC % 7 == 0 else 11
                    NG = NFC // KGS
                    for blk in range(NBK):
                        bs = slice(blk * 512, (blk + 1) * 512)
                        for och in range(2):
                            accs = [psum.get() for _ in range(4)]
                            for kg in range(NG):
                                wt, wb = wrot.get()
                                wdt = wt[:, 0:KGS * 512].rearrange("p (k c) -> p k c", c=512)
                                cx.dma(wq, wdt, wd_v[:, kg * KGS:(kg + 1) * KGS, och * 512:(och + 1) * 512], wrd, [wb])
                                for o4 in range(4):
                                    for k in range(KGS):
                                        kc = kg * KGS + k
                                        cx.mm(accs[o4].t[:, :], wdt[:, k, o4 * 128:(o4 + 1) * 128], R.t[:, kc, bs], kc == 0,
                                              kc == NFC - 1, [wb, rbuf(kc)], [accs[o4].b])
                            for o4 in range(4):
                                oc = och * 4 + o4
                                ps = accs[o4]
                                if moe:
                                    tm = tmp.get()
                                    cx.tt("vector", tm.t[:, :], ps.t[:, :], gb.t[:, bs], ALU.mult, [ps.b, gb.b], [tm.b])
                                    cx.tt("vector", xt.t[:, oc, bs], xt.t[:, oc, bs], tm.t[:, :], ALU.add, [tm.b, xt.b], [xt.b])
                                else:
                                    cx.tt("vector", xt.t[:, oc, bs], xt.t[:, oc, bs], ps.t[:, :], ALU.add, [ps.b, xt.b], [xt.b])

            def routed_ffn():
                bh2t, bpm, bsel, bselt, bh2e, bacte, bye = [Buf(n_) for n_ in "h2t pm sel selt h2e acte ye".split()]
                for tile in range(8):
                    for kc in range(8):
                        cx.transpose(pTb[:, kc * 128:(kc + 1) * 128], h2.t[:, kc, tile * 128:(tile + 1) * 128], c["id_b"].t[:, :],
                                     [h2.b, c["id_b"].b], [bpT])
                    cx.copy("vector" if tile % 2 == 0 else "scalar", H2T[:, tile, :], pTb[:, :], [bpT], [bh2t])
                for tile in range(8):
                    ps = psum.get()
                    cx.transpose(ps.t[:, 0:8], CUM.t[0:8, tile * 128:(tile + 1) * 128], c["id_f"].t[0:8, 0:8], [CUM.b, c["id_f"].b], [ps.b])
                    cx.transpose(ps.t[:, 8:16], LT.t[0:8, tile * 128:(tile + 1) * 128], c["id_f"].t[0:8, 0:8], [LT.b, c["id_f"].b], [ps.b])
                    cx.copy("vector", PM.t[:, tile, :], ps.t[:, 0:16], [ps.b], [PM.b])
                cx.fw.barrier()
                for e_ in range(NEXP):
                    wg_v = ffw[2][e_].rearrange("(kc p) n -> p kc n", p=128)
                    wu_v = ffw[3][e_].rearrange("(kc p) n -> p kc n", p=128)
                    wd_v = ffw[4][e_].rearrange("(kc p) c -> p kc c", p=128)
                    gb = gbpool.get()
                    for blk in range(NBK):
                        bs = slice(blk * 512, (blk + 1) * 512)
                        ps = psum.get()
                        cx.mm(ps.t[:, :], SEL.t[0:8, e_, :], GT.t[0:8, bs], True, True, [SEL.b, GT.b], [ps.b])
                        cx.copy("scalar", gb.t[:, bs], ps.t[:, :], [ps.b], [gb.b])
                        ps = psum.get()
                        cx.mm(ps.t[:, :], SEL.t[0:8, e_, :], CUM.t[0:8, bs], True, True, [SEL.b, CUM.b], [ps.b])
                        cx.copy("scalar", posb.t[:, bs], ps.t[:, :], [ps.b], [posb.b])
                    for tile in range(8):
                        cx.ts("vector", SELt[:, tile, :], IOTA1.t[:, :], PM.t[:, tile, e_:e_ + 1], ALU.is_equal,
                              [IOTA1.b, PM.b], [bsel], s2=PM.t[:, tile, 8 + e_:9 + e_], op1=ALU.mult)
                    for st_ in range(NST):
                        cx.stt(SELT[:, st_, :], posb.t[:, :], SLOT.t[:, st_:st_ + 1], gb.t[:, :], ALU.is_equal, ALU.mult,
                               [posb.b, SLOT.b, gb.b], [bselt])
                    for kc in range(8):
                        ps = psum.get()
                        for tile in range(8):
                            cx.mm(ps.t[:, 0:CAP], H2T[:, tile, kc * 128:(kc + 1) * 128], SELt[:, tile, :], tile == 0, tile == 7,
                                  [bh2t, bsel], [ps.b])
                        cx.copy("vector" if kc % 2 == 0 else "scalar", H2E[:, kc, :], ps.t[:, 0:CAP], [ps.b], [bh2e])
                    for s0 in range(0, F, 512):
                        wt, wb = wrot.get()
                        wgt = wt[:, 0:4096].rearrange("p (k c) -> p k c", c=512)
                        wut = wt[:, 4096:8192].rearrange("p (k c) -> p k c", c=512)
                        cx.dma(wq, wgt, wg_v[:, :, s0:s0 + 512], wrd, [wb])
                        cx.dma(wq, wut, wu_v[:, :, s0:s0 + 512], wrd, [wb])
                        for sub in range(4):
                            ch = s0 // 128 + sub
                            pg_, pu_ = psum.get(), psum.get()
                            for kc in range(8):
                                cx.mm(pg_.t[:, 0:CAP], wgt[:, kc, sub * 128:(sub + 1) * 128], H2E[:, kc, :], kc == 0, kc == 7,
                                      [wb, bh2e], [pg_.b])
                            for kc in range(8):
                                cx.mm(pu_.t[:, 0:CAP], wut[:, kc, sub * 128:(sub + 1) * 128], H2E[:, kc, :], kc == 0, kc == 7,
                                      [wb, bh2e], [pu_.b])
                            sg = tmp.get()
                            cx.act(sg.t[:, 0:CAP], pg_.t[:, 0:CAP], AF.Silu, [pg_.b], [sg.b])
                            cx.tt("vector", ACTE[:, ch, :], pu_.t[:, 0:CAP], sg.t[:, 0:CAP], ALU.mult, [pu_.b, sg.b], [bacte])
                    for fh in range(2):
                        accs = [psum.get() for _ in range(NST)]
                        for kg in range(4):
                            wt, wb = wrot.get()
                            wdt = wt[:, 0:7 * 512].rearrange("p (k c) -> p k c", c=512)
                            cx.dma(wq, wdt, wd_v[:, kg * 7:(kg + 1) * 7, fh * 512:(fh + 1) * 512], wrd, [wb])
                            for st_ in range(NST):
                                for k in range(7):
                                    kc = kg * 7 + k
                                    cx.mm(accs[st_].t[:, :], ACTE[:, kc, st_ * 128:(st_ + 1) * 128], wdt[:, k, :], kc == 0, kc == 27,
                                          [wb, bacte], [accs[st_].b])
                        for st_ in range(NST):
                            cx.copy("scalar" if st_ % 2 == 0 else "vector", YE[:, st_, fh * 512:(fh + 1) * 512], accs[st_].t[:, :],
                                    [accs[st_].b], [bye])
                    for fc in range(8):
                        for blk in range(NBK):
                            bs = slice(blk * 512, (blk + 1) * 512)
                            ps = psum.get()
                            for st_ in range(NST):
                                cx.mm(ps.t[:, :], YE[:, st_, fc * 128:(fc + 1) * 128], SELT[:, st_, bs], st_ == 0, st_ == NST - 1,
                                      [bye, bselt], [ps.b])
                            cx.tt("vector", xt.t[:, fc, bs], xt.t[:, fc, bs], ps.t[:, :], ALU.add, [ps.b, xt.b], [xt.b])

            if moe and os.environ.get("MOE_DENSE") is None:
                cx.ts("vector", LT.t[0:8, :], GT.t[0:8, :], 0.0, ALU.is_gt, [GT.b], [LT.b])
                cx.fw.op("vector", lambda e: e.tensor_tensor_scan(out=CUM.t[0:8, :], data0=LT.t[0:8, :], data1=LT.t[0:8, :],
                                                                  initial=0.0, op0=ALU.add, op1=ALU.max), [LT.b], [CUM.b])
                ps = psum.get()
                cx.transpose(ps.t[0:1, 0:8], CUM.t[0:8, TS - 1:TS], c["id_f"].t[0:8, 0:8], [CUM.b, c["id_f"].b], [ps.b])
                cx.copy("vector", flg.t[0:1, 0:8], ps.t[0:1, 0:8], [ps.b], [flg.b])
                cx.fw.op("vector", lambda e: e.tensor_reduce(out=flg.t[0:1, 0:1], in_=flg.t[0:1, 0:8], axis=AX.X, op=ALU.max),
                         [flg.b], [flg.b])
                cx.ts("vector", flg.t[0:1, 0:1], flg.t[0:1, 0:1], float(CAP) + 0.5, ALU.is_lt, [flg.b], [flg.b])
                cx.copy("vector", flgi.t[0:1, 0:1], flg.t[0:1, 0:1], [flg.b], [flgi.b])
                cx.fw.begin_if(flgi.t[0:1, 0:1])
                routed_ffn()
                cx.fw.begin_else()
                dense_ffn()
                cx.fw.end_if()
            else:
                dense_ffn()
            if final_g is not None:
                for blk in range(NBK):
                    bs = slice(blk * 512, (blk + 1) * 512)
                    rstd = rmsnorm_stats(cx, c, lambda kc: xt.t[:, kc, bs], xt.b, 512, psum, sqpool, tmp)
                    for kc in range(8):
                        cx.stt(xt.t[:, kc, bs], xt.t[:, kc, bs], gf.t[:, kc:kc + 1], rstd.t[:, :], ALU.mult, ALU.mult,
                               [xt.b, gf.b, rstd.b], [xt.b])
            cx.dma("sync", oTv[:, :, t0:t0 + TS], xt.t[:], [xt.b], ())
        cx.fw.barrier()
        cx.st = old


def precast_jobs(cx, pairs, buf):
    jobs = []
    for dst, src in pairs:
        E_, R_, C_ = src.shape
        for e in range(E_):
            for h in range(2):
                jobs.append(lambda dst=dst, src=src, e=e, h=h, R_=R_: cx.dma(
                    "gpsimd", dst[e, h * (R_ // 2):(h + 1) * (R_ // 2), :], src[e, h * (R_ // 2):(h + 1) * (R_ // 2), :], (), [buf]))
    return jobs


def emit_precast(cx, pairs, buf):
    for j in precast_jobs(cx, pairs, buf):
        j()


def build_phase_c(Tc, moe, final):
    nc = bass.Bass("TRN2", target_bir_lowering=False)
    F = D_FFE if moe else D_FF
    xT = dram(nc, "xT", [D, Tc], F32, "ExternalInput")
    MIN = dram(nc, "min", [2, 768, Tc], BF16, "ExternalInput")
    GATES = dram(nc, "gates", [3072, Tc], BF16, "ExternalInput")
    w_br = dram(nc, "w_br", [3, 512, D], F32, "ExternalInput")
    w_out = dram(nc, "w_out", [D, D], F32, "ExternalInput")
    ffn_g = dram(nc, "ffn_g", [128, 8], F32, "ExternalInput")
    if moe:
        ffw = (dram(nc, "w_router", [D, NEXP], F32, "ExternalInput"),
               dram(nc, "b_router", [NEXP, 1], F32, "ExternalInput"),
               dram(nc, "w_gate", [NEXP, D, F], F32, "ExternalInput"),
               dram(nc, "w_up", [NEXP, D, F], F32, "ExternalInput"),
               dram(nc, "w_down", [NEXP, F, D], F32, "ExternalInput"))
    else:
        ffw = (dram(nc, "w_gate", [D, F], F32, "ExternalInput"),
               dram(nc, "w_up", [D, F], F32, "ExternalInput"),
               dram(nc, "w_down", [F, D], F32, "ExternalInput"))
    final_g = dram(nc, "final_g", [128, 8], F32, "ExternalInput") if final else None
    outT = dram(nc, "outT", [D, Tc], F32, "ExternalOutput")
    cx = Cx(nc)
    c = make_consts(cx)
    add_eps(cx, c)
    psum = cx.psum_banks(7)
    if moe:
        N = lambda name, shape: nc.dram_tensor(name, list(shape), BF16).ap()
        g16, u16, d16 = N("w16g", [NEXP, D, F]), N("w16u", [NEXP, D, F]), N("w16d", [NEXP, F, D])
        b16 = Buf("w16")
        emit_precast(cx, [(g16, ffw[2]), (u16, ffw[3]), (d16, ffw[4])], b16)
        ffw = (ffw[0], ffw[1], g16, u16, d16)
        emit_phase_c(cx, c, Tc, xT, MIN, GATES, w_br, w_out, ffn_g, ffw, outT, psum, moe=moe, final_g=final_g, wq="sync", wbuf16=b16)
    else:
        emit_phase_c(cx, c, Tc, xT, MIN, GATES, w_br, w_out, ffn_g, ffw, outT, psum, moe=moe, final_g=final_g)
    cx.finish()
    return nc


def emit_phase_b(cx, c, Tc, PFM, VTOK, GTOK, FFM, pool_w, pool_scale, pool_coef, pool_rc, conv_w, ml_b, ml_ng,
                 fox_b, MOUT, psum, dyn=None, xch=None, pre_fox=None):
    dyn = dyn or {"pfm": 0, "vtok": 0, "gtok": 0, "ffm": 0}
    S = 2 * Tc
    NCH = S // 128
    nc = cx.nc
    banks = psum.items

    def loc(t):
        return t // Tc, t % Tc

    with contextlib.ExitStack() as st:
        old, cx.st = cx.st, st
        PB = min(1024, Tc)
        L = PB + 16
        pw_f = cx.sb([128, 2, 128], F32, "p_wf")
        pw = cx.sb([128, 2, 128], BF16, "p_w")
        cx.dma("gpsimd", pw.t[:], pool_w[:, :, :], (), [pw.b])
        psc = cx.sb([128, 2], F32, "p_sc")
        cx.dma("sync", psc.t[:], pool_scale[:, :], (), [psc.b])
        pco = cx.sb([128, 2, 4], F32, "p_co")
        cx.dma("sync", pco.t[:], pool_coef[:, :, :], (), [pco.b])
        prc = cx.sb([128, 2, 4, 16], F32, "p_rc")
        cx.dma("sync", prc.t[:], pool_rc[:, :, :, :], (), [prc.b])
        upool = cx.sbpool(3, [128, L], BF16, "p_u")
        wtsets = Rot([[cx.sb([128, L], F32, f"p_w{j}_{i}") for i in range(4)] for j in range(3)])
        dpool = cx.sbpool(3, [128, L], F32, "p_d")
        dbp = cx.sbpool(3, [128, PB], BF16, "p_db")
        opool = cx.sbpool(3, [128, PB], BF16, "p_o")
        t16 = cx.sbpool(4, [128, 16], F32, "p_t16")
        for gi in range(2):
            for t0 in range(0, S, PB):
                hf, tl = loc(t0)
                U = upool.get()
                wt = wtsets.get()
                cx.dma("sync", U.t[:, 16:L], PFM[hf, gi * 128:(gi + 1) * 128, tl:tl + PB], (), [U.b], dyn=dyn["pfm"])
                if t0 == 0:
                    cx.memset("gpsimd", U.t[:, 0:16], 0.0, [U.b])
                else:
                    hp, tp = loc(t0 - 16)
                    cx.dma("sync", U.t[:, 0:16], PFM[hp, gi * 128:(gi + 1) * 128, tp:tp + 16], (), [U.b], dyn=dyn["pfm"])
                src = U
                for k, sh in enumerate((1, 2, 4, 8)):
                    lo = 2 * sh - 1
                    cx.tt("gpsimd", wt[k].t[:, lo:L], src.t[:, lo:L], src.t[:, lo - sh:L - sh],
                          ALU.add, [src.b], [wt[k].b])
                    src = wt[k]
                d = dpool.get()
                cx.stt(d.t[:, 16:L], wt[0].t[:, 16:L], pco.t[:, gi, 0:1], U.t[:, 16:L], ALU.mult, ALU.subtract,
                       [wt[0].b, pco.b, U.b], [d.b])
                for k in range(1, 4):
                    cx.stt(d.t[:, 16:L], wt[k].t[:, 16:L], pco.t[:, gi, k:k + 1], d.t[:, 16:L], ALU.mult, ALU.add,
                           [wt[k].b, pco.b, d.b], [d.b])
                if t0 == 0:
                    a0 = t16.get()
                    cx.tt("vector", a0.t[:, :], wt[0].t[:, 16:32], prc.t[:, gi, 0, :], ALU.mult, [wt[0].b, prc.b], [a0.b])
                    for k in range(1, 4):
                        a1 = t16.get()
                        cx.tt("vector", a1.t[:, :], wt[k].t[:, 16:32], prc.t[:, gi, k, :], ALU.mult, [wt[k].b, prc.b], [a1.b])
                        cx.tt("vector", a0.t[:, :], a0.t[:, :], a1.t[:, :], ALU.add, [a0.b, a1.b], [a0.b])
                    cx.tt("vector", d.t[:, 16:32], a0.t[:, :], U.t[:, 16:32], ALU.subtract, [a0.b, U.b], [d.b])
                db = dbp.get()
                cx.copy("scalar", db.t[:, :], d.t[:, 16:L], [d.b], [db.b])
                o = opool.get()
                for blk in range(PB // 512):
                    ps = psum.get()
                    cx.mm(ps.t[:, :], pw.t[:, gi, :], db.t[:, blk * 512:(blk + 1) * 512], True, True, [pw.b, db.b], [ps.b])
                    cx.act(o.t[:, blk * 512:(blk + 1) * 512], ps.t[:, :], AF.Copy, [ps.b, psc.b], [o.b],
                           scale=psc.t[:, gi:gi + 1])
                db_ = Buf("moutrow")
                cx.dma("scalar", MOUT[hf, gi * 128:(gi + 1) * 128, tl:tl + PB], o.t[:, :], [o.b], [db_])
                if xch:
                    xch.wrote(hf * 768 + gi * 128, hf * 768 + (gi + 1) * 128, PB, db_)
        if xch:
            xch.flush()
        cx.fw.barrier()
        cx.st = old

    with contextlib.ExitStack() as st:
        old, cx.st = cx.st, st
        TRI = cx.sb([128, 128], F32, "m_tri")
        cx.memset("gpsimd", TRI.t[:], 1.0, [TRI.b])
        cx.fw.op("gpsimd", lambda e: e.affine_select(out=TRI.t[:], in_=TRI.t[:], compare_op=ALU.is_ge, fill=0.0, base=0,
                                                     pattern=[[1, 128]], channel_multiplier=-1), [TRI.b], [TRI.b])
        NTRI = cx.sb([128, 128], F32, "m_ntri")
        cx.memset("gpsimd", NTRI.t[:], 0.0, [NTRI.b])
        cx.fw.op("gpsimd", lambda e: e.affine_select(out=NTRI.t[:], in_=NTRI.t[:], compare_op=ALU.is_ge, fill=-30000.0, base=0,
                                                     pattern=[[1, 128]], channel_multiplier=-1), [NTRI.b], [NTRI.b])
        cw = cx.sb([128, 2, 2, 4], F32, "m_cw")
        cx.dma("sync", cw.t[:], conv_w[:, :, :, :], (), [cw.b])
        mlb = cx.sb([128, 4], F32, "m_b")
        cx.dma("sync", mlb.t[:], ml_b[:, :], (), [mlb.b])
        mng = cx.sb([128, 2], F32, "m_ng")
        cx.dma("sync", mng.t[:], ml_ng[:, :], (), [mng.b])
        QK = [[cx.sb([128, S], BF16, f"m_qk{a}{h}") for h in range(2)] for a in range(2)]
        V = cx.sb([128, NCH, 256], BF16, "m_v")
        for hf in range(2):
            cx.dma("sync", V.t[:, hf * (Tc // 128):(hf + 1) * (Tc // 128), :],
                   VTOK[hf].rearrange("(n p) c -> p n c", p=128)[:, :, 0:256], (), [V.b], dyn=dyn["vtok"])
        GT = cx.sb([128, NCH, 4], F32, "m_gt")
        for hf in range(2):
            cx.dma("sync", GT.t[:, hf * (Tc // 128):(hf + 1) * (Tc // 128), :], GTOK[hf], (), [GT.b], dyn=dyn["gtok"])
        for col in range(4):
            cx.ts("vector", GT.t[:, :, col:col + 1], GT.t[:, :, col:col + 1], mlb.t[:, col:col + 1], ALU.add,
                  [GT.b, mlb.b], [GT.b])
        LF = cx.sb([128, NCH, 2], F32, "m_lf")
        cx.act(LF.t[:, :, :], GT.t[:, :, 2:4], AF.Exp, [GT.b], [LF.b], scale=-1.0)
        cx.act(LF.t[:, :, :], LF.t[:, :, :], AF.Ln, [LF.b, c["ones_f"].b], [LF.b], bias=c["ones_f"].t[:, 0:1])
        cx.ts("vector", LF.t[:, :, :], LF.t[:, :, :], -1.0, ALU.mult, [LF.b], [LF.b])
        CB = min(2048, Tc)
        with contextlib.ExitStack() as st2:
            cx.st = st2
            rpool = cx.sbpool(2, [128, CB + 3], BF16, "m_raw")
            apool = cx.sbpool(2, [128, CB], F32, "m_acc")
            for a in range(2):
                for h in range(2):
                    for t0 in range(0, S, CB):
                        hf, tl = loc(t0)
                        r0 = 256 * (1 + a) + h * 128
                        raw = rpool.get()
                        cx.dma("sync", raw.t[:, 3:CB + 3], PFM[hf, r0:r0 + 128, tl:tl + CB], (), [raw.b], dyn=dyn["pfm"])
                        if t0 == 0:
                            cx.memset("gpsimd", raw.t[:, 0:3], 0.0, [raw.b])
                        else:
                            hp, tp = loc(t0 - 3)
                            cx.dma("sync", raw.t[:, 0:3], PFM[hp, r0:r0 + 128, tp:tp + 3], (), [raw.b], dyn=dyn["pfm"])
                        acc = apool.get()
                        cx.ts("vector", acc.t[:, :], raw.t[:, 3:CB + 3], cw.t[:, a, h, 3:4], ALU.mult, [raw.b, cw.b], [acc.b])
                        for tap in range(3):
                            cx.stt(acc.t[:, :], raw.t[:, tap:CB + tap], cw.t[:, a, h, tap:tap + 1], acc.t[:, :],
                                   ALU.mult, ALU.add, [raw.b, cw.b, acc.b], [acc.b])
                        if a == 0:
                            cx.act(QK[a][h].t[:, t0:t0 + CB], acc.t[:, :], AF.Silu, [acc.b], [QK[a][h].b])
                        else:
                            cx.act(acc.t[:, :], acc.t[:, :], AF.Silu, [acc.b], [acc.b])
                            cx.ts("gpsimd", QK[a][h].t[:, t0:t0 + CB], acc.t[:, :], 128.0 ** -0.5, ALU.mult, [acc.b],
                                  [QK[a][h].b])
            cx.fw.barrier()
            cx.st = st
        CT = [cx.sb([128, 256], F32, f"m_ct{h}") for h in range(2)]
        CTb = [cx.sb([128, 256], BF16, f"m_ctb{h}") for h in range(2)]
        for h in range(2):
            cx.memset("gpsimd", CT[h].t[:], 0.0, [CT[h].b])
            cx.memset("gpsimd", CTb[h].t[:], 0.0, [CTb[h].b])
        H = [cx.sbpool(2, [128, 512], F32, f"m_H{h}") for h in range(2)]
        sm = cx.sbpool(16, [128, 2], F32, "m_sm")
        f128 = cx.sbpool(18, [128, 128], F32, "m_f128")
        b128 = cx.sbpool(16, [128, 128], BF16, "m_b128")
        f512 = cx.sbpool(6, [128, 512], F32, "m_f512")
        sqp = cx.sbpool(2, [128, 512], F32, "m_sq")
        opl = cx.sbpool(3, [128, 512], BF16, "m_o")
        cx.n += 1
        pT = cx.st.enter_context(nc.psum_tensor(f"m_psT{cx.n}", [128, 1024], BF16))
        prot = Rot(banks[0:7])
        pTb = [Buf("pT0"), Buf("pT1")]
        Hcur = [None, None]

        def stage_a(ch):
            cs = slice(ch * 128, (ch + 1) * 128)
            pss = prot.get()
            cx.mm(pss.t[:, 0:2], TRI.t[:, :], LF.t[:, ch, :], True, True, [TRI.b, LF.b], [pss.b])
            cx.mm(pss.t[:, 2:4], c["ones_f"].t[:, :], LF.t[:, ch, :], True, True, [c["ones_f"].b, LF.b], [pss.b])
            a_ = sm.get()
            cx.tt("vector", a_.t[:, :], GT.t[:, ch, 0:2], pss.t[:, 0:2], ALU.subtract, [GT.b, pss.b], [a_.b])
            ab = sm.get()
            cx.tt("vector", ab.t[:, :], a_.t[:, :], pss.t[:, 2:4], ALU.add, [a_.b, pss.b], [ab.b])
            wk = sm.get()
            cx.act(wk.t[:, :], ab.t[:, :], AF.Exp, [ab.b], [wk.b])
            dec = sm.get()
            cx.act(dec.t[:, :], pss.t[:, 2:4], AF.Exp, [pss.b], [dec.b])
            out = {"dec": dec, "h": []}
            for h in range(2):
                qT, kT = QK[0][h], QK[1][h]
                lfb = f128.get()
                cx.ts("gpsimd", lfb.t[:, :], c["ones_f"].t[:, :], LF.t[:, ch, h:h + 1], ALU.mult, [c["ones_f"].b, LF.b], [lfb.b])
                pb = prot.get()
                cx.mm(pb.t[:, 0:128], lfb.t[:, :], TRI.t[:, :], True, True, [lfb.b, TRI.b], [pb.b])
                cx.mm(pb.t[:, 128:256], kT.t[:, cs], qT.t[:, cs], True, True, [kT.b, qT.b], [pb.b])
                DT = f128.get()
                cx.act(DT.t[:, :], pb.t[:, 0:128], AF.Exp, [pb.b, a_.b], [DT.b], bias=a_.t[:, h:h + 1])
                cx.tt("gpsimd", DT.t[:, :], DT.t[:, :], TRI.t[:, :], ALU.mult, [DT.b, TRI.b], [DT.b])
                EB = f128.get()
                cx.act(EB.t[:, :], pb.t[:, 0:128], AF.Exp, [pb.b], [EB.b])
                PT = b128.get()
                cx.tt("vector", PT.t[:, :], DT.t[:, :], pb.t[:, 128:256], ALU.mult, [DT.b, pb.b], [PT.b])
                qs = b128.get()
                cx.tt("gpsimd", qs.t[:, :], qT.t[:, cs], EB.t[:, :], ALU.mult, [qT.b, EB.b], [qs.b])
                tb = pTb[h]
                cx.transpose(pT[:, h * 128:(h + 1) * 128], kT.t[:, cs], c["id_b"].t[:, :], [kT.b, c["id_b"].b], [tb])
                Kw = b128.get()
                cx.act(Kw.t[:, :], pT[:, h * 128:(h + 1) * 128], AF.Copy, [tb, wk.b], [Kw.b], scale=wk.t[:, h:h + 1])
                out["h"].append((PT, qs, Kw))
            return out

        def stage_b(ch, sa):
            dec = sa["dec"]
            for h in range(2):
                PT, qs, Kw = sa["h"][h]
                if ch % 4 == 0:
                    Hcur[h] = H[h].get()
                pn = prot.get()
                cx.mm(pn.t[:, 0:128], CTb[h].t[:, 0:128], qs.t[:, :], True, False, [CTb[h].b, qs.b], [pn.b])
                cx.mm(pn.t[:, 0:128], V.t[:, ch, h * 128:(h + 1) * 128], PT.t[:, :], False, True, [V.b, PT.b], [pn.b])
                cx.mm(pn.t[:, 128:256], CTb[h].t[:, 128:256], qs.t[:, :], True, False, [CTb[h].b, qs.b], [pn.b])
                cx.mm(pn.t[:, 128:256], c["ones_b"].t[:, :], PT.t[:, :], False, True, [c["ones_b"].b, PT.b], [pn.b])
                pu = prot.get()
                cx.mm(pu.t[:, 0:128], Kw.t[:, :], V.t[:, ch, h * 128:(h + 1) * 128], True, True, [Kw.b, V.b], [pu.b])
                cx.mm(pu.t[:, 128:256], Kw.t[:, :], c["ones_b"].t[:, :], True, True, [Kw.b, c["ones_b"].b], [pu.b])
                cx.stt(CT[h].t[:, :], CT[h].t[:, :], dec.t[:, h:h + 1], pu.t[:, 0:256], ALU.mult, ALU.add,
                       [CT[h].b, dec.b, pu.b], [CT[h].b])
                cx.copy("gpsimd", CTb[h].t[:, :], CT[h].t[:, :], [CT[h].b], [CTb[h].b])
                r = f128.get()
                cx.ts("vector", r.t[:, :], pn.t[:, 128:256], 1.0, ALU.max, [pn.b], [r.b])
                cx.stt(r.t[:, :], pn.t[:, 128:256], -1.0, r.t[:, :], ALU.mult, ALU.max, [pn.b, r.b], [r.b])
                cx.fw.op("vector", lambda e, r=r: e.reciprocal(out=r.t[:, :], in_=r.t[:, :]), [r.b], [r.b])
                Hc = Hcur[h]
                cx.tt("vector", Hc.t[:, (ch % 4) * 128:(ch % 4 + 1) * 128], pn.t[:, 0:128], r.t[:, :], ALU.mult,
                      [pn.b, r.b], [Hc.b])
                if ch % 4 == 3:
                    t0 = (ch - 3) * 128
                    hf, tl = loc(t0)
                    sq = sqp.get()
                    cx.tt("gpsimd", sq.t[:, :], Hc.t[:, :], Hc.t[:, :], ALU.mult, [Hc.b], [sq.b])
                    ps = prot.get()
                    cx.mm(ps.t[:, :], c["ones_f"].t[:, :], sq.t[:, :], True, True, [c["ones_f"].b, sq.b], [ps.b])
                    ln = f512.get()
                    cx.act(ln.t[:, :], ps.t[:, :], AF.Ln, [ps.b, c["eps"].b], [ln.b], bias=c["eps"].t[:, 0:1], scale=1.0 / 128)
                    rstd = f512.get()
                    cx.act(rstd.t[:, :], ln.t[:, :], AF.Exp, [ln.b], [rstd.b], scale=-0.5)
                    hn = f512.get()
                    cx.stt(hn.t[:, :], Hc.t[:, :], mng.t[:, h:h + 1], rstd.t[:, :], ALU.mult, ALU.mult,
                           [Hc.b, mng.b, rstd.b], [hn.b])
                    og = opl.get()
                    cx.dma("sync", og.t[:, :], PFM[hf, 768 + h * 128:768 + (h + 1) * 128, tl:tl + 512], (), [og.b], dyn=dyn["pfm"])
                    eo = f512.get()
                    cx.act(eo.t[:, :], og.t[:, :], AF.Exp, [og.b], [eo.b], scale=-1.0)
                    cx.ts("gpsimd", eo.t[:, :], eo.t[:, :], 1.0, ALU.add, [eo.b], [eo.b])
                    cx.fw.op("vector", lambda e, eo=eo: e.reciprocal(out=eo.t[:, :], in_=eo.t[:, :]), [eo.b], [eo.b])
                    ob = opl.get()
                    cx.tt("vector", ob.t[:, :], hn.t[:, :], eo.t[:, :], ALU.mult, [hn.b, eo.b], [ob.b])
                    db_ = Buf("moutrow")
                    cx.dma("sync", MOUT[hf, 256 + h * 128:256 + (h + 1) * 128, tl:tl + 512], ob.t[:, :], [ob.b], [db_])
                    if xch:
                        xch.wrote(hf * 768 + 256 + h * 128, hf * 768 + 256 + (h + 1) * 128, 512, db_)

        pend = stage_a(0)
        for ch in range(NCH):
            nxt_a = stage_a(ch + 1) if ch + 1 < NCH else None
            stage_b(ch, pend)
            pend = nxt_a
        if xch:
            xch.flush()
        cx.fw.barrier()
        cx.st = old

    with contextlib.ExitStack() as st:
        old, cx.st = cx.st, st
        pre_fox = list(pre_fox or [])
        fb = cx.sb([4, 1], F32, "f_b")
        cx.dma("sync", fb.t[:], fox_b[:, :], (), [fb.b])
        cx.n += 1
        SCR = nc.dram_tensor(f"f_scr{cx.n}", [4, 6, S], BF16).ap()
        bscr = Buf("scr")
        SEG = min(2048, Tc)
        with contextlib.ExitStack() as st2:
            cx.st = st2
            onesr = cx.sb([4, SEG], F32, "f_ones")
            cx.memset("gpsimd", onesr.t[:], 1.0, [onesr.b])
            fcp = cx.sbpool(2, [4, SEG], F32, "f_F")
            f2p = cx.sbpool(2, [4, SEG], F32, "f_F2")
            p3p = cx.sbpool(2, [4, 6, SEG], BF16, "f_P3")
            prev = None
            for t0 in range(0, S, SEG):
                hf, tl = loc(t0)
                Fc, F2, P3 = fcp.get(), f2p.get(), p3p.get()
                cx.dma("sync", Fc.t[:, :], FFM[hf, :, tl:tl + SEG], (), [Fc.b], dyn=dyn["ffm"])
                cx.ts("vector", Fc.t[:, :], Fc.t[:, :], fb.t[:, 0:1], ALU.add, [Fc.b, fb.b], [Fc.b])
                cx.act(Fc.t[:, :], Fc.t[:, :], AF.Exp, [Fc.b], [Fc.b], scale=-1.0)
                cx.act(Fc.t[:, :], Fc.t[:, :], AF.Ln, [Fc.b, c["ones_f"].b], [Fc.b], bias=c["ones_f"].t[0:4, 0:1])
                cx.ts("vector", Fc.t[:, :], Fc.t[:, :], -1.0, ALU.mult, [Fc.b], [Fc.b])
                if prev is None:
                    cx.fw.op("vector", lambda e, F2=F2, Fc=Fc: e.tensor_tensor_scan(
                        out=F2.t[:, :], data0=onesr.t[:, :], data1=Fc.t[:, :], initial=0.0, op0=ALU.mult, op1=ALU.add),
                        [onesr.b, Fc.b], [F2.b])
                else:
                    cx.fw.op("vector", lambda e, F2=F2, Fc=Fc, prev=prev: e.tensor_tensor_scan(
                        out=F2.t[:, :], data0=onesr.t[:, :], data1=Fc.t[:, :], initial=prev.t[:, SEG - 1:SEG],
                        op0=ALU.mult, op1=ALU.add), [onesr.b, Fc.b, prev.b], [F2.b])
                prev = F2
                cx.ts("vector", P3.t[:, 0, :], F2.t[:, :], 8.0, ALU.mult, [F2.b], [P3.b])
                cx.stt(Fc.t[:, :], F2.t[:, :], 8.0, P3.t[:, 0, :], ALU.mult, ALU.subtract, [F2.b, P3.b], [Fc.b])
                cx.copy("vector", P3.t[:, 1, :], Fc.t[:, :], [Fc.b], [P3.b])
                cx.tt("vector", Fc.t[:, :], Fc.t[:, :], P3.t[:, 1, :], ALU.subtract, [Fc.b, P3.b], [Fc.b])
                cx.copy("vector", P3.t[:, 2, :], Fc.t[:, :], [Fc.b], [P3.b])
                cx.ts("vector", P3.t[:, 3:6, :], P3.t[:, 0:3, :], -1.0, ALU.mult, [P3.b], [P3.b])
                cx.dma("sync", SCR[:, :, t0:t0 + SEG], P3.t[:, :, :], [P3.b], [bscr])
            cx.fw.barrier()
            cx.st = st
        MASK = []
        for r in range(4):
            m = cx.sb([128, 512], BF16, f"f_mask{r}")
            cx.memset("gpsimd", m.t[:], 0.0, [m.b])
            cx.fw.op("gpsimd", lambda e, m=m, r=r: e.affine_select(out=m.t[:], in_=m.t[:], compare_op=ALU.is_ge, fill=-30000.0,
                                                                    base=-128 * r, pattern=[[1, 512]], channel_multiplier=-1),
                     [m.b], [m.b])
            MASK.append(m)
        QA = cx.sbpool(2, [128, S], BF16, "f_qa")
        KA = cx.sbpool(2, [128, S], BF16, "f_ka")
        VA = cx.sbpool(2, [128, NCH, 128], BF16, "f_va")
        ptp = cx.sbpool(6, [128, 512], BF16, "f_pt")
        rp = cx.sbpool(2, [128, 512], F32, "f_r")
        r0p = cx.sbpool(2, [128, 512], F32, "f_r0")
        ost = cx.sbpool(3, [64, 512], BF16, "f_ost")
        srot = Rot(banks[0:5])
        orot = Rot(banks[5:8])
        def load_head(h):
            qa, ka, va = QA.get(), KA.get(), VA.get()
            cx.memset("gpsimd", qa.t[64:70, :], 1.0, [qa.b])
            cx.memset("gpsimd", ka.t[64:70, :], 1.0, [ka.b])
            cx.memset("gpsimd", va.t[:, :, :], 1.0, [va.b])
            for hf in range(2):
                cx.dma("sync", qa.t[0:64, hf * Tc:(hf + 1) * Tc], PFM[hf, 1024 + h * 64:1024 + (h + 1) * 64, :], (), [qa.b], dyn=dyn["pfm"])
                cx.dma("sync", ka.t[0:64, hf * Tc:(hf + 1) * Tc], PFM[hf, 1280 + h * 64:1280 + (h + 1) * 64, :], (), [ka.b], dyn=dyn["pfm"])
                cx.dma("sync", va.t[:, hf * (Tc // 128):(hf + 1) * (Tc // 128), 0:64],
                       VTOK[hf].rearrange("(n p) c -> p n c", p=128)[:, :, 256 + h * 64:256 + (h + 1) * 64], (), [va.b], dyn=dyn["vtok"])
            cx.dma("sync", qa.t[64:67, :], SCR[h, 0:3, :], [bscr], [qa.b])
            cx.dma("sync", ka.t[67:70, :], SCR[h, 3:6, :], [bscr], [ka.b])
            return qa, ka, va

        nxt = load_head(0)
        for h in range(4):
            qa, ka, va = nxt
            if h + 1 < 4:
                nxt = load_head(h + 1)
            for qb in range(S // 512):
                qs_ = slice(qb * 512, (qb + 1) * 512)
                nk = 4 * qb + 4
                if pre_fox:
                    pre_fox.pop(0)()
                po = orot.get()
                pend = []

                def issue_s(kt):
                    ps = srot.get()
                    diag = kt >= 4 * qb
                    cx.mm(ps.t[:, :], ka.t[0:70, kt * 128:(kt + 1) * 128], qa.t[0:70, qs_], True, not diag, [ka.b, qa.b], [ps.b])
                    if diag:
                        mk = MASK[kt - 4 * qb]
                        cx.mm(ps.t[:, :], c["id_b"].t[:, :], mk.t[:, :], False, True, [c["id_b"].b, mk.b], [ps.b])
                    return ps
                SKEW = 2
                for kt in range(min(SKEW, nk)):
                    pend.append(issue_s(kt))
                for kt in range(nk):
                    if kt + SKEW < nk:
                        pend.append(issue_s(kt + SKEW))
                    ps = pend.pop(0)
                    pt = ptp.get()
                    cx.act(pt.t[:, :], ps.t[:, :], AF.Exp, [ps.b], [pt.b], scale=0.125)
                    cx.mm(po.t[:, :], va.t[:, kt, :], pt.t[:, :], kt == 0, kt == nk - 1, [va.b, pt.b], [po.b])
                rr = rp.get()
                cx.fw.op("vector", lambda e, rr=rr, po=po: e.reciprocal(out=rr.t[64:128, :], in_=po.t[64:128, :]), [po.b], [rr.b])
                r0 = r0p.get()
                cx.copy("vector", r0.t[0:64, :], rr.t[64:128, :], [rr.b], [r0.b])
                os_ = ost.get()
                cx.tt("vector", os_.t[0:64, :], po.t[0:64, :], r0.t[0:64, :], ALU.mult, [po.b, r0.b], [os_.b])
                hf, tl = loc(qb * 512)
                db_ = Buf("moutrow")
                cx.dma("sync", MOUT[hf, 512 + h * 64:512 + (h + 1) * 64, tl:tl + 512], os_.t[0:64, :], [os_.b], [db_])
                if xch:
                    xch.wrote(hf * 768 + 512 + h * 64, hf * 768 + 512 + (h + 1) * 64, 512, db_)
        while pre_fox:
            pre_fox.pop(0)()
        cx.fw.barrier()
        cx.st = old
    cx.fw.wait_all("sync")


def build_phase_b(Tc):
    nc = bass.Bass("TRN2", target_bir_lowering=False)
    PFM = dram(nc, "pfm", [2, 1536, Tc], BF16, "ExternalInput")
    VTOK = dram(nc, "vtok", [2, Tc, 512], BF16, "ExternalInput")
    GTOK = dram(nc, "gtok", [2, 128, Tc // 128, 4], F32, "ExternalInput")
    FFM = dram(nc, "ffm", [2, 4, Tc], F32, "ExternalInput")
    pool_w = dram(nc, "pool_w", [128, 2, 128], F32, "ExternalInput")
    pool_scale = dram(nc, "pool_scale", [128, 2], F32, "ExternalInput")
    pool_coef = dram(nc, "pool_coef", [128, 2, 4], F32, "ExternalInput")
    pool_rc = dram(nc, "pool_rc", [128, 2, 4, 16], F32, "ExternalInput")
    conv_w = dram(nc, "conv_w", [128, 2, 2, 4], F32, "ExternalInput")
    ml_b = dram(nc, "ml_b", [128, 4], F32, "ExternalInput")
    ml_ng = dram(nc, "ml_ng", [128, 2], F32, "ExternalInput")
    fox_b = dram(nc, "fox_b", [4, 1], F32, "ExternalInput")
    MOUT = dram(nc, "mout", [2, 768, Tc], BF16, "ExternalOutput")
    cx = Cx(nc)
    c = make_consts(cx)
    add_eps(cx, c)
    psum = cx.psum_banks(7)
    emit_phase_b(cx, c, Tc, PFM, VTOK, GTOK, FFM, pool_w, pool_scale, pool_coef, pool_rc, conv_w, ml_b, ml_ng, fox_b,
                 MOUT, psum)
    cx.finish()
    return nc


POOL_WINDOWS = (2, 4, 8, 16)


def lay128(v):
    v = np.asarray(v, np.float32)
    return np.ascontiguousarray(v.reshape(-1, 128).T)


def small_b_inputs(g, pool_w_grp, pool_scale, ml_conv_w, ml_b_i, ml_b_f, ml_norm_g, fox_b_f):
    d = {}
    d["pool_w"] = np.ascontiguousarray(np.stack([pool_w_grp[2 * g + gi] for gi in range(2)], axis=1)).astype(np.float32)
    d["pool_scale"] = lay128(pool_scale[g * 256:(g + 1) * 256])
    coef = np.zeros((128, 2, 4), np.float32)
    rc = np.zeros((128, 2, 4, 16), np.float32)
    for gi in range(2):
        w = POOL_WINDOWS[2 * g + gi]
        k = POOL_WINDOWS.index(w)
        coef[:, gi, k] = 1.0 / w
        rc[:, gi, k, :] = 1.0 / np.minimum(np.arange(16) + 1, w)
    d["pool_coef"] = coef
    d["pool_rc"] = rc
    cw = np.zeros((128, 2, 2, 4), np.float32)
    for a in range(2):
        for h in range(2):
            c0 = a * 512 + (2 * g + h) * 128
            cw[:, a, h, :] = ml_conv_w[:, c0:c0 + 128].T
    d["conv_w"] = cw
    mb = np.array([ml_b_i[2 * g], ml_b_i[2 * g + 1], ml_b_f[2 * g], ml_b_f[2 * g + 1]], np.float32)
    d["ml_b"] = np.ascontiguousarray(np.broadcast_to(mb, (128, 4)))
    d["ml_ng"] = lay128(ml_norm_g[g * 256:(g + 1) * 256])
    d["fox_b"] = np.asarray(fox_b_f[4 * g:4 * g + 4], np.float32).reshape(4, 1).copy()
    return d


GROUPS = [[0, 1], [2, 3], [4, 5], [6, 7]]
B_SMALL = (("pool_w", [128, 2, 128]), ("pool_scale", [128, 2]), ("pool_coef", [128, 2, 4]), ("pool_rc", [128, 2, 4, 16]),
           ("conv_w", [128, 2, 2, 4]), ("ml_b", [128, 4]), ("ml_ng", [128, 2]), ("fox_b", [4, 1]))


CC_MAX_BYTES = 2 * 1024 * 1024


class Xch:
    def __init__(self, cx, name, src2d, elem_bytes):
        self.cx = cx
        self.src = src2d
        rows, cols = src2d.shape
        self.rows, self.cols = rows, cols
        half = rows // 2
        R = 1
        for r_ in range(1, half + 1):
            if half % r_ == 0 and r_ * cols * elem_bytes <= CC_MAX_BYTES:
                R = r_
        self.R = R
        self.nch = rows // R
        self.GA = cx.nc.dram_tensor(name, [self.nch, 2, R, cols], src2d.dtype).ap()
        self.area = [0] * self.nch
        self.bufs = [[] for _ in range(self.nch)]
        self.issued = [False] * self.nch

    def wrote(self, row0, row1, ncols, buf):
        R = self.R
        for k in range(row0 // R, (row1 - 1) // R + 1):
            lo, hi = max(row0, k * R), min(row1, (k + 1) * R)
            self.area[k] += (hi - lo) * ncols
            self.bufs[k].append(buf)

    def flush(self):
        R = self.R
        for k in range(self.nch):
            if not self.issued[k] and self.area[k] >= R * self.cols:
                assert self.area[k] == R * self.cols, (k, self.area[k])
                self.issued[k] = True
                src, GA = self.src, self.GA
                self.cx.fw.cc_op(lambda e, k=k: e.collective_compute(
                    "AllGather", ALU.bypass, replica_groups=GROUPS, ins=[src[k * R:(k + 1) * R, :]],
                    outs=[GA[k].rearrange("r i c -> (r i) c")]), self.bufs[k])

    def select(self, dst3):
        assert all(self.issued), self.issued
        hk = self.nch // 2
        mult = hk * 2 * self.R * self.cols
        for r_ in range(2):
            self.cx.dma("sync", dst3[r_].rearrange("(kk i) c -> kk i c", i=self.R), self.GA[0:hk, r_], (), (), dyn=mult)


def build_fused(Tc, depth=2):
    nc = bass.Bass("TRN2", target_bir_lowering=False)
    I = lambda name, shape, dt=F32: dram(nc, name, shape, dt, "ExternalInput")
    xT = I("xT", [D, Tc])
    mix_g = I("mix_g", [depth, 128, 8])
    w_in = I("w_in", [depth, D, IN_W])
    bsm = [{k: I(f"{k}{l}", shp) for k, shp in B_SMALL} for l in range(depth)]
    w_br = I("w_br", [depth, 3, 512, D])
    w_out = I("w_out", [depth, D, D])
    ffn_g = I("ffn_g", [depth, 128, 8])
    ff_g = I("ff_w_gate", [(depth + 1) // 2, D, D_FF])
    ff_u = I("ff_w_up", [(depth + 1) // 2, D, D_FF])
    ff_d = I("ff_w_down", [(depth + 1) // 2, D_FF, D])
    m_r = I("moe_w_router", [depth // 2, D, NEXP])
    m_b = I("moe_b_router", [depth // 2, NEXP, 1])
    m_g = I("moe_w_gate", [depth // 2, NEXP, D, D_FFE])
    m_u = I("moe_w_up", [depth // 2, NEXP, D, D_FFE])
    m_d = I("moe_w_down", [depth // 2, NEXP, D_FFE, D])
    final_g = I("final_g", [128, 8])
    outT = dram(nc, "outT", [D, Tc], F32, "ExternalOutput")
    N = lambda name, shape, dt: nc.dram_tensor(name, list(shape), dt).ap()
    PFM = N("s_pfm", [2, 1536, Tc], BF16)
    GATES = N("s_gates", [3072, Tc], BF16)
    VTOK = N("s_vtok", [2, Tc, 512], BF16)
    GTOK = N("s_gtok", [2, 128, Tc // 128, 4], F32)
    FFM = N("s_ffm", [2, 4, Tc], F32)
    MOUT = N("s_mout", [2, 768, Tc], BF16)
    XN = N("s_xn", [D, Tc], F32)
    PFMS = N("s_pfms", [2, 1536, Tc], BF16)
    VTOKS = N("s_vtoks", [2, Tc, 512], BF16)
    GTOKS = N("s_gtoks", [2, 128, Tc // 128, 4], F32)
    FFMS = N("s_ffms", [2, 4, Tc], F32)
    MINS = N("s_mins", [2, 768, Tc], BF16)
    cx = Cx(nc)
    c = make_consts(cx)
    add_eps(cx, c)
    psum = cx.psum_banks(7)
    xcur = xT
    NM = depth // 2
    W16 = [(N(f"w16g{i}", [NEXP, D, D_FFE], BF16), N(f"w16u{i}", [NEXP, D, D_FFE], BF16), N(f"w16d{i}", [NEXP, D_FFE, D], BF16))
           for i in range(NM)]
    b16 = Buf("w16")

    pc_jobs = []
    for i in range(NM):
        pc_jobs += precast_jobs(cx, [(W16[i][0], m_g[i]), (W16[i][1], m_u[i]), (W16[i][2], m_d[i])], b16)
    for l in range(depth):
        xa = {"pfm": Xch(cx, f"ga_pfm{l}", PFM.rearrange("a b c -> (a b) c"), 2),
              "vtok": Xch(cx, f"ga_vtok{l}", VTOK.rearrange("a b c -> (a b) c"), 2),
              "gtok": Xch(cx, f"ga_gtok{l}", GTOK.rearrange("a p n c -> (a p) (n c)"), 4),
              "ffm": Xch(cx, f"ga_ffm{l}", FFM.rearrange("a h t -> (a h) t"), 4)}
        emit_phase_a(cx, c, Tc, xcur, mix_g[l], w_in[l], PFM, GATES, VTOK, GTOK, FFM, psum, xch=xa)
        for x_ in xa.values():
            x_.flush()
        cx.fw.cc_wait()
        xa["pfm"].select(PFMS)
        xa["vtok"].select(VTOKS)
        xa["gtok"].select(GTOKS.rearrange("r p n c -> r p (n c)"))
        xa["ffm"].select(FFMS)
        cx.fw.barrier()
        b = bsm[l]
        xm = Xch(cx, f"ga_mout{l}", MOUT.rearrange("a b c -> (a b) c"), 2)
        emit_phase_b(cx, c, Tc, PFMS, VTOKS, GTOKS, FFMS, b["pool_w"], b["pool_scale"], b["pool_coef"],
                     b["pool_rc"], b["conv_w"], b["ml_b"], b["ml_ng"], b["fox_b"], MOUT, psum, xch=xm,
                     pre_fox=pc_jobs if l == 0 else None)
        xm.flush()
        cx.fw.cc_wait()
        xm.select(MINS)
        cx.fw.barrier()
        moe = (l % 2 == 1)
        final = (l == depth - 1)
        if moe:
            ffw = (m_r[l // 2], m_b[l // 2]) + W16[l // 2]
        else:
            ffw = (ff_g[l // 2], ff_u[l // 2], ff_d[l // 2])
        dst = outT if final else XN
        emit_phase_c(cx, c, Tc, xcur, MINS, GATES, w_br[l], w_out[l], ffn_g[l], ffw, dst, psum, moe=moe,
                     final_g=final_g if final else None, wq="sync" if moe else "gpsimd", wbuf16=b16 if moe else None)
        xcur = XN
    cx.finish()
    return nc


_PROGS = {}


def kernel(x, mix_norm_g, w_in, pool_w_grp, pool_scale, ml_conv_w, ml_b_i, ml_b_f, ml_norm_g, fox_b_f,
           w_br_pool, w_br_ml, w_br_fox, w_out, ffn_norm_g, ff_w_gate, ff_w_up, ff_w_down,
           moe_w_router, moe_b_router, moe_w_gate, moe_w_up, moe_w_down, final_norm_g):
    f32 = lambda a: np.ascontiguousarray(np.asarray(a, dtype=np.float32))
    x = f32(x)
    Tc = SEQ // 2
    depth = int(np.asarray(w_in).shape[0])
    if "fused" not in _PROGS:
        _PROGS["fused"] = build_fused(Tc, depth)
    nc = _PROGS["fused"]
    shared = {
        "mix_g": np.stack([lay128(mix_norm_g[l]) for l in range(depth)]),
        "w_in": f32(w_in),
        "w_br": np.ascontiguousarray(np.stack([np.stack([f32(w_br_pool[l]), f32(w_br_ml[l]), f32(w_br_fox[l])]) for l in range(depth)])),
        "w_out": f32(w_out),
        "ffn_g": np.stack([lay128(ffn_norm_g[l]) for l in range(depth)]),
        "ff_w_gate": f32(ff_w_gate), "ff_w_up": f32(ff_w_up), "ff_w_down": f32(ff_w_down),
        "moe_w_router": f32(moe_w_router), "moe_b_router": f32(moe_b_router).reshape(-1, NEXP, 1),
        "moe_w_gate": f32(moe_w_gate), "moe_w_up": f32(moe_w_up), "moe_w_down": f32(moe_w_down),
        "final_g": lay128(final_norm_g),
    }
    smalls = [[small_b_inputs(g, f32(pool_w_grp[l]), f32(pool_scale[l]), f32(ml_conv_w[l]), f32(ml_b_i[l]), f32(ml_b_f[l]),
                              f32(ml_norm_g[l]), f32(fox_b_f[l])) for g in range(2)] for l in range(depth)]
    in_maps = []
    for c in range(NCORES):
        d = dict(shared)
        d["xT"] = np.ascontiguousarray(x[c // 2, (c % 2) * Tc:(c % 2 + 1) * Tc, :].T)
        for l in range(depth):
            for k, v in smalls[l][c % 2].items():
                d[f"{k}{l}"] = v
        in_maps.append(d)
    res = run_bass_kernel_spmd(nc, in_maps, core_ids=list(range(NCORES)))
    out = np.empty((BATCH, SEQ, D), np.float32)
    for c in range(NCORES):
        out[c // 2, (c % 2) * Tc:(c % 2 + 1) * Tc, :] = np.asarray(res.results[c]["outT"]).T
    return out
```

```python
import contextlib
import os
import numpy as np
import concourse.bass as bass
import concourse.mybir as mybir
from concourse.bass_utils import run_bass_kernel_spmd

F32 = mybir.dt.float32
BF16 = mybir.dt.bfloat16
AF = mybir.ActivationFunctionType
ALU = mybir.AluOpType
AX = mybir.AxisListType

D = 1024
SEQ = 8192
BATCH = 4
NCORES = 8
IN_W = 7184
D_FF = 2816
D_FFE = 3584
NEXP = 8
EPS = 1e-6


ALL_BUFS = []


class Buf:
    __slots__ = ("name", "w", "r")

    def __init__(self, name=""):
        self.name = name
        self.w = None
        self.r = {}
        ALL_BUFS.append(self)


class Lane:
    def __init__(self, name, sem, step):
        self.name = name
        self.sem = sem
        self.step = step
        self.count = 0
        self.seen = {}
        self.snaps = {}


SYNC_SAME = {"tensor": False, "vector": True, "scalar": True, "gpsimd": True, "sync": False}


class FW:
    def __init__(self, nc, n_dma_lanes=24):
        self.nc = nc
        self.ops = {k: [] for k in ("tensor", "vector", "scalar", "gpsimd", "sync")}
        self.eng = {}
        for k in self.ops:
            self.eng[k] = Lane(k, nc.alloc_semaphore(name=f"prog_{k}"), 1)
        self.dma_lanes = [Lane(f"dma{i}", nc.alloc_semaphore(name=f"dma_{i}"), 16)
                          for i in range(n_dma_lanes)]
        self.dma_rr = 0
        self.cc = Lane("cc", nc.alloc_semaphore(name="cc_sem"), 1)

    def cc_op(self, fn, reads=()):
        E = self.eng["gpsimd"]
        self._emit_waits(E, self._needs(reads, ()))
        self.cc.count += 1
        self.ops["gpsimd"].append(("cc", fn, self.cc.sem))

    def cc_wait(self, only=None):
        for E in self.eng.values():
            if only is None or E.name == only:
                self._emit_waits(E, {self.cc: self.cc.count})

    def _needs(self, reads, writes):
        needs = {}
        for b in reads:
            if b.w is not None:
                l, i = b.w
                if needs.get(l, 0) < i:
                    needs[l] = i
        for b in writes:
            if b.w is not None:
                l, i = b.w
                if needs.get(l, 0) < i:
                    needs[l] = i
            for l, i in b.r.items():
                if needs.get(l, 0) < i:
                    needs[l] = i
        return needs

    def _emit_waits(self, E, needs):
        for l, i in needs.items():
            if l is E and not SYNC_SAME[E.name]:
                continue
            if E.seen.get(l, 0) >= i:
                continue
            self.ops[E.name].append(("wait", l.sem, i * l.step))
            E.seen[l] = i
            snap = l.snaps.get(i)
            if snap:
                for l2, i2 in snap.items():
                    if E.seen.get(l2, 0) < i2:
                        E.seen[l2] = i2

    def op(self, engine, fn, reads=(), writes=()):
        E = self.eng[engine]
        self._emit_waits(E, self._needs(reads, writes))
        E.count += 1
        idx = E.count
        E.snaps[idx] = dict(E.seen)
        self.ops[engine].append(("op", fn, E.sem))
        for b in reads:
            b.r[E] = idx
        for b in writes:
            b.w = (E, idx)
            b.r = {}

    def dma(self, queue, out, in_, reads=(), writes=(), dyn=0, **kw):
        E = self.eng[queue]
        L = self.dma_lanes[self.dma_rr]
        self.dma_rr = (self.dma_rr + 1) % len(self.dma_lanes)
        needs = self._needs(reads, writes)
        if L.count > 0:
            needs[L] = max(needs.get(L, 0), L.count)
        self._emit_waits(E, needs)
        L.count += 1
        idx = L.count
        L.snaps[idx] = dict(E.seen)
        if dyn:
            self.ops[queue].append(("dyndma", out, in_, dyn, L.sem))
        else:
            self.ops[queue].append(("dma", out, in_, kw, L.sem))
        for b in reads:
            b.r[L] = idx
        for b in writes:
            b.w = (L, idx)
            b.r = {}

    def _lanes(self):
        return list(self.eng.values()) + self.dma_lanes + [self.cc]

    def begin_if(self, flag_ap):
        self.barrier()
        self._snap = ({l: l.count for l in self._lanes()}, {l: dict(l.seen) for l in self._lanes()},
                      [(b, b.w, dict(b.r)) for b in ALL_BUFS], self.dma_rr)
        for name in self.ops:
            self.ops[name].append(("if", flag_ap))

    def _restore(self):
        counts, seens, bufs, rr = self._snap
        for l, c_ in counts.items():
            l.count = c_
            l.seen = dict(seens[l])
        for b, w, r in bufs:
            b.w = w
            b.r = dict(r)
        self.dma_rr = rr

    def begin_else(self):
        self._endA = {l: l.count for l in self._lanes()}
        self._padA = {name: [] for name in self.ops}
        for name in self.ops:
            self.ops[name].append(("else", self._padA[name]))
        self._restore()

    def end_if(self):
        endB = {l: l.count for l in self._lanes()}
        padB = {name: [] for name in self.ops}
        for l in self._lanes():
            fin = max(self._endA[l], endB[l])
            owner = l.name if l.name in self.ops else "sync"
            for end, pads in ((self._endA[l], self._padA), (endB[l], padB)):
                if fin > end:
                    pads[owner].append((l.sem, end * l.step, (fin - end) * l.step))
            l.count = fin
        for name in self.ops:
            self.ops[name].append(("endif", padB[name]))
        counts, seens, bufs, rr = self._snap
        for l in self._lanes():
            l.seen = dict(seens[l])
        self.barrier()

    def barrier(self):
        lanes = list(self.eng.values()) + self.dma_lanes
        counts = {l: l.count for l in lanes if l.count > 0}
        for E in self.eng.values():
            self._emit_waits(E, {l: i for l, i in counts.items() if l is not E})

    def wait_all(self, engine):
        E = self.eng[engine]
        needs = {}
        for l in list(self.eng.values()) + self.dma_lanes:
            if l.count > 0 and l is not E:
                needs[l] = l.count
        self._emit_waits(E, needs)

    def emit(self):
        with self.nc.Block() as block:
            for name in self.ops:
                ops = self.ops[name]
                if not ops:
                    continue

                def body(eng, ops=ops):
                    me = None
                    stack = []

                    def pad(pads):
                        for sem, cur, amt in pads:
                            eng.wait_ge(sem, cur)
                            eng.sem_inc(sem, amt)
                    for o in ops:
                        if o[0] == "if":
                            reg = eng.alloc_register()
                            eng.reg_load(reg, o[1])
                            cm = eng.If_eq(reg, 1)
                            cm.__enter__()
                            stack.append(cm)
                        elif o[0] == "else":
                            pad(o[1])
                            stack.pop().__exit__(None, None, None)
                            cm = eng.Else()
                            cm.__enter__()
                            stack.append(cm)
                        elif o[0] == "endif":
                            pad(o[1])
                            stack.pop().__exit__(None, None, None)
                        elif o[0] == "wait":
                            eng.wait_ge(o[1], o[2])
                        elif o[0] == "op":
                            o[1](eng).then_inc(o[2], 1)
                        elif o[0] == "cc":
                            o[1](eng).then_inc(o[2], 1)
                        elif o[0] == "dyndma":
                            _, out, in_, mult, sem = o
                            if me is None:
                                me = eng.partition_id() % 2
                            off = me * mult
                            eng.dma_start(out=out, in_=bass.AP(in_.tensor, in_.offset + off, in_.ap)).then_inc(sem, 16)
                        else:
                            _, out, in_, kw, sem = o
                            eng.dma_start(out=out, in_=in_, **kw).then_inc(sem, 16)

                getattr(block, name)(body)


class T:
    __slots__ = ("t", "b")

    def __init__(self, t, b):
        self.t = t
        self.b = b


class Rot:
    def __init__(self, items):
        self.items = items
        self.i = 0

    def get(self):
        x = self.items[self.i]
        self.i = (self.i + 1) % len(self.items)
        return x


class Cx:
    def __init__(self, nc):
        self.nc = nc
        self.fw = FW(nc)
        self.st = contextlib.ExitStack()
        self.n = 0

    def sb(self, shape, dtype, name=None):
        self.n += 1
        nm = f"{name or 'sb'}_{self.n}"
        t = self.st.enter_context(self.nc.sbuf_tensor(nm, list(shape), dtype))
        return T(t, Buf(nm))

    def sbpool(self, n, shape, dtype, name=None):
        return Rot([self.sb(shape, dtype, (name or "p") + f"_{self.n}_{i}") for i in range(n)])

    def psum_banks(self, n=8, dtype=F32):
        out = []
        for i in range(n):
            self.n += 1
            cols = 512 if dtype == F32 else 1024
            t = self.st.enter_context(self.nc.psum_tensor(f"ps{self.n}", [128, cols], dtype))
            out.append(T(t, Buf(f"ps{self.n}")))
        return Rot(out)

    def mm(self, out, lhsT, rhs, start, stop, reads, writes):
        self.fw.op("tensor", lambda e: e.matmul(out, lhsT=lhsT, rhs=rhs, start=start, stop=stop),
                   reads, writes)

    def transpose(self, out, in_, ident, reads, writes):
        self.fw.op("tensor", lambda e: e.transpose(out, in_, ident), reads, writes)

    def act(self, out, in_, func, reads, writes, bias=None, scale=None):
        kw = {}
        if bias is not None:
            kw["bias"] = bias
        if scale is not None:
            kw["scale"] = scale
        self.fw.op("scalar", lambda e: e.activation(out=out, in_=in_, func=func, **kw), reads, writes)

    def tt(self, eng, out, in0, in1, op, reads, writes):
        self.fw.op(eng, lambda e: e.tensor_tensor(out=out, in0=in0, in1=in1, op=op), reads, writes)

    def ts(self, eng, out, in0, s1, op0, reads, writes, s2=None, op1=None):
        if op1 is None:
            self.fw.op(eng, lambda e: e.tensor_scalar(out=out, in0=in0, scalar1=s1, scalar2=None, op0=op0),
                       reads, writes)
        else:
            self.fw.op(eng, lambda e: e.tensor_scalar(out=out, in0=in0, scalar1=s1, scalar2=s2, op0=op0, op1=op1),
                       reads, writes)

    def stt(self, out, in0, scalar, in1, op0, op1, reads, writes):
        self.fw.op("vector", lambda e: e.scalar_tensor_tensor(out=out, in0=in0, scalar=scalar, in1=in1,
                                                              op0=op0, op1=op1), reads, writes)

    def copy(self, eng, out, in_, reads, writes):
        if eng == "scalar":
            self.fw.op("scalar", lambda e: e.activation(out=out, in_=in_, func=AF.Copy), reads, writes)
        else:
            self.fw.op(eng, lambda e: e.tensor_copy(out=out, in_=in_), reads, writes)

    def memset(self, eng, ap, val, writes):
        self.fw.op(eng, lambda e: e.memset(ap, val), (), writes)

    def dma(self, q, out, in_, reads=(), writes=(), dyn=0, **kw):
        self.fw.dma(q, out, in_, reads, writes, dyn=dyn, **kw)

    def finish(self):
        self.fw.wait_all("sync")
        self.fw.emit()
        self.st.close()


def dram(nc, name, shape, dtype, kind):
    return nc.dram_tensor(name, list(shape), dtype, kind=kind).ap()


def make_consts(cx):
    c = {}
    c["ones_f"] = cx.sb([128, 128], F32, "ones_f")
    c["ones_b"] = cx.sb([128, 128], BF16, "ones_b")
    c["id_f"] = cx.sb([128, 128], F32, "id_f")
    c["id_b"] = cx.sb([128, 128], BF16, "id_b")
    cx.memset("gpsimd", c["ones_f"].t[:], 1.0, [c["ones_f"].b])
    cx.memset("gpsimd", c["ones_b"].t[:], 1.0, [c["ones_b"].b])
    cx.memset("gpsimd", c["id_f"].t[:], 0.0, [c["id_f"].b])
    idf = c["id_f"]
    cx.fw.op("gpsimd", lambda e: e.affine_select(out=idf.t[:], in_=idf.t[:], compare_op=ALU.not_equal,
                                                 fill=1.0, base=0, pattern=[[-1, 128]], channel_multiplier=1),
             [idf.b], [idf.b])
    cx.copy("gpsimd", c["id_b"].t[:], idf.t[:], [idf.b], [c["id_b"].b])
    return c


def rmsnorm_stats(cx, c, xt, xb, ncols, psum, sqpool, tmp_pool, nfeat_chunks=8, nfeat=1024.0):
    ps = psum.get()
    for kc in range(nfeat_chunks):
        sq = sqpool.get()
        cx.tt("gpsimd", sq.t[:, :ncols], xt(kc), xt(kc), ALU.mult, [xb], [sq.b])
        cx.mm(ps.t[:, :ncols], c["ones_f"].t[:, :], sq.t[:, :ncols], kc == 0, kc == nfeat_chunks - 1,
              [sq.b, c["ones_f"].b], [ps.b])
    ln = tmp_pool.get()
    cx.act(ln.t[:, :ncols], ps.t[:, :ncols], AF.Ln, [ps.b], [ln.b], bias=c["eps"].t[:, 0:1], scale=1.0 / nfeat)
    rstd = tmp_pool.get()
    cx.act(rstd.t[:, :ncols], ln.t[:, :ncols], AF.Exp, [ln.b], [rstd.b], scale=-0.5)
    return rstd


def add_eps(cx, c):
    c["eps"] = cx.sb([128, 1], F32, "eps")
    cx.memset("gpsimd", c["eps"].t[:], EPS, [c["eps"].b])


FM_BASES = [0, 512, 1024, 2048, 2568, 3080]


def emit_phase_a(cx, c, Tc, xT, g_in, w_in, PFM, GATES, VTOK, GTOK, FFM, psum, xch=None, mid_gates=None):
    NB = Tc // 512
    xTv = xT.rearrange("(kc p) t -> p kc t", p=128)
    wv = w_in.rearrange("(kc p) n -> p kc n", p=128)
    with contextlib.ExitStack() as st:
        old, cx.st = cx.st, st
        gt = cx.sb([128, 8], F32, "a_g")
        cx.dma("sync", gt.t[:], g_in[:, :], (), [gt.b])
        hT = cx.sb([128, 8, Tc], BF16, "a_hT")
        bh = [Buf(f"hT{b}") for b in range(NB)]
        xpool = cx.sbpool(2, [128, 8, 512], F32, "a_x")
        sqpool = cx.sbpool(3, [128, 512], F32, "a_sq")
        tmp = cx.sbpool(4, [128, 512], F32, "a_tmp")
        wpool = cx.sbpool(3, [128, 8, 512], BF16, "a_w")
        stpool = cx.sbpool(3, [128, Tc], BF16, "a_st")
        for blk in range(NB):
            x = xpool.get()
            cx.dma("sync", x.t[:], xTv[:, :, blk * 512:(blk + 1) * 512], (), [x.b])
            rstd = rmsnorm_stats(cx, c, lambda kc: x.t[:, kc, :], x.b, 512, psum, sqpool, tmp)
            for kc in range(8):
                cx.stt(hT.t[:, kc, blk * 512:(blk + 1) * 512], x.t[:, kc, :], gt.t[:, kc:kc + 1], rstd.t[:, :],
                       ALU.mult, ALU.mult, [x.b, gt.b, rstd.b], [bh[blk]])
        jobs = []
        for g in range(2):
            for j, base in enumerate(FM_BASES):
                jobs.append((base + g * 256, 256, "pfm", (g, j * 256)))
        gate_jobs = [(4112 + n * 512, 512, "gate", n * 512) for n in range(6)]
        ev = 0

        def flush_all():
            if xch:
                for x_ in xch.values():
                    x_.flush()

        def run_jobs(jobs):
          nonlocal ev
          for (c0, ncols, kind, dest) in jobs:
            w = wpool.get()
            cx.dma("gpsimd", w.t[:, :, :ncols], wv[:, :, c0:c0 + ncols], (), [w.b])
            flush_all()
            for sub in range(ncols // 128):
                stg = stpool.get()
                for blk in range(NB):
                    ps = psum.get()
                    for kc in range(8):
                        cx.mm(ps.t[:, :], w.t[:, kc, sub * 128:(sub + 1) * 128], hT.t[:, kc, blk * 512:(blk + 1) * 512],
                              kc == 0, kc == 7, [w.b, bh[blk]], [ps.b])
                    o = stg.t[:, blk * 512:(blk + 1) * 512]
                    if kind == "gate":
                        cx.act(o, ps.t[:, :], AF.Sigmoid, [ps.b], [stg.b])
                    else:
                        cx.copy("vector" if ev % 2 == 0 else "scalar", o, ps.t[:, :], [ps.b], [stg.b])
                        ev += 1
                if kind == "gate":
                    dst = GATES[dest + sub * 128:dest + (sub + 1) * 128, :]
                    cx.dma("sync", dst, stg.t[:, :], [stg.b], ())
                else:
                    dst = PFM[dest[0], dest[1] + sub * 128:dest[1] + (sub + 1) * 128, :]
                    db_ = Buf("pfmrow")
                    cx.dma("sync", dst, stg.t[:, :], [stg.b], [db_])
                    if xch:
                        r0_ = dest[0] * 1536 + dest[1] + sub * 128
                        xch["pfm"].wrote(r0_, r0_ + 128, Tc, db_)
        run_jobs(jobs)
        vst = cx.sbpool(2, [128, 4, 512], BF16, "a_vst")
        for g in range(2):
            w = wpool.get()
            cx.dma("gpsimd", w.t[:, :, 0:256], wv[:, :, 1536 + g * 256:1536 + (g + 1) * 256], (), [w.b])
            cx.dma("gpsimd", w.t[:, :, 256:512], wv[:, :, 3592 + g * 256:3592 + (g + 1) * 256], (), [w.b])
            vv = VTOK[g].rearrange("(n p) c -> p n c", p=128)
            for t4 in range(Tc // 512):
                stg = vst.get()
                for ti in range(4):
                    tt_ = t4 * 4 + ti
                    ps = psum.get()
                    for kc in range(8):
                        cx.mm(ps.t[:, :], hT.t[:, kc, tt_ * 128:(tt_ + 1) * 128], w.t[:, kc, :], kc == 0, kc == 7,
                              [w.b, bh[tt_ // 4]], [ps.b])
                    cx.copy("vector" if ti % 2 == 0 else "scalar", stg.t[:, ti, :], ps.t[:, :], [ps.b], [stg.b])
                db_ = Buf("vtokrow")
                cx.dma("sync", vv[:, t4 * 4:(t4 + 1) * 4, :], stg.t[:, :, :], [stg.b], [db_])
                if xch:
                    xch["vtok"].wrote(g * Tc + t4 * 512, g * Tc + (t4 + 1) * 512, 512, db_)
                    flush_all()
        w8 = cx.sb([128, 8, 16], BF16, "a_w8")
        for g in range(2):
            cx.dma("gpsimd", w8.t[:, :, g * 4:g * 4 + 2], wv[:, :, 2560 + 2 * g:2560 + 2 * g + 2], (), [w8.b])
            cx.dma("gpsimd", w8.t[:, :, g * 4 + 2:g * 4 + 4], wv[:, :, 2564 + 2 * g:2564 + 2 * g + 2], (), [w8.b])
        cx.dma("gpsimd", w8.t[:, :, 8:16], wv[:, :, 4104:4112], (), [w8.b])
        NT = Tc // 128
        gacc = cx.sb([128, NT, 8], F32, "a_gacc")
        ps = psum.get()
        for tt_ in range(NT):
            for kc in range(8):
                cx.mm(ps.t[:, tt_ * 8:(tt_ + 1) * 8], hT.t[:, kc, tt_ * 128:(tt_ + 1) * 128], w8.t[:, kc, 0:8],
                      kc == 0, kc == 7, [w8.b, bh[tt_ // 4]], [ps.b])
        cx.copy("vector", gacc.t[:, :, :], ps.t[:, 0:NT * 8].rearrange("p (n c) -> p n c", c=8), [ps.b], [gacc.b])
        for g in range(2):
            db_ = Buf("gtokrow")
            cx.dma("sync", GTOK[g], gacc.t[:, :, g * 4:(g + 1) * 4], [gacc.b], [db_])
            if xch:
                xch["gtok"].wrote(g * 128, (g + 1) * 128, (Tc // 128) * 4, db_)
        fst = cx.sb([8, Tc], F32, "a_fst")
        for blk in range(NB):
            ps = psum.get()
            for kc in range(8):
                cx.mm(ps.t[0:8, :], w8.t[:, kc, 8:16], hT.t[:, kc, blk * 512:(blk + 1) * 512], kc == 0, kc == 7,
                      [w8.b, bh[blk]], [ps.b])
            cx.copy("vector", fst.t[0:8, blk * 512:(blk + 1) * 512], ps.t[0:8, :], [ps.b], [fst.b])
        db_ = Buf("ffmrow")
        cx.dma("sync", FFM.rearrange("g h t -> (g h) t"), fst.t[:, :], [fst.b], [db_])
        if xch:
            xch["ffm"].wrote(0, 8, Tc, db_)
        flush_all()
        run_jobs(gate_jobs[:3])
        if mid_gates is not None:
            mid_gates()
        run_jobs(gate_jobs[3:])
        cx.fw.barrier()
        cx.st = old


def build_phase_a(Tc):
    nc = bass.Bass("TRN2", target_bir_lowering=False)
    xT = dram(nc, "xT", [D, Tc], F32, "ExternalInput")
    g_in = dram(nc, "norm_g", [128, 8], F32, "ExternalInput")
    w_in = dram(nc, "w_in", [D, IN_W], F32, "ExternalInput")
    PFM = dram(nc, "pfm", [2, 1536, Tc], BF16, "ExternalOutput")
    GATES = dram(nc, "gates", [3072, Tc], BF16, "ExternalOutput")
    VTOK = dram(nc, "vtok", [2, Tc, 512], BF16, "ExternalOutput")
    GTOK = dram(nc, "gtok", [2, 128, Tc // 128, 4], F32, "ExternalOutput")
    FFM = dram(nc, "ffm", [2, 4, Tc], F32, "ExternalOutput")
    cx = Cx(nc)
    c = make_consts(cx)
    add_eps(cx, c)
    psum = cx.psum_banks(8)
    emit_phase_a(cx, c, Tc, xT, g_in, w_in, PFM, GATES, VTOK, GTOK, FFM, psum)
    cx.finish()
    return nc


TS = 1024


DBG = None


def emit_phase_c(cx, c, Tc, xT, MIN, GATES, w_br, w_out, ffn_g, ffw, outT, psum, moe=False, final_g=None, dyn_min=0,
                 wq="gpsimd", wbuf16=None):
    wrd = [wbuf16] if wbuf16 is not None else []
    F = D_FFE if moe else D_FF
    NFC = F // 128
    NSB = Tc // TS
    NBK = TS // 512
    xTv = xT.rearrange("(kc p) t -> p kc t", p=128)
    oTv = outT.rearrange("(kc p) t -> p kc t", p=128)
    gav = GATES.rearrange("(br oc p) t -> p br oc t", br=3, oc=8, p=128)
    with contextlib.ExitStack() as st:
        old, cx.st = cx.st, st
        gn = cx.sb([128, 8], F32, "c_gn")
        cx.dma("sync", gn.t[:], ffn_g[:, :], (), [gn.b])
        if final_g is not None:
            gf = cx.sb([128, 8], F32, "c_gf")
            cx.dma("sync", gf.t[:], final_g[:, :], (), [gf.b])
        xt = cx.sb([128, 8, TS], F32, "c_x")
        h2 = cx.sb([128, 8, TS], BF16, "c_h2")
        R = cx.sb([128, 28, TS], BF16, "c_R")
        bR = [Buf("R_mt"), Buf("R_mg"), Buf("R_rest")]

        def rbuf(ch):
            return bR[0] if ch < 12 else (bR[1] if ch < 20 else bR[2])
        W = cx.sb([128, 3 * 8192], BF16, "c_W")
        bW = [Buf("W0"), Buf("W1"), Buf("W2")]
        wbr = W.t[:, 0:12288].rearrange("p (q c) -> p q c", c=1024)
        wout = W.t[:, 12288:20480].rearrange("p (q c) -> p q c", c=1024)
        wrot = Rot([(W.t[:, i * 8192:(i + 1) * 8192], bW[i]) for i in range(3)])
        gpool = cx.sbpool(2, [128, 3, 512], BF16, "c_gt")
        sqpool = cx.sbpool(3, [128, 512], F32, "c_sq")
        tmp = cx.sbpool(5, [128, 512], F32, "c_tmp")
        if moe:
            w_router, b_router = ffw[0], ffw[1]
            wr = cx.sb([128, 8, 8], F32, "c_wr")
            cx.dma("sync", wr.t[:], w_router.rearrange("(kc p) e -> p kc e", p=128), (), [wr.b])
            for kc in range(8):
                cx.ts("vector", wr.t[:, kc, :], wr.t[:, kc, :], gn.t[:, kc:kc + 1], ALU.mult, [wr.b, gn.b], [wr.b])
            br_ = cx.sb([8, 1], F32, "c_br")
            cx.dma("sync", br_.t[:], b_router[:, :], (), [br_.b])
            SEL = cx.sb([8, 8, 128], F32, "c_sel")
            cx.memset("gpsimd", SEL.t[:], 0.0, [SEL.b])
            cx.fw.op("gpsimd", lambda e: e.affine_select(out=SEL.t[:], in_=SEL.t[:], compare_op=ALU.not_equal,
                                                         fill=1.0, base=0, pattern=[[-1, 8], [0, 128]],
                                                         channel_multiplier=1), [SEL.b], [SEL.b])
            LT = cx.sb([8, TS], F32, "c_LT")
            GT = cx.sb([8, TS], F32, "c_GT")
            gbpool = cx.sbpool(2, [128, TS], F32, "c_gb")
            small = cx.sbpool(12, [128, 8], F32, "c_small")
            CAP = int(os.environ.get("MOE_CAP", "384"))
            NST = CAP // 128
            CUM = cx.sb([8, TS], F32, "c_cum")
            posb = cx.sb([128, TS], F32, "c_posb")
            PM = cx.sb([128, 8, 16], F32, "c_pm")
            IOTA1 = cx.sb([128, CAP], F32, "c_iota")
            SLOT = cx.sb([128, NST], F32, "c_slot")
            cx.fw.op("gpsimd", lambda e: e.iota(IOTA1.t[:, :], pattern=[[1, CAP]], base=1, channel_multiplier=0,
                                                allow_small_or_imprecise_dtypes=True), (), [IOTA1.b])
            cx.fw.op("gpsimd", lambda e: e.iota(SLOT.t[:, :], pattern=[[128, NST]], base=1, channel_multiplier=1,
                                                allow_small_or_imprecise_dtypes=True), (), [SLOT.b])
            flg = cx.sb([1, 8], F32, "c_flg")
            flgi = cx.sb([1, 1], mybir.dt.int32, "c_flgi")
            cx.n += 1
            pTb = cx.st.enter_context(cx.nc.psum_tensor(f"c_psT{cx.n}", [128, 1024], BF16))
            bpT = Buf("c_pT")
            Rf = R.t[:, :, :].rearrange("p a b -> p (a b)")
            ACTE = Rf[:, 0:28 * CAP].rearrange("p (a b) -> p a b", b=CAP)
            H2T = Rf[:, 10752:10752 + 8192].rearrange("p (a b) -> p a b", b=1024)
            SELt = Rf[:, 18944:18944 + 8 * CAP].rearrange("p (a b) -> p a b", b=CAP)
            SELT = Rf[:, 22016:22016 + NST * 1024].rearrange("p (a b) -> p a b", b=1024)
            Xf = h2.t[:, :, :].rearrange("p a b -> p (a b)")
            H2E = Xf[:, 0:8 * CAP].rearrange("p (a b) -> p a b", b=CAP)
            YE = Xf[:, 3072:3072 + NST * 1024].rearrange("p (a b) -> p a b", b=1024)
        for sb in range(NSB):
            t0 = sb * TS
            cx.dma("sync", xt.t[:], xTv[:, :, t0:t0 + TS], (), [xt.b])
            for g_ in range(2):
                cx.dma("sync", R.t[:, g_ * 6:(g_ + 1) * 6, :], MIN[g_].rearrange("(j p) t -> p j t", p=128)[:, :, t0:t0 + TS],
                       (), [bR[0]], dyn=dyn_min)
            for br in range(3):
                cx.dma("gpsimd", wbr[:, br * 4:(br + 1) * 4, :], w_br[br].rearrange("(n p) c -> p n c", p=128), (),
                       [bW[0], bW[1]])
            cx.dma("gpsimd", wout, w_out.rearrange("(kc p) c -> p kc c", p=128), (), [bW[1], bW[2]])
            for oc in range(8):
                for blk in range(NBK):
                    bs = slice(blk * 512, (blk + 1) * 512)
                    gt = gpool.get()
                    cx.dma("sync", gt.t[:], gav[:, :, oc, t0 + blk * 512:t0 + (blk + 1) * 512], (), [gt.b])
                    pp = []
                    for br in range(3):
                        ps = psum.get()
                        for n in range(4):
                            ch = (n // 2) * 6 + 2 * br + (n % 2)
                            cx.mm(ps.t[:, :], wbr[:, br * 4 + n, oc * 128:(oc + 1) * 128], R.t[:, ch, bs],
                                  n == 0, n == 3, [bW[0], bW[1], bR[0]], [ps.b])
                        pp.append(ps)
                    t1, t2, t3 = tmp.get(), tmp.get(), tmp.get()
                    cx.tt("vector", t1.t[:, :], pp[0].t[:, :], gt.t[:, 0, :], ALU.mult, [pp[0].b, gt.b], [t1.b])
                    cx.tt("vector", t2.t[:, :], pp[1].t[:, :], gt.t[:, 1, :], ALU.mult, [pp[1].b, gt.b], [t2.b])
                    cx.tt("vector", t3.t[:, :], pp[2].t[:, :], gt.t[:, 2, :], ALU.mult, [pp[2].b, gt.b], [t3.b])
                    cx.tt("gpsimd", t1.t[:, :], t1.t[:, :], t2.t[:, :], ALU.add, [t1.b, t2.b], [t1.b])
                    cx.tt("gpsimd", R.t[:, 12 + oc, bs], t1.t[:, :], t3.t[:, :], ALU.add, [t1.b, t3.b], [bR[1]])
            for oc in range(8):
                for blk in range(NBK):
                    bs = slice(blk * 512, (blk + 1) * 512)
                    ps = psum.get()
                    for kc in range(8):
                        cx.mm(ps.t[:, :], wout[:, kc, oc * 128:(oc + 1) * 128], R.t[:, 12 + kc, bs], kc == 0, kc == 7,
                              [bW[1], bW[2], bR[1]], [ps.b])
                    cx.tt("vector", xt.t[:, oc, bs], xt.t[:, oc, bs], ps.t[:, :], ALU.add, [ps.b, xt.b], [xt.b])
            for blk in range(NBK):
                bs = slice(blk * 512, (blk + 1) * 512)
                rstd = rmsnorm_stats(cx, c, lambda kc: xt.t[:, kc, bs], xt.b, 512, psum, sqpool, tmp)
                for kc in range(8):
                    cx.stt(h2.t[:, kc, bs], xt.t[:, kc, bs], gn.t[:, kc:kc + 1], rstd.t[:, :], ALU.mult, ALU.mult,
                           [xt.b, gn.b, rstd.b], [h2.b])
                if moe:
                    ps = psum.get()
                    for kc in range(8):
                        cx.mm(ps.t[0:8, :], wr.t[:, kc, :], xt.t[:, kc, bs], kc == 0, kc == 7, [wr.b, xt.b], [ps.b])
                    cx.tt("vector", LT.t[0:8, bs], ps.t[0:8, :], rstd.t[0:8, :], ALU.mult, [ps.b, rstd.b], [LT.b])
                    cx.ts("vector", LT.t[0:8, bs], LT.t[0:8, bs], br_.t[0:8, 0:1], ALU.add, [LT.b, br_.b], [LT.b])
                    pg = psum.get()
                    for ti in range(4):
                        cs = slice(blk * 512 + ti * 128, blk * 512 + (ti + 1) * 128)
                        pl = psum.get()
                        cx.transpose(pl.t[:, 0:8], LT.t[0:8, cs], c["id_f"].t[0:8, 0:8], [LT.b, c["id_f"].b], [pl.b])
                        lg, m1, eq, lg2, m2, sel, nm1, ex, w_, den, G = [small.get() for _ in range(11)]
                        cx.copy("vector", lg.t[:, :], pl.t[:, 0:8], [pl.b], [lg.b])
                        cx.fw.op("vector", lambda e, m1=m1, lg=lg: e.tensor_reduce(out=m1.t[:, 0:1], in_=lg.t[:, :], axis=AX.X, op=ALU.max), [lg.b], [m1.b])
                        cx.ts("vector", eq.t[:, :], lg.t[:, :], m1.t[:, 0:1], ALU.is_equal, [lg.b, m1.b], [eq.b])
                        cx.stt(lg2.t[:, :], eq.t[:, :], -1e30, lg.t[:, :], ALU.mult, ALU.add, [eq.b, lg.b], [lg2.b])
                        cx.fw.op("vector", lambda e, m2=m2, lg2=lg2: e.tensor_reduce(out=m2.t[:, 0:1], in_=lg2.t[:, :], axis=AX.X, op=ALU.max), [lg2.b], [m2.b])
                        cx.ts("vector", sel.t[:, :], lg.t[:, :], m2.t[:, 0:1], ALU.is_ge, [lg.b, m2.b], [sel.b])
                        cx.ts("vector", nm1.t[:, 0:1], m1.t[:, 0:1], -1.0, ALU.mult, [m1.b], [nm1.b])
                        cx.act(ex.t[:, :], lg.t[:, :], AF.Exp, [lg.b, nm1.b], [ex.b], bias=nm1.t[:, 0:1])
                        cx.tt("vector", w_.t[:, :], ex.t[:, :], sel.t[:, :], ALU.mult, [ex.b, sel.b], [w_.b])
                        cx.fw.op("vector", lambda e, den=den, w_=w_: e.tensor_reduce(out=den.t[:, 0:1], in_=w_.t[:, :], axis=AX.X, op=ALU.add), [w_.b], [den.b])
                        cx.fw.op("vector", lambda e, den=den: e.reciprocal(out=den.t[:, 0:1], in_=den.t[:, 0:1]), [den.b], [den.b])
                        cx.ts("vector", G.t[:, :], w_.t[:, :], den.t[:, 0:1], ALU.mult, [w_.b, den.b], [G.b])
                        cx.transpose(pg.t[0:8, ti * 128:(ti + 1) * 128], G.t[:, 0:8], c["id_f"].t[:, :], [G.b, c["id_f"].b], [pg.b])
                    cx.copy("vector", GT.t[0:8, bs], pg.t[0:8, :], [pg.b], [GT.b])
                if moe and DBG is not None:
                    cx.dma("sync", DBG[0:8, t0:t0 + TS], GT.t[0:8, :], [GT.b], ())
                    cx.dma("sync", DBG[8:16, t0:t0 + TS], LT.t[0:8, :], [LT.b], ())
            def dense_ffn():
                for e_ in range(NEXP if moe else 1):
                    if moe:
                        wg_v = ffw[2][e_].rearrange("(kc p) n -> p kc n", p=128)
                        wu_v = ffw[3][e_].rearrange("(kc p) n -> p kc n", p=128)
                        wd_v = ffw[4][e_].rearrange("(kc p) c -> p kc c", p=128)
                        gb = gbpool.get()
                        for blk in range(NBK):
                            bs = slice(blk * 512, (blk + 1) * 512)
                            ps = psum.get()
                            cx.mm(ps.t[:, :], SEL.t[0:8, e_, :], GT.t[0:8, bs], True, True, [SEL.b, GT.b], [ps.b])
                            cx.copy("vector", gb.t[:, bs], ps.t[:, :], [ps.b], [gb.b])
                    else:
                        wg_v = ffw[0].rearrange("(kc p) n -> p kc n", p=128)
                        wu_v = ffw[1].rearrange("(kc p) n -> p kc n", p=128)
                        wd_v = ffw[2].rearrange("(kc p) c -> p kc c", p=128)
                    for s0 in range(0, F, 512):
                        ncol = min(512, F - s0)
                        wt, wb = wrot.get()
                        wgt = wt[:, 0:4096].rearrange("p (k c) -> p k c", c=512)
                        wut = wt[:, 4096:8192].rearrange("p (k c) -> p k c", c=512)
                        cx.dma(wq, wgt[:, :, :ncol], wg_v[:, :, s0:s0 + ncol], wrd, [wb])
                        cx.dma(wq, wut[:, :, :ncol], wu_v[:, :, s0:s0 + ncol], wrd, [wb])
                        for sub in range(ncol // 128):
                            ch = s0 // 128 + sub
                            for blk in range(NBK):
                                bs = slice(blk * 512, (blk + 1) * 512)
                                pg_, pu_ = psum.get(), psum.get()
                                for kc in range(8):
                                    cx.mm(pg_.t[:, :], wgt[:, kc, sub * 128:(sub + 1) * 128], h2.t[:, kc, bs], kc == 0, kc == 7,
                                          [wb, h2.b], [pg_.b])
                                for kc in range(8):
                                    cx.mm(pu_.t[:, :], wut[:, kc, sub * 128:(sub + 1) * 128], h2.t[:, kc, bs], kc == 0, kc == 7,
                                          [wb, h2.b], [pu_.b])
                                sg = tmp.get()
                                cx.act(sg.t[:, :], pg_.t[:, :], AF.Silu, [pg_.b], [sg.b])
                                cx.tt("vector", R.t[:, ch, bs], pu_.t[:, :], sg.t[:, :], ALU.mult, [pu_.b, sg.b], [rbuf(ch)])
                    KGS = 7 if NFC % 7 == 0 else 11
                    NG = NFC // KGS
                    for blk in range(NBK):
                        bs = slice(blk * 512, (blk + 1) * 512)
                        for och in range(2):
                            accs = [psum.get() for _ in range(4)]
                            for kg in range(NG):
                                wt, wb = wrot.get()
                                wdt = wt[:, 0:KGS * 512].rearrange("p (k c) -> p k c", c=512)
                                cx.dma(wq, wdt, wd_v[:, kg * KGS:(kg + 1) * KGS, och * 512:(och + 1) * 512], wrd, [wb])
                                for o4 in range(4):
                                    for k in range(KGS):
                                        kc = kg * KGS + k
                                        cx.mm(accs[o4].t[:, :], wdt[:, k, o4 * 128:(o4 + 1) * 128], R.t[:, kc, bs], kc == 0,
                                              kc == NFC - 1, [wb, rbuf(kc)], [accs[o4].b])
                            for o4 in range(4):
                                oc = och * 4 + o4
                                ps = accs[o4]
                                if moe:
                                    tm = tmp.get()
                                    cx.tt("vector", tm.t[:, :], ps.t[:, :], gb.t[:, bs], ALU.mult, [ps.b, gb.b], [tm.b])
                                    cx.tt("vector", xt.t[:, oc, bs], xt.t[:, oc, bs], tm.t[:, :], ALU.add, [tm.b, xt.b], [xt.b])
                                else:
                                    cx.tt("vector", xt.t[:, oc, bs], xt.t[:, oc, bs], ps.t[:, :], ALU.add, [ps.b, xt.b], [xt.b])

            def routed_ffn():
                bh2t, bpm, bsel, bselt, bh2e, bacte, bye = [Buf(n_) for n_ in "h2t pm sel selt h2e acte ye".split()]
                for tile in range(8):
                    for kc in range(8):
                        cx.transpose(pTb[:, kc * 128:(kc + 1) * 128], h2.t[:, kc, tile * 128:(tile + 1) * 128], c["id_b"].t[:, :],
                                     [h2.b, c["id_b"].b], [bpT])
                    cx.copy("vector" if tile % 2 == 0 else "scalar", H2T[:, tile, :], pTb[:, :], [bpT], [bh2t])
                for tile in range(8):
                    ps = psum.get()
                    cx.transpose(ps.t[:, 0:8], CUM.t[0:8, tile * 128:(tile + 1) * 128], c["id_f"].t[0:8, 0:8], [CUM.b, c["id_f"].b], [ps.b])
                    cx.transpose(ps.t[:, 8:16], LT.t[0:8, tile * 128:(tile + 1) * 128], c["id_f"].t[0:8, 0:8], [LT.b, c["id_f"].b], [ps.b])
                    cx.copy("vector", PM.t[:, tile, :], ps.t[:, 0:16], [ps.b], [PM.b])
                cx.fw.barrier()
                for e_ in range(NEXP):
                    wg_v = ffw[2][e_].rearrange("(kc p) n -> p kc n", p=128)
                    wu_v = ffw[3][e_].rearrange("(kc p) n -> p kc n", p=128)
                    wd_v = ffw[4][e_].rearrange("(kc p) c -> p kc c", p=128)
                    gb = gbpool.get()
                    for blk in range(NBK):
                        bs = slice(blk * 512, (blk + 1) * 512)
                        ps = psum.get()
                        cx.mm(ps.t[:, :], SEL.t[0:8, e_, :], GT.t[0:8, bs], True, True, [SEL.b, GT.b], [ps.b])
                        cx.copy("scalar", gb.t[:, bs], ps.t[:, :], [ps.b], [gb.b])
                        ps = psum.get()
                        cx.mm(ps.t[:, :], SEL.t[0:8, e_, :], CUM.t[0:8, bs], True, True, [SEL.b, CUM.b], [ps.b])
                        cx.copy("scalar", posb.t[:, bs], ps.t[:, :], [ps.b], [posb.b])
                    for tile in range(8):
                        cx.ts("vector", SELt[:, tile, :], IOTA1.t[:, :], PM.t[:, tile, e_:e_ + 1], ALU.is_equal,
                              [IOTA1.b, PM.b], [bsel], s2=PM.t[:, tile, 8 + e_:9 + e_], op1=ALU.mult)
                    for st_ in range(NST):
                        cx.stt(SELT[:, st_, :], posb.t[:, :], SLOT.t[:, st_:st_ + 1], gb.t[:, :], ALU.is_equal, ALU.mult,
                               [posb.b, SLOT.b, gb.b], [bselt])
                    for kc in range(8):
                        ps = psum.get()
                        for tile in range(8):
                            cx.mm(ps.t[:, 0:CAP], H2T[:, tile, kc * 128:(kc + 1) * 128], SELt[:, tile, :], tile == 0, tile == 7,
                                  [bh2t, bsel], [ps.b])
                        cx.copy("vector" if kc % 2 == 0 else "scalar", H2E[:, kc, :], ps.t[:, 0:CAP], [ps.b], [bh2e])
                    for s0 in range(0, F, 512):
                        wt, wb = wrot.get()
                        wgt = wt[:, 0:4096].rearrange("p (k c) -> p k c", c=512)
                        wut = wt[:, 4096:8192].rearrange("p (k c) -> p k c", c=512)
                        cx.dma(wq, wgt, wg_v[:, :, s0:s0 + 512], wrd, [wb])
                        cx.dma(wq, wut, wu_v[:, :, s0:s0 + 512], wrd, [wb])
                        for sub in range(4):
                            ch = s0 // 128 + sub
                            pg_, pu_ = psum.get(), psum.get()
                            for kc in range(8):
                                cx.mm(pg_.t[:, 0:CAP], wgt[:, kc, sub * 128:(sub + 1) * 128], H2E[:, kc, :], kc == 0, kc == 7,
                                      [wb, bh2e], [pg_.b])
                            for kc in range(8):
                                cx.mm(pu_.t[:, 0:CAP], wut[:, kc, sub * 128:(sub + 1) * 128], H2E[:, kc, :], kc == 0, kc == 7,
                                      [wb, bh2e], [pu_.b])
                            sg = tmp.get()
                            cx.act(sg.t[:, 0:CAP], pg_.t[:, 0:CAP], AF.Silu, [pg_.b], [sg.b])
                            cx.tt("vector", ACTE[:, ch, :], pu_.t[:, 0:CAP], sg.t[:, 0:CAP], ALU.mult, [pu_.b, sg.b], [bacte])
                    for fh in range(2):
                        accs = [psum.get() for _ in range(NST)]
                        for kg in range(4):
                            wt, wb = wrot.get()
                            wdt = wt[:, 0:7 * 512].rearrange("p (k c) -> p k c", c=512)
                            cx.dma(wq, wdt, wd_v[:, kg * 7:(kg + 1) * 7, fh * 512:(fh + 1) * 512], wrd, [wb])
                            for st_ in range(NST):
                                for k in range(7):
                                    kc = kg * 7 + k
                                    cx.mm(accs[st_].t[:, :], ACTE[:, kc, st_ * 128:(st_ + 1) * 128], wdt[:, k, :], kc == 0, kc == 27,
                                          [wb, bacte], [accs[st_].b])
                        for st_ in range(NST):
                            cx.copy("scalar" if st_ % 2 == 0 else "vector", YE[:, st_, fh * 512:(fh + 1) * 512], accs[st_].t[:, :],
                                    [accs[st_].b], [bye])
                    for fc in range(8):
                        for blk in range(NBK):
                            bs = slice(blk * 512, (blk + 1) * 512)
                            ps = psum.get()
                            for st_ in range(NST):
                                cx.mm(ps.t[:, :], YE[:, st_, fc * 128:(fc + 1) * 128], SELT[:, st_, bs], st_ == 0, st_ == NST - 1,
                                      [bye, bselt], [ps.b])
                            cx.tt("vector", xt.t[:, fc, bs], xt.t[:, fc, bs], ps.t[:, :], ALU.add, [ps.b, xt.b], [xt.b])

            if moe and os.environ.get("MOE_DENSE") is None:
                cx.ts("vector", LT.t[0:8, :], GT.t[0:8, :], 0.0, ALU.is_gt, [GT.b], [LT.b])
                cx.fw.op("vector", lambda e: e.tensor_tensor_scan(out=CUM.t[0:8, :], data0=LT.t[0:8, :], data1=LT.t[0:8, :],
                                                                  initial=0.0, op0=ALU.add, op1=ALU.max), [LT.b], [CUM.b])
                ps = psum.get()
                cx.transpose(ps.t[0:1, 0:8], CUM.t[0:8, TS - 1:TS], c["id_f"].t[0:8, 0:8], [CUM.b, c["id_f"].b], [ps.b])
                cx.copy("vector", flg.t[0:1, 0:8], ps.t[0:1, 0:8], [ps.b], [flg.b])
                cx.fw.op("vector", lambda e: e.tensor_reduce(out=flg.t[0:1, 0:1], in_=flg.t[0:1, 0:8], axis=AX.X, op=ALU.max),
                         [flg.b], [flg.b])
                cx.ts("vector", flg.t[0:1, 0:1], flg.t[0:1, 0:1], float(CAP) + 0.5, ALU.is_lt, [flg.b], [flg.b])
                cx.copy("vector", flgi.t[0:1, 0:1], flg.t[0:1, 0:1], [flg.b], [flgi.b])
                cx.fw.begin_if(flgi.t[0:1, 0:1])
                routed_ffn()
                cx.fw.begin_else()
                dense_ffn()
                cx.fw.end_if()
            else:
                dense_ffn()
            if final_g is not None:
                for blk in range(NBK):
                    bs = slice(blk * 512, (blk + 1) * 512)
                    rstd = rmsnorm_stats(cx, c, lambda kc: xt.t[:, kc, bs], xt.b, 512, psum, sqpool, tmp)
                    for kc in range(8):
                        cx.stt(xt.t[:, kc, bs], xt.t[:, kc, bs], gf.t[:, kc:kc + 1], rstd.t[:, :], ALU.mult, ALU.mult,
                               [xt.b, gf.b, rstd.b], [xt.b])
            cx.dma("sync", oTv[:, :, t0:t0 + TS], xt.t[:], [xt.b], ())
        cx.fw.barrier()
        cx.st = old


def precast_jobs(cx, pairs, buf):
    jobs = []
    for dst, src in pairs:
        E_, R_, C_ = src.shape
        for e in range(E_):
            for h in range(2):
                jobs.append(lambda dst=dst, src=src, e=e, h=h, R_=R_: cx.dma(
                    "gpsimd", dst[e, h * (R_ // 2):(h + 1) * (R_ // 2), :], src[e, h * (R_ // 2):(h + 1) * (R_ // 2), :], (), [buf]))
    return jobs


def emit_precast(cx, pairs, buf):
    for j in precast_jobs(cx, pairs, buf):
        j()


def build_phase_c(Tc, moe, final):
    nc = bass.Bass("TRN2", target_bir_lowering=False)
    F = D_FFE if moe else D_FF
    xT = dram(nc, "xT", [D, Tc], F32, "ExternalInput")
    MIN = dram(nc, "min", [2, 768, Tc], BF16, "ExternalInput")
    GATES = dram(nc, "gates", [3072, Tc], BF16, "ExternalInput")
    w_br = dram(nc, "w_br", [3, 512, D], F32, "ExternalInput")
    w_out = dram(nc, "w_out", [D, D], F32, "ExternalInput")
    ffn_g = dram(nc, "ffn_g", [128, 8], F32, "ExternalInput")
    if moe:
        ffw = (dram(nc, "w_router", [D, NEXP], F32, "ExternalInput"),
               dram(nc, "b_router", [NEXP, 1], F32, "ExternalInput"),
               dram(nc, "w_gate", [NEXP, D, F], F32, "ExternalInput"),
               dram(nc, "w_up", [NEXP, D, F], F32, "ExternalInput"),
               dram(nc, "w_down", [NEXP, F, D], F32, "ExternalInput"))
    else:
        ffw = (dram(nc, "w_gate", [D, F], F32, "ExternalInput"),
               dram(nc, "w_up", [D, F], F32, "ExternalInput"),
               dram(nc, "w_down", [F, D], F32, "ExternalInput"))
    final_g = dram(nc, "final_g", [128, 8], F32, "ExternalInput") if final else None
    outT = dram(nc, "outT", [D, Tc], F32, "ExternalOutput")
    cx = Cx(nc)
    c = make_consts(cx)
    add_eps(cx, c)
    psum = cx.psum_banks(7)
    if moe:
        N = lambda name, shape: nc.dram_tensor(name, list(shape), BF16).ap()
        g16, u16, d16 = N("w16g", [NEXP, D, F]), N("w16u", [NEXP, D, F]), N("w16d", [NEXP, F, D])
        b16 = Buf("w16")
        emit_precast(cx, [(g16, ffw[2]), (u16, ffw[3]), (d16, ffw[4])], b16)
        ffw = (ffw[0], ffw[1], g16, u16, d16)
        emit_phase_c(cx, c, Tc, xT, MIN, GATES, w_br, w_out, ffn_g, ffw, outT, psum, moe=moe, final_g=final_g, wq="sync", wbuf16=b16)
    else:
        emit_phase_c(cx, c, Tc, xT, MIN, GATES, w_br, w_out, ffn_g, ffw, outT, psum, moe=moe, final_g=final_g)
    cx.finish()
    return nc


def emit_phase_b(cx, c, Tc, PFM, VTOK, GTOK, FFM, pool_w, pool_scale, pool_coef, pool_rc, conv_w, ml_b, ml_ng,
                 fox_b, MOUT, psum, dyn=None, xch=None, pre_fox=None):
    dyn = dyn or {"pfm": 0, "vtok": 0, "gtok": 0, "ffm": 0}
    S = 2 * Tc
    NCH = S // 128
    nc = cx.nc
    banks = psum.items

    def loc(t):
        return t // Tc, t % Tc

    with contextlib.ExitStack() as st:
        old, cx.st = cx.st, st
        PB = min(1024, Tc)
        L = PB + 16
        pw_f = cx.sb([128, 2, 128], F32, "p_wf")
        pw = cx.sb([128, 2, 128], BF16, "p_w")
        cx.dma("gpsimd", pw.t[:], pool_w[:, :, :], (), [pw.b])
        psc = cx.sb([128, 2], F32, "p_sc")
        cx.dma("sync", psc.t[:], pool_scale[:, :], (), [psc.b])
        pco = cx.sb([128, 2, 4], F32, "p_co")
        cx.dma("sync", pco.t[:], pool_coef[:, :, :], (), [pco.b])
        prc = cx.sb([128, 2, 4, 16], F32, "p_rc")
        cx.dma("sync", prc.t[:], pool_rc[:, :, :, :], (), [prc.b])
        upool = cx.sbpool(3, [128, L], BF16, "p_u")
        wtsets = Rot([[cx.sb([128, L], F32, f"p_w{j}_{i}") for i in range(4)] for j in range(3)])
        dpool = cx.sbpool(3, [128, L], F32, "p_d")
        dbp = cx.sbpool(3, [128, PB], BF16, "p_db")
        opool = cx.sbpool(3, [128, PB], BF16, "p_o")
        t16 = cx.sbpool(4, [128, 16], F32, "p_t16")
        for gi in range(2):
            for t0 in range(0, S, PB):
                hf, tl = loc(t0)
                U = upool.get()
                wt = wtsets.get()
                cx.dma("sync", U.t[:, 16:L], PFM[hf, gi * 128:(gi + 1) * 128, tl:tl + PB], (), [U.b], dyn=dyn["pfm"])
                if t0 == 0:
                    cx.memset("gpsimd", U.t[:, 0:16], 0.0, [U.b])
                else:
                    hp, tp = loc(t0 - 16)
                    cx.dma("sync", U.t[:, 0:16], PFM[hp, gi * 128:(gi + 1) * 128, tp:tp + 16], (), [U.b], dyn=dyn["pfm"])
                src = U
                for k, sh in enumerate((1, 2, 4, 8)):
                    lo = 2 * sh - 1
                    cx.tt("gpsimd", wt[k].t[:, lo:L], src.t[:, lo:L], src.t[:, lo - sh:L - sh],
                          ALU.add, [src.b], [wt[k].b])
                    src = wt[k]
                d = dpool.get()
                cx.stt(d.t[:, 16:L], wt[0].t[:, 16:L], pco.t[:, gi, 0:1], U.t[:, 16:L], ALU.mult, ALU.subtract,
                       [wt[0].b, pco.b, U.b], [d.b])
                for k in range(1, 4):
                    cx.stt(d.t[:, 16:L], wt[k].t[:, 16:L], pco.t[:, gi, k:k + 1], d.t[:, 16:L], ALU.mult, ALU.add,
                           [wt[k].b, pco.b, d.b], [d.b])
                if t0 == 0:
                    a0 = t16.get()
                    cx.tt("vector", a0.t[:, :], wt[0].t[:, 16:32], prc.t[:, gi, 0, :], ALU.mult, [wt[0].b, prc.b], [a0.b])
                    for k in range(1, 4):
                        a1 = t16.get()
                        cx.tt("vector", a1.t[:, :], wt[k].t[:, 16:32], prc.t[:, gi, k, :], ALU.mult, [wt[k].b, prc.b], [a1.b])
                        cx.tt("vector", a0.t[:, :], a0.t[:, :], a1.t[:, :], ALU.add, [a0.b, a1.b], [a0.b])
                    cx.tt("vector", d.t[:, 16:32], a0.t[:, :], U.t[:, 16:32], ALU.subtract, [a0.b, U.b], [d.b])
                db = dbp.get()
                cx.copy("scalar", db.t[:, :], d.t[:, 16:L], [d.b], [db.b])
                o = opool.get()
                for blk in range(PB // 512):
                    ps = psum.get()
                    cx.mm(ps.t[:, :], pw.t[:, gi, :], db.t[:, blk * 512:(blk + 1) * 512], True, True, [pw.b, db.b], [ps.b])
                    cx.act(o.t[:, blk * 512:(blk + 1) * 512], ps.t[:, :], AF.Copy, [ps.b, psc.b], [o.b],
                           scale=psc.t[:, gi:gi + 1])
                db_ = Buf("moutrow")
                cx.dma("scalar", MOUT[hf, gi * 128:(gi + 1) * 128, tl:tl + PB], o.t[:, :], [o.b], [db_])
                if xch:
                    xch.wrote(hf * 768 + gi * 128, hf * 768 + (gi + 1) * 128, PB, db_)
        if xch:
            xch.flush()
        cx.fw.barrier()
        cx.st = old

    with contextlib.ExitStack() as st:
        old, cx.st = cx.st, st
        TRI = cx.sb([128, 128], F32, "m_tri")
        cx.memset("gpsimd", TRI.t[:], 1.0, [TRI.b])
        cx.fw.op("gpsimd", lambda e: e.affine_select(out=TRI.t[:], in_=TRI.t[:], compare_op=ALU.is_ge, fill=0.0, base=0,
                                                     pattern=[[1, 128]], channel_multiplier=-1), [TRI.b], [TRI.b])
        NTRI = cx.sb([128, 128], F32, "m_ntri")
        cx.memset("gpsimd", NTRI.t[:], 0.0, [NTRI.b])
        cx.fw.op("gpsimd", lambda e: e.affine_select(out=NTRI.t[:], in_=NTRI.t[:], compare_op=ALU.is_ge, fill=-30000.0, base=0,
                                                     pattern=[[1, 128]], channel_multiplier=-1), [NTRI.b], [NTRI.b])
        cw = cx.sb([128, 2, 2, 4], F32, "m_cw")
        cx.dma("sync", cw.t[:], conv_w[:, :, :, :], (), [cw.b])
        mlb = cx.sb([128, 4], F32, "m_b")
        cx.dma("sync", mlb.t[:], ml_b[:, :], (), [mlb.b])
        mng = cx.sb([128, 2], F32, "m_ng")
        cx.dma("sync", mng.t[:], ml_ng[:, :], (), [mng.b])
        QK = [[cx.sb([128, S], BF16, f"m_qk{a}{h}") for h in range(2)] for a in range(2)]
        V = cx.sb([128, NCH, 256], BF16, "m_v")
        for hf in range(2):
            cx.dma("sync", V.t[:, hf * (Tc // 128):(hf + 1) * (Tc // 128), :],
                   VTOK[hf].rearrange("(n p) c -> p n c", p=128)[:, :, 0:256], (), [V.b], dyn=dyn["vtok"])
        GT = cx.sb([128, NCH, 4], F32, "m_gt")
        for hf in range(2):
            cx.dma("sync", GT.t[:, hf * (Tc // 128):(hf + 1) * (Tc // 128), :], GTOK[hf], (), [GT.b], dyn=dyn["gtok"])
        for col in range(4):
            cx.ts("vector", GT.t[:, :, col:col + 1], GT.t[:, :, col:col + 1], mlb.t[:, col:col + 1], ALU.add,
                  [GT.b, mlb.b], [GT.b])
        LF = cx.sb([128, NCH, 2], F32, "m_lf")
        cx.act(LF.t[:, :, :], GT.t[:, :, 2:4], AF.Exp, [GT.b], [LF.b], scale=-1.0)
        cx.act(LF.t[:, :, :], LF.t[:, :, :], AF.Ln, [LF.b, c["ones_f"].b], [LF.b], bias=c["ones_f"].t[:, 0:1])
        cx.ts("vector", LF.t[:, :, :], LF.t[:, :, :], -1.0, ALU.mult, [LF.b], [LF.b])
        CB = min(2048, Tc)
        with contextlib.ExitStack() as st2:
            cx.st = st2
            rpool = cx.sbpool(2, [128, CB + 3], BF16, "m_raw")
            apool = cx.sbpool(2, [128, CB], F32, "m_acc")
            for a in range(2):
                for h in range(2):
                    for t0 in range(0, S, CB):
                        hf, tl = loc(t0)
                        r0 = 256 * (1 + a) + h * 128
                        raw = rpool.get()
                        cx.dma("sync", raw.t[:, 3:CB + 3], PFM[hf, r0:r0 + 128, tl:tl + CB], (), [raw.b], dyn=dyn["pfm"])
                        if t0 == 0:
                            cx.memset("gpsimd", raw.t[:, 0:3], 0.0, [raw.b])
                        else:
                            hp, tp = loc(t0 - 3)
                            cx.dma("sync", raw.t[:, 0:3], PFM[hp, r0:r0 + 128, tp:tp + 3], (), [raw.b], dyn=dyn["pfm"])
                        acc = apool.get()
                        cx.ts("vector", acc.t[:, :], raw.t[:, 3:CB + 3], cw.t[:, a, h, 3:4], ALU.mult, [raw.b, cw.b], [acc.b])
                        for tap in range(3):
                            cx.stt(acc.t[:, :], raw.t[:, tap:CB + tap], cw.t[:, a, h, tap:tap + 1], acc.t[:, :],
                                   ALU.mult, ALU.add, [raw.b, cw.b, acc.b], [acc.b])
                        if a == 0:
                            cx.act(QK[a][h].t[:, t0:t0 + CB], acc.t[:, :], AF.Silu, [acc.b], [QK[a][h].b])
                        else:
                            cx.act(acc.t[:, :], acc.t[:, :], AF.Silu, [acc.b], [acc.b])
                            cx.ts("gpsimd", QK[a][h].t[:, t0:t0 + CB], acc.t[:, :], 128.0 ** -0.5, ALU.mult, [acc.b],
                                  [QK[a][h].b])
            cx.fw.barrier()
            cx.st = st
        CT = [cx.sb([128, 256], F32, f"m_ct{h}") for h in range(2)]
        CTb = [cx.sb([128, 256], BF16, f"m_ctb{h}") for h in range(2)]
        for h in range(2):
            cx.memset("gpsimd", CT[h].t[:], 0.0, [CT[h].b])
            cx.memset("gpsimd", CTb[h].t[:], 0.0, [CTb[h].b])
        H = [cx.sbpool(2, [128, 512], F32, f"m_H{h}") for h in range(2)]
        sm = cx.sbpool(16, [128, 2], F32, "m_sm")
        f128 = cx.sbpool(18, [128, 128], F32, "m_f128")
        b128 = cx.sbpool(16, [128, 128], BF16, "m_b128")
        f512 = cx.sbpool(6, [128, 512], F32, "m_f512")
        sqp = cx.sbpool(2, [128, 512], F32, "m_sq")
        opl = cx.sbpool(3, [128, 512], BF16, "m_o")
        cx.n += 1
        pT = cx.st.enter_context(nc.psum_tensor(f"m_psT{cx.n}", [128, 1024], BF16))
        prot = Rot(banks[0:7])
        pTb = [Buf("pT0"), Buf("pT1")]
        Hcur = [None, None]

        def stage_a(ch):
            cs = slice(ch * 128, (ch + 1) * 128)
            pss = prot.get()
            cx.mm(pss.t[:, 0:2], TRI.t[:, :], LF.t[:, ch, :], True, True, [TRI.b, LF.b], [pss.b])
            cx.mm(pss.t[:, 2:4], c["ones_f"].t[:, :], LF.t[:, ch, :], True, True, [c["ones_f"].b, LF.b], [pss.b])
            a_ = sm.get()
            cx.tt("vector", a_.t[:, :], GT.t[:, ch, 0:2], pss.t[:, 0:2], ALU.subtract, [GT.b, pss.b], [a_.b])
            ab = sm.get()
            cx.tt("vector", ab.t[:, :], a_.t[:, :], pss.t[:, 2:4], ALU.add, [a_.b, pss.b], [ab.b])
            wk = sm.get()
            cx.act(wk.t[:, :], ab.t[:, :], AF.Exp, [ab.b], [wk.b])
            dec = sm.get()
            cx.act(dec.t[:, :], pss.t[:, 2:4], AF.Exp, [pss.b], [dec.b])
            out = {"dec": dec, "h": []}
            for h in range(2):
                qT, kT = QK[0][h], QK[1][h]
                lfb = f128.get()
                cx.ts("gpsimd", lfb.t[:, :], c["ones_f"].t[:, :], LF.t[:, ch, h:h + 1], ALU.mult, [c["ones_f"].b, LF.b], [lfb.b])
                pb = prot.get()
                cx.mm(pb.t[:, 0:128], lfb.t[:, :], TRI.t[:, :], True, True, [lfb.b, TRI.b], [pb.b])
                cx.mm(pb.t[:, 128:256], kT.t[:, cs], qT.t[:, cs], True, True, [kT.b, qT.b], [pb.b])
                DT = f128.get()
                cx.act(DT.t[:, :], pb.t[:, 0:128], AF.Exp, [pb.b, a_.b], [DT.b], bias=a_.t[:, h:h + 1])
                cx.tt("gpsimd", DT.t[:, :], DT.t[:, :], TRI.t[:, :], ALU.mult, [DT.b, TRI.b], [DT.b])
                EB = f128.get()
                cx.act(EB.t[:, :], pb.t[:, 0:128], AF.Exp, [pb.b], [EB.b])
                PT = b128.get()
                cx.tt("vector", PT.t[:, :], DT.t[:, :], pb.t[:, 128:256], ALU.mult, [DT.b, pb.b], [PT.b])
                qs = b128.get()
                cx.tt("gpsimd", qs.t[:, :], qT.t[:, cs], EB.t[:, :], ALU.mult, [qT.b, EB.b], [qs.b])
                tb = pTb[h]
                cx.transpose(pT[:, h * 128:(h + 1) * 128], kT.t[:, cs], c["id_b"].t[:, :], [kT.b, c["id_b"].b], [tb])
                Kw = b128.get()
                cx.act(Kw.t[:, :], pT[:, h * 128:(h + 1) * 128], AF.Copy, [tb, wk.b], [Kw.b], scale=wk.t[:, h:h + 1])
                out["h"].append((PT, qs, Kw))
            return out

        def stage_b(ch, sa):
            dec = sa["dec"]
            for h in range(2):
                PT, qs, Kw = sa["h"][h]
                if ch % 4 == 0:
                    Hcur[h] = H[h].get()
                pn = prot.get()
                cx.mm(pn.t[:, 0:128], CTb[h].t[:, 0:128], qs.t[:, :], True, False, [CTb[h].b, qs.b], [pn.b])
                cx.mm(pn.t[:, 0:128], V.t[:, ch, h * 128:(h + 1) * 128], PT.t[:, :], False, True, [V.b, PT.b], [pn.b])
                cx.mm(pn.t[:, 128:256], CTb[h].t[:, 128:256], qs.t[:, :], True, False, [CTb[h].b, qs.b], [pn.b])
                cx.mm(pn.t[:, 128:256], c["ones_b"].t[:, :], PT.t[:, :], False, True, [c["ones_b"].b, PT.b], [pn.b])
                pu = prot.get()
                cx.mm(pu.t[:, 0:128], Kw.t[:, :], V.t[:, ch, h * 128:(h + 1) * 128], True, True, [Kw.b, V.b], [pu.b])
                cx.mm(pu.t[:, 128:256], Kw.t[:, :], c["ones_b"].t[:, :], True, True, [Kw.b, c["ones_b"].b], [pu.b])
                cx.stt(CT[h].t[:, :], CT[h].t[:, :], dec.t[:, h:h + 1], pu.t[:, 0:256], ALU.mult, ALU.add,
                       [CT[h].b, dec.b, pu.b], [CT[h].b])
                cx.copy("gpsimd", CTb[h].t[:, :], CT[h].t[:, :], [CT[h].b], [CTb[h].b])
                r = f128.get()
                cx.ts("vector", r.t[:, :], pn.t[:, 128:256], 1.0, ALU.max, [pn.b], [r.b])
                cx.stt(r.t[:, :], pn.t[:, 128:256], -1.0, r.t[:, :], ALU.mult, ALU.max, [pn.b, r.b], [r.b])
                cx.fw.op("vector", lambda e, r=r: e.reciprocal(out=r.t[:, :], in_=r.t[:, :]), [r.b], [r.b])
                Hc = Hcur[h]
                cx.tt("vector", Hc.t[:, (ch % 4) * 128:(ch % 4 + 1) * 128], pn.t[:, 0:128], r.t[:, :], ALU.mult,
                      [pn.b, r.b], [Hc.b])
                if ch % 4 == 3:
                    t0 = (ch - 3) * 128
                    hf, tl = loc(t0)
                    sq = sqp.get()
                    cx.tt("gpsimd", sq.t[:, :], Hc.t[:, :], Hc.t[:, :], ALU.mult, [Hc.b], [sq.b])
                    ps = prot.get()
                    cx.mm(ps.t[:, :], c["ones_f"].t[:, :], sq.t[:, :], True, True, [c["ones_f"].b, sq.b], [ps.b])
                    ln = f512.get()
                    cx.act(ln.t[:, :], ps.t[:, :], AF.Ln, [ps.b, c["eps"].b], [ln.b], bias=c["eps"].t[:, 0:1], scale=1.0 / 128)
                    rstd = f512.get()
                    cx.act(rstd.t[:, :], ln.t[:, :], AF.Exp, [ln.b], [rstd.b], scale=-0.5)
                    hn = f512.get()
                    cx.stt(hn.t[:, :], Hc.t[:, :], mng.t[:, h:h + 1], rstd.t[:, :], ALU.mult, ALU.mult,
                           [Hc.b, mng.b, rstd.b], [hn.b])
                    og = opl.get()
                    cx.dma("sync", og.t[:, :], PFM[hf, 768 + h * 128:768 + (h + 1) * 128, tl:tl + 512], (), [og.b], dyn=dyn["pfm"])
                    eo = f512.get()
                    cx.act(eo.t[:, :], og.t[:, :], AF.Exp, [og.b], [eo.b], scale=-1.0)
                    cx.ts("gpsimd", eo.t[:, :], eo.t[:, :], 1.0, ALU.add, [eo.b], [eo.b])
                    cx.fw.op("vector", lambda e, eo=eo: e.reciprocal(out=eo.t[:, :], in_=eo.t[:, :]), [eo.b], [eo.b])
                    ob = opl.get()
                    cx.tt("vector", ob.t[:, :], hn.t[:, :], eo.t[:, :], ALU.mult, [hn.b, eo.b], [ob.b])
                    db_ = Buf("moutrow")
                    cx.dma("sync", MOUT[hf, 256 + h * 128:256 + (h + 1) * 128, tl:tl + 512], ob.t[:, :], [ob.b], [db_])
                    if xch:
                        xch.wrote(hf * 768 + 256 + h * 128, hf * 768 + 256 + (h + 1) * 128, 512, db_)

        pend = stage_a(0)
        for ch in range(NCH):
            nxt_a = stage_a(ch + 1) if ch + 1 < NCH else None
            stage_b(ch, pend)
            pend = nxt_a
        if xch:
            xch.flush()
        cx.fw.barrier()
        cx.st = old

    with contextlib.ExitStack() as st:
        old, cx.st = cx.st, st
        pre_fox = list(pre_fox or [])
        fb = cx.sb([4, 1], F32, "f_b")
        cx.dma("sync", fb.t[:], fox_b[:, :], (), [fb.b])
        cx.n += 1
        SCR = nc.dram_tensor(f"f_scr{cx.n}", [4, 6, S], BF16).ap()
        bscr = Buf("scr")
        SEG = min(2048, Tc)
        with contextlib.ExitStack() as st2:
            cx.st = st2
            onesr = cx.sb([4, SEG], F32, "f_ones")
            cx.memset("gpsimd", onesr.t[:], 1.0, [onesr.b])
            fcp = cx.sbpool(2, [4, SEG], F32, "f_F")
            f2p = cx.sbpool(2, [4, SEG], F32, "f_F2")
            p3p = cx.sbpool(2, [4, 6, SEG], BF16, "f_P3")
            prev = None
            for t0 in range(0, S, SEG):
                hf, tl = loc(t0)
                Fc, F2, P3 = fcp.get(), f2p.get(), p3p.get()
                cx.dma("sync", Fc.t[:, :], FFM[hf, :, tl:tl + SEG], (), [Fc.b], dyn=dyn["ffm"])
                cx.ts("vector", Fc.t[:, :], Fc.t[:, :], fb.t[:, 0:1], ALU.add, [Fc.b, fb.b], [Fc.b])
                cx.act(Fc.t[:, :], Fc.t[:, :], AF.Exp, [Fc.b], [Fc.b], scale=-1.0)
                cx.act(Fc.t[:, :], Fc.t[:, :], AF.Ln, [Fc.b, c["ones_f"].b], [Fc.b], bias=c["ones_f"].t[0:4, 0:1])
                cx.ts("vector", Fc.t[:, :], Fc.t[:, :], -1.0, ALU.mult, [Fc.b], [Fc.b])
                if prev is None:
                    cx.fw.op("vector", lambda e, F2=F2, Fc=Fc: e.tensor_tensor_scan(
                        out=F2.t[:, :], data0=onesr.t[:, :], data1=Fc.t[:, :], initial=0.0, op0=ALU.mult, op1=ALU.add),
                        [onesr.b, Fc.b], [F2.b])
                else:
                    cx.fw.op("vector", lambda e, F2=F2, Fc=Fc, prev=prev: e.tensor_tensor_scan(
                        out=F2.t[:, :], data0=onesr.t[:, :], data1=Fc.t[:, :], initial=prev.t[:, SEG - 1:SEG],
                        op0=ALU.mult, op1=ALU.add), [onesr.b, Fc.b, prev.b], [F2.b])
                prev = F2
                cx.ts("vector", P3.t[:, 0, :], F2.t[:, :], 8.0, ALU.mult, [F2.b], [P3.b])
                cx.stt(Fc.t[:, :], F2.t[:, :], 8.0, P3.t[:, 0, :], ALU.mult, ALU.subtract, [F2.b, P3.b], [Fc.b])
                cx.copy("vector", P3.t[:, 1, :], Fc.t[:, :], [Fc.b], [P3.b])
                cx.tt("vector", Fc.t[:, :], Fc.t[:, :], P3.t[:, 1, :], ALU.subtract, [Fc.b, P3.b], [Fc.b])
                cx.copy("vector", P3.t[:, 2, :], Fc.t[:, :], [Fc.b], [P3.b])
                cx.ts("vector", P3.t[:, 3:6, :], P3.t[:, 0:3, :], -1.0, ALU.mult, [P3.b], [P3.b])
                cx.dma("sync", SCR[:, :, t0:t0 + SEG], P3.t[:, :, :], [P3.b], [bscr])
            cx.fw.barrier()
            cx.st = st
        MASK = []
        for r in range(4):
            m = cx.sb([128, 512], BF16, f"f_mask{r}")
            cx.memset("gpsimd", m.t[:], 0.0, [m.b])
            cx.fw.op("gpsimd", lambda e, m=m, r=r: e.affine_select(out=m.t[:], in_=m.t[:], compare_op=ALU.is_ge, fill=-30000.0,
                                                                    base=-128 * r, pattern=[[1, 512]], channel_multiplier=-1),
                     [m.b], [m.b])
            MASK.append(m)
        QA = cx.sbpool(2, [128, S], BF16, "f_qa")
        KA = cx.sbpool(2, [128, S], BF16, "f_ka")
        VA = cx.sbpool(2, [128, NCH, 128], BF16, "f_va")
        ptp = cx.sbpool(6, [128, 512], BF16, "f_pt")
        rp = cx.sbpool(2, [128, 512], F32, "f_r")
        r0p = cx.sbpool(2, [128, 512], F32, "f_r0")
        ost = cx.sbpool(3, [64, 512], BF16, "f_ost")
        srot = Rot(banks[0:5])
        orot = Rot(banks[5:8])
        def load_head(h):
            qa, ka, va = QA.get(), KA.get(), VA.get()
            cx.memset("gpsimd", qa.t[64:70, :], 1.0, [qa.b])
            cx.memset("gpsimd", ka.t[64:70, :], 1.0, [ka.b])
            cx.memset("gpsimd", va.t[:, :, :], 1.0, [va.b])
            for hf in range(2):
                cx.dma("sync", qa.t[0:64, hf * Tc:(hf + 1) * Tc], PFM[hf, 1024 + h * 64:1024 + (h + 1) * 64, :], (), [qa.b], dyn=dyn["pfm"])
                cx.dma("sync", ka.t[0:64, hf * Tc:(hf + 1) * Tc], PFM[hf, 1280 + h * 64:1280 + (h + 1) * 64, :], (), [ka.b], dyn=dyn["pfm"])
                cx.dma("sync", va.t[:, hf * (Tc // 128):(hf + 1) * (Tc // 128), 0:64],
                       VTOK[hf].rearrange("(n p) c -> p n c", p=128)[:, :, 256 + h * 64:256 + (h + 1) * 64], (), [va.b], dyn=dyn["vtok"])
            cx.dma("sync", qa.t[64:67, :], SCR[h, 0:3, :], [bscr], [qa.b])
            cx.dma("sync", ka.t[67:70, :], SCR[h, 3:6, :], [bscr], [ka.b])
            return qa, ka, va

        nxt = load_head(0)
        for h in range(4):
            qa, ka, va = nxt
            if h + 1 < 4:
                nxt = load_head(h + 1)
            for qb in range(S // 512):
                qs_ = slice(qb * 512, (qb + 1) * 512)
                nk = 4 * qb + 4
                if pre_fox:
                    pre_fox.pop(0)()
                po = orot.get()
                pend = []

                def issue_s(kt):
                    ps = srot.get()
                    diag = kt >= 4 * qb
                    cx.mm(ps.t[:, :], ka.t[0:70, kt * 128:(kt + 1) * 128], qa.t[0:70, qs_], True, not diag, [ka.b, qa.b], [ps.b])
                    if diag:
                        mk = MASK[kt - 4 * qb]
                        cx.mm(ps.t[:, :], c["id_b"].t[:, :], mk.t[:, :], False, True, [c["id_b"].b, mk.b], [ps.b])
                    return ps
                SKEW = 2
                for kt in range(min(SKEW, nk)):
                    pend.append(issue_s(kt))
                for kt in range(nk):
                    if kt + SKEW < nk:
                        pend.append(issue_s(kt + SKEW))
                    ps = pend.pop(0)
                    pt = ptp.get()
                    cx.act(pt.t[:, :], ps.t[:, :], AF.Exp, [ps.b], [pt.b], scale=0.125)
                    cx.mm(po.t[:, :], va.t[:, kt, :], pt.t[:, :], kt == 0, kt == nk - 1, [va.b, pt.b], [po.b])
                rr = rp.get()
                cx.fw.op("vector", lambda e, rr=rr, po=po: e.reciprocal(out=rr.t[64:128, :], in_=po.t[64:128, :]), [po.b], [rr.b])
                r0 = r0p.get()
                cx.copy("vector", r0.t[0:64, :], rr.t[64:128, :], [rr.b], [r0.b])
                os_ = ost.get()
                cx.tt("vector", os_.t[0:64, :], po.t[0:64, :], r0.t[0:64, :], ALU.mult, [po.b, r0.b], [os_.b])
                hf, tl = loc(qb * 512)
                db_ = Buf("moutrow")
                cx.dma("sync", MOUT[hf, 512 + h * 64:512 + (h + 1) * 64, tl:tl + 512], os_.t[0:64, :], [os_.b], [db_])
                if xch:
                    xch.wrote(hf * 768 + 512 + h * 64, hf * 768 + 512 + (h + 1) * 64, 512, db_)
        while pre_fox:
            pre_fox.pop(0)()
        cx.fw.barrier()
        cx.st = old
    cx.fw.wait_all("sync")


def build_phase_b(Tc):
    nc = bass.Bass("TRN2", target_bir_lowering=False)
    PFM = dram(nc, "pfm", [2, 1536, Tc], BF16, "ExternalInput")
    VTOK = dram(nc, "vtok", [2, Tc, 512], BF16, "ExternalInput")
    GTOK = dram(nc, "gtok", [2, 128, Tc // 128, 4], F32, "ExternalInput")
    FFM = dram(nc, "ffm", [2, 4, Tc], F32, "ExternalInput")
    pool_w = dram(nc, "pool_w", [128, 2, 128], F32, "ExternalInput")
    pool_scale = dram(nc, "pool_scale", [128, 2], F32, "ExternalInput")
    pool_coef = dram(nc, "pool_coef", [128, 2, 4], F32, "ExternalInput")
    pool_rc = dram(nc, "pool_rc", [128, 2, 4, 16], F32, "ExternalInput")
    conv_w = dram(nc, "conv_w", [128, 2, 2, 4], F32, "ExternalInput")
    ml_b = dram(nc, "ml_b", [128, 4], F32, "ExternalInput")
    ml_ng = dram(nc, "ml_ng", [128, 2], F32, "ExternalInput")
    fox_b = dram(nc, "fox_b", [4, 1], F32, "ExternalInput")
    MOUT = dram(nc, "mout", [2, 768, Tc], BF16, "ExternalOutput")
    cx = Cx(nc)
    c = make_consts(cx)
    add_eps(cx, c)
    psum = cx.psum_banks(7)
    emit_phase_b(cx, c, Tc, PFM, VTOK, GTOK, FFM, pool_w, pool_scale, pool_coef, pool_rc, conv_w, ml_b, ml_ng, fox_b,
                 MOUT, psum)
    cx.finish()
    return nc


POOL_WINDOWS = (2, 4, 8, 16)


def lay128(v):
    v = np.asarray(v, np.float32)
    return np.ascontiguousarray(v.reshape(-1, 128).T)


def small_b_inputs(g, pool_w_grp, pool_scale, ml_conv_w, ml_b_i, ml_b_f, ml_norm_g, fox_b_f):
    d = {}
    d["pool_w"] = np.ascontiguousarray(np.stack([pool_w_grp[2 * g + gi] for gi in range(2)], axis=1)).astype(np.float32)
    d["pool_scale"] = lay128(pool_scale[g * 256:(g + 1) * 256])
    coef = np.zeros((128, 2, 4), np.float32)
    rc = np.zeros((128, 2, 4, 16), np.float32)
    for gi in range(2):
        w = POOL_WINDOWS[2 * g + gi]
        k = POOL_WINDOWS.index(w)
        coef[:, gi, k] = 1.0 / w
        rc[:, gi, k, :] = 1.0 / np.minimum(np.arange(16) + 1, w)
    d["pool_coef"] = coef
    d["pool_rc"] = rc
    cw = np.zeros((128, 2, 2, 4), np.float32)
    for a in range(2):
        for h in range(2):
            c0 = a * 512 + (2 * g + h) * 128
            cw[:, a, h, :] = ml_conv_w[:, c0:c0 + 128].T
    d["conv_w"] = cw
    mb = np.array([ml_b_i[2 * g], ml_b_i[2 * g + 1], ml_b_f[2 * g], ml_b_f[2 * g + 1]], np.float32)
    d["ml_b"] = np.ascontiguousarray(np.broadcast_to(mb, (128, 4)))
    d["ml_ng"] = lay128(ml_norm_g[g * 256:(g + 1) * 256])
    d["fox_b"] = np.asarray(fox_b_f[4 * g:4 * g + 4], np.float32).reshape(4, 1).copy()
    return d


GROUPS = [[0, 1], [2, 3], [4, 5], [6, 7]]
B_SMALL = (("pool_w", [128, 2, 128]), ("pool_scale", [128, 2]), ("pool_coef", [128, 2, 4]), ("pool_rc", [128, 2, 4, 16]),
           ("conv_w", [128, 2, 2, 4]), ("ml_b", [128, 4]), ("ml_ng", [128, 2]), ("fox_b", [4, 1]))


CC_MAX_BYTES = 2 * 1024 * 1024


class Xch:
    def __init__(self, cx, name, src2d, elem_bytes):
        self.cx = cx
        self.src = src2d
        rows, cols = src2d.shape
        self.rows, self.cols = rows, cols
        half = rows // 2
        R = 1
        for r_ in range(1, half + 1):
            if half % r_ == 0 and r_ * cols * elem_bytes <= CC_MAX_BYTES:
                R = r_
        self.R = R
        self.nch = rows // R
        self.GA = cx.nc.dram_tensor(name, [self.nch, 2, R, cols], src2d.dtype).ap()
        self.area = [0] * self.nch
        self.bufs = [[] for _ in range(self.nch)]
        self.issued = [False] * self.nch

    def wrote(self, row0, row1, ncols, buf):
        R = self.R
        for k in range(row0 // R, (row1 - 1) // R + 1):
            lo, hi = max(row0, k * R), min(row1, (k + 1) * R)
            self.area[k] += (hi - lo) * ncols
            self.bufs[k].append(buf)

    def flush(self):
        R = self.R
        for k in range(self.nch):
            if not self.issued[k] and self.area[k] >= R * self.cols:
                assert self.area[k] == R * self.cols, (k, self.area[k])
                self.issued[k] = True
                src, GA = self.src, self.GA
                self.cx.fw.cc_op(lambda e, k=k: e.collective_compute(
                    "AllGather", ALU.bypass, replica_groups=GROUPS, ins=[src[k * R:(k + 1) * R, :]],
                    outs=[GA[k].rearrange("r i c -> (r i) c")]), self.bufs[k])

    def select(self, dst3):
        assert all(self.issued), self.issued
        hk = self.nch // 2
        mult = hk * 2 * self.R * self.cols
        for r_ in range(2):
            self.cx.dma("sync", dst3[r_].rearrange("(kk i) c -> kk i c", i=self.R), self.GA[0:hk, r_], (), (), dyn=mult)


def build_fused(Tc, depth=2):
    nc = bass.Bass("TRN2", target_bir_lowering=False)
    I = lambda name, shape, dt=F32: dram(nc, name, shape, dt, "ExternalInput")
    xT = I("xT", [D, Tc])
    mix_g = I("mix_g", [depth, 128, 8])
    w_in = I("w_in", [depth, D, IN_W])
    bsm = [{k: I(f"{k}{l}", shp) for k, shp in B_SMALL} for l in range(depth)]
    w_br = I("w_br", [depth, 3, 512, D])
    w_out = I("w_out", [depth, D, D])
    ffn_g = I("ffn_g", [depth, 128, 8])
    ff_g = I("ff_w_gate", [(depth + 1) // 2, D, D_FF])
    ff_u = I("ff_w_up", [(depth + 1) // 2, D, D_FF])
    ff_d = I("ff_w_down", [(depth + 1) // 2, D_FF, D])
    m_r = I("moe_w_router", [depth // 2, D, NEXP])
    m_b = I("moe_b_router", [depth // 2, NEXP, 1])
    m_g = I("moe_w_gate", [depth // 2, NEXP, D, D_FFE])
    m_u = I("moe_w_up", [depth // 2, NEXP, D, D_FFE])
    m_d = I("moe_w_down", [depth // 2, NEXP, D_FFE, D])
    final_g = I("final_g", [128, 8])
    outT = dram(nc, "outT", [D, Tc], F32, "ExternalOutput")
    N = lambda name, shape, dt: nc.dram_tensor(name, list(shape), dt).ap()
    PFM = N("s_pfm", [2, 1536, Tc], BF16)
    GATES = N("s_gates", [3072, Tc], BF16)
    VTOK = N("s_vtok", [2, Tc, 512], BF16)
    GTOK = N("s_gtok", [2, 128, Tc // 128, 4], F32)
    FFM = N("s_ffm", [2, 4, Tc], F32)
    MOUT = N("s_mout", [2, 768, Tc], BF16)
    XN = N("s_xn", [D, Tc], F32)
    PFMS = N("s_pfms", [2, 1536, Tc], BF16)
    VTOKS = N("s_vtoks", [2, Tc, 512], BF16)
    GTOKS = N("s_gtoks", [2, 128, Tc // 128, 4], F32)
    FFMS = N("s_ffms", [2, 4, Tc], F32)
    MINS = N("s_mins", [2, 768, Tc], BF16)
    cx = Cx(nc)
    c = make_consts(cx)
    add_eps(cx, c)
    psum = cx.psum_banks(7)
    xcur = xT
    NM = depth // 2
    W16 = [(N(f"w16g{i}", [NEXP, D, D_FFE], BF16), N(f"w16u{i}", [NEXP, D, D_FFE], BF16), N(f"w16d{i}", [NEXP, D_FFE, D], BF16))
           for i in range(NM)]
    b16 = Buf("w16")

    pc_jobs = []
    for i in range(NM):
        pc_jobs += precast_jobs(cx, [(W16[i][0], m_g[i]), (W16[i][1], m_u[i]), (W16[i][2], m_d[i])], b16)
    for l in range(depth):
        xa = {"pfm": Xch(cx, f"ga_pfm{l}", PFM.rearrange("a b c -> (a b) c"), 2),
              "vtok": Xch(cx, f"ga_vtok{l}", VTOK.rearrange("a b c -> (a b) c"), 2),
              "gtok": Xch(cx, f"ga_gtok{l}", GTOK.rearrange("a p n c -> (a p) (n c)"), 4),
              "ffm": Xch(cx, f"ga_ffm{l}", FFM.rearrange("a h t -> (a h) t"), 4)}
        def early_select(xa=xa):
            for x_ in xa.values():
                x_.flush()
            cx.fw.cc_wait(only="sync")
            xa["pfm"].select(PFMS)
            xa["vtok"].select(VTOKS)
            xa["gtok"].select(GTOKS.rearrange("r p n c -> r p (n c)"))
            xa["ffm"].select(FFMS)
        emit_phase_a(cx, c, Tc, xcur, mix_g[l], w_in[l], PFM, GATES, VTOK, GTOK, FFM, psum, xch=xa, mid_gates=early_select)
        cx.fw.cc_wait()
        cx.fw.barrier()
        b = bsm[l]
        xm = Xch(cx, f"ga_mout{l}", MOUT.rearrange("a b c -> (a b) c"), 2)
        emit_phase_b(cx, c, Tc, PFMS, VTOKS, GTOKS, FFMS, b["pool_w"], b["pool_scale"], b["pool_coef"],
                     b["pool_rc"], b["conv_w"], b["ml_b"], b["ml_ng"], b["fox_b"], MOUT, psum, xch=xm,
                     pre_fox=pc_jobs if l == 0 else None)
        xm.flush()
        cx.fw.cc_wait()
        xm.select(MINS)
        cx.fw.barrier()
        moe = (l % 2 == 1)
        final = (l == depth - 1)
        if moe:
            ffw = (m_r[l // 2], m_b[l // 2]) + W16[l // 2]
        else:
            ffw = (ff_g[l // 2], ff_u[l // 2], ff_d[l // 2])
        dst = outT if final else XN
        emit_phase_c(cx, c, Tc, xcur, MINS, GATES, w_br[l], w_out[l], ffn_g[l], ffw, dst, psum, moe=moe,
                     final_g=final_g if final else None, wq="sync" if moe else "gpsimd", wbuf16=b16 if moe else None)
        xcur = XN
    cx.finish()
    return nc


_PROGS = {}


def kernel(x, mix_norm_g, w_in, pool_w_grp, pool_scale, ml_conv_w, ml_b_i, ml_b_f, ml_norm_g, fox_b_f,
           w_br_pool, w_br_ml, w_br_fox, w_out, ffn_norm_g, ff_w_gate, ff_w_up, ff_w_down,
           moe_w_router, moe_b_router, moe_w_gate, moe_w_up, moe_w_down, final_norm_g):
    f32 = lambda a: np.ascontiguousarray(np.asarray(a, dtype=np.float32))
    x = f32(x)
    Tc = SEQ // 2
    depth = int(np.asarray(w_in).shape[0])
    if "fused" not in _PROGS:
        _PROGS["fused"] = build_fused(Tc, depth)
    nc = _PROGS["fused"]
    shared = {
        "mix_g": np.stack([lay128(mix_norm_g[l]) for l in range(depth)]),
        "w_in": f32(w_in),
        "w_br": np.ascontiguousarray(np.stack([np.stack([f32(w_br_pool[l]), f32(w_br_ml[l]), f32(w_br_fox[l])]) for l in range(depth)])),
        "w_out": f32(w_out),
        "ffn_g": np.stack([lay128(ffn_norm_g[l]) for l in range(depth)]),
        "ff_w_gate": f32(ff_w_gate), "ff_w_up": f32(ff_w_up), "ff_w_down": f32(ff_w_down),
        "moe_w_router": f32(moe_w_router), "moe_b_router": f32(moe_b_router).reshape(-1, NEXP, 1),
        "moe_w_gate": f32(moe_w_gate), "moe_w_up": f32(moe_w_up), "moe_w_down": f32(moe_w_down),
        "final_g": lay128(final_norm_g),
    }
    smalls = [[small_b_inputs(g, f32(pool_w_grp[l]), f32(pool_scale[l]), f32(ml_conv_w[l]), f32(ml_b_i[l]), f32(ml_b_f[l]),
                              f32(ml_norm_g[l]), f32(fox_b_f[l])) for g in range(2)] for l in range(depth)]
    in_maps = []
    for c in range(NCORES):
        d = dict(shared)
        d["xT"] = np.ascontiguousarray(x[c // 2, (c % 2) * Tc:(c % 2 + 1) * Tc, :].T)
        for l in range(depth):
            for k, v in smalls[l][c % 2].items():
                d[f"{k}{l}"] = v
        in_maps.append(d)
    res = run_bass_kernel_spmd(nc, in_maps, core_ids=list(range(NCORES)))
    out = np.empty((BATCH, SEQ, D), np.float32)
    for c in range(NCORES):
        out[c // 2, (c % 2) * Tc:(c % 2 + 1) * Tc, :] = np.asarray(res.results[c]["outT"]).T
    return out
```

```python
import contextlib
import os
import numpy as np
import concourse.bass as bass
import concourse.mybir as mybir
from concourse.bass_utils import run_bass_kernel_spmd

F32 = mybir.dt.float32
BF16 = mybir.dt.bfloat16
AF = mybir.ActivationFunctionType
ALU = mybir.AluOpType
AX = mybir.AxisListType

D = 1024
SEQ = 8192
BATCH = 4
NCORES = 8
IN_W = 7184
D_FF = 2816
D_FFE = 3584
NEXP = 8
EPS = 1e-6


ALL_BUFS = []


class Buf:
    __slots__ = ("name", "w", "r")

    def __init__(self, name=""):
        self.name = name
        self.w = None
        self.r = {}
        ALL_BUFS.append(self)


class Lane:
    def __init__(self, name, sem, step):
        self.name = name
        self.sem = sem
        self.step = step
        self.count = 0
        self.seen = {}
        self.snaps = {}


SYNC_SAME = {"tensor": False, "vector": True, "scalar": True, "gpsimd": True, "sync": False}


class FW:
    def __init__(self, nc, n_dma_lanes=24):
        self.nc = nc
        self.ops = {k: [] for k in ("tensor", "vector", "scalar", "gpsimd", "sync")}
        self.eng = {}
        for k in self.ops:
            self.eng[k] = Lane(k, nc.alloc_semaphore(name=f"prog_{k}"), 1)
        self.dma_lanes = [Lane(f"dma{i}", nc.alloc_semaphore(name=f"dma_{i}"), 16)
                          for i in range(n_dma_lanes)]
        self.dma_rr = 0
        self.cc = Lane("cc", nc.alloc_semaphore(name="cc_sem"), 1)

    def cc_op(self, fn, reads=()):
        E = self.eng["gpsimd"]
        self._emit_waits(E, self._needs(reads, ()))
        self.cc.count += 1
        self.ops["gpsimd"].append(("cc", fn, self.cc.sem))

    def cc_wait(self, only=None):
        for E in self.eng.values():
            if only is None or E.name == only:
                self._emit_waits(E, {self.cc: self.cc.count})

    def _needs(self, reads, writes):
        needs = {}
        for b in reads:
            if b.w is not None:
                l, i = b.w
                if needs.get(l, 0) < i:
                    needs[l] = i
        for b in writes:
            if b.w is not None:
                l, i = b.w
                if needs.get(l, 0) < i:
                    needs[l] = i
            for l, i in b.r.items():
                if needs.get(l, 0) < i:
                    needs[l] = i
        return needs

    def _emit_waits(self, E, needs):
        for l, i in needs.items():
            if l is E and not SYNC_SAME[E.name]:
                continue
            if E.seen.get(l, 0) >= i:
                continue
            self.ops[E.name].append(("wait", l.sem, i * l.step))
            E.seen[l] = i
            snap = l.snaps.get(i)
            if snap:
                for l2, i2 in snap.items():
                    if E.seen.get(l2, 0) < i2:
                        E.seen[l2] = i2

    def op(self, engine, fn, reads=(), writes=()):
        E = self.eng[engine]
        self._emit_waits(E, self._needs(reads, writes))
        E.count += 1
        idx = E.count
        E.snaps[idx] = dict(E.seen)
        self.ops[engine].append(("op", fn, E.sem))
        for b in reads:
            b.r[E] = idx
        for b in writes:
            b.w = (E, idx)
            b.r = {}

    def dma(self, queue, out, in_, reads=(), writes=(), dyn=0, **kw):
        E = self.eng[queue]
        L = self.dma_lanes[self.dma_rr]
        self.dma_rr = (self.dma_rr + 1) % len(self.dma_lanes)
        needs = self._needs(reads, writes)
        if L.count > 0:
            needs[L] = max(needs.get(L, 0), L.count)
        self._emit_waits(E, needs)
        L.count += 1
        idx = L.count
        L.snaps[idx] = dict(E.seen)
        if dyn:
            self.ops[queue].append(("dyndma", out, in_, dyn, L.sem))
        else:
            self.ops[queue].append(("dma", out, in_, kw, L.sem))
        for b in reads:
            b.r[L] = idx
        for b in writes:
            b.w = (L, idx)
            b.r = {}

    def _lanes(self):
        return list(self.eng.values()) + self.dma_lanes + [self.cc]

    def begin_if(self, flag_ap):
        self.barrier()
        self._snap = ({l: l.count for l in self._lanes()}, {l: dict(l.seen) for l in self._lanes()},
                      [(b, b.w, dict(b.r)) for b in ALL_BUFS], self.dma_rr)
        for name in self.ops:
            self.ops[name].append(("if", flag_ap))

    def _restore(self):
        counts, seens, bufs, rr = self._snap
        for l, c_ in counts.items():
            l.count = c_
            l.seen = dict(seens[l])
        for b, w, r in bufs:
            b.w = w
            b.r = dict(r)
        self.dma_rr = rr

    def begin_else(self):
        self._endA = {l: l.count for l in self._lanes()}
        self._padA = {name: [] for name in self.ops}
        for name in self.ops:
            self.ops[name].append(("else", self._padA[name]))
        self._restore()

    def end_if(self):
        endB = {l: l.count for l in self._lanes()}
        padB = {name: [] for name in self.ops}
        for l in self._lanes():
            fin = max(self._endA[l], endB[l])
            owner = l.name if l.name in self.ops else "sync"
            for end, pads in ((self._endA[l], self._padA), (endB[l], padB)):
                if fin > end:
                    pads[owner].append((l.sem, end * l.step, (fin - end) * l.step))
            l.count = fin
        for name in self.ops:
            self.ops[name].append(("endif", padB[name]))
        counts, seens, bufs, rr = self._snap
        for l in self._lanes():
            l.seen = dict(seens[l])
        self.barrier()

    def barrier(self):
        lanes = list(self.eng.values()) + self.dma_lanes
        counts = {l: l.count for l in lanes if l.count > 0}
        for E in self.eng.values():
            self._emit_waits(E, {l: i for l, i in counts.items() if l is not E})

    def wait_all(self, engine):
        E = self.eng[engine]
        needs = {}
        for l in list(self.eng.values()) + self.dma_lanes:
            if l.count > 0 and l is not E:
                needs[l] = l.count
        self._emit_waits(E, needs)

    def emit(self):
        with self.nc.Block() as block:
            for name in self.ops:
                ops = self.ops[name]
                if not ops:
                    continue

                def body(eng, ops=ops):
                    me = None
                    stack = []

                    def pad(pads):
                        for sem, cur, amt in pads:
                            eng.wait_ge(sem, cur)
                            eng.sem_inc(sem, amt)
                    for o in ops:
                        if o[0] == "if":
                            reg = eng.alloc_register()
                            eng.reg_load(reg, o[1])
                            cm = eng.If_eq(reg, 1)
                            cm.__enter__()
                            stack.append(cm)
                        elif o[0] == "else":
                            pad(o[1])
                            stack.pop().__exit__(None, None, None)
                            cm = eng.Else()
                            cm.__enter__()
                            stack.append(cm)
                        elif o[0] == "endif":
                            pad(o[1])
                            stack.pop().__exit__(None, None, None)
                        elif o[0] == "wait":
                            eng.wait_ge(o[1], o[2])
                        elif o[0] == "op":
                            o[1](eng).then_inc(o[2], 1)
                        elif o[0] == "cc":
                            o[1](eng).then_inc(o[2], 1)
                        elif o[0] == "dyndma":
                            _, out, in_, mult, sem = o
                            if me is None:
                                me = eng.partition_id() % 2
                            off = me * mult
                            eng.dma_start(out=out, in_=bass.AP(in_.tensor, in_.offset + off, in_.ap)).then_inc(sem, 16)
                        else:
                            _, out, in_, kw, sem = o
                            eng.dma_start(out=out, in_=in_, **kw).then_inc(sem, 16)

                getattr(block, name)(body)


class T:
    __slots__ = ("t", "b")

    def __init__(self, t, b):
        self.t = t
        self.b = b


class Rot:
    def __init__(self, items):
        self.items = items
        self.i = 0

    def get(self):
        x = self.items[self.i]
        self.i = (self.i + 1) % len(self.items)
        return x


class Cx:
    def __init__(self, nc):
        self.nc = nc
        self.fw = FW(nc)
        self.st = contextlib.ExitStack()
        self.n = 0

    def sb(self, shape, dtype, name=None):
        self.n += 1
        nm = f"{name or 'sb'}_{self.n}"
        t = self.st.enter_context(self.nc.sbuf_tensor(nm, list(shape), dtype))
        return T(t, Buf(nm))

    def sbpool(self, n, shape, dtype, name=None):
        return Rot([self.sb(shape, dtype, (name or "p") + f"_{self.n}_{i}") for i in range(n)])

    def psum_banks(self, n=8, dtype=F32):
        out = []
        for i in range(n):
            self.n += 1
            cols = 512 if dtype == F32 else 1024
            t = self.st.enter_context(self.nc.psum_tensor(f"ps{self.n}", [128, cols], dtype))
            out.append(T(t, Buf(f"ps{self.n}")))
        return Rot(out)

    def mm(self, out, lhsT, rhs, start, stop, reads, writes):
        self.fw.op("tensor", lambda e: e.matmul(out, lhsT=lhsT, rhs=rhs, start=start, stop=stop),
                   reads, writes)

    def transpose(self, out, in_, ident, reads, writes):
        self.fw.op("tensor", lambda e: e.transpose(out, in_, ident), reads, writes)

    def act(self, out, in_, func, reads, writes, bias=None, scale=None):
        kw = {}
        if bias is not None:
            kw["bias"] = bias
        if scale is not None:
            kw["scale"] = scale
        self.fw.op("scalar", lambda e: e.activation(out=out, in_=in_, func=func, **kw), reads, writes)

    def tt(self, eng, out, in0, in1, op, reads, writes):
        self.fw.op(eng, lambda e: e.tensor_tensor(out=out, in0=in0, in1=in1, op=op), reads, writes)

    def ts(self, eng, out, in0, s1, op0, reads, writes, s2=None, op1=None):
        if op1 is None:
            self.fw.op(eng, lambda e: e.tensor_scalar(out=out, in0=in0, scalar1=s1, scalar2=None, op0=op0),
                       reads, writes)
        else:
            self.fw.op(eng, lambda e: e.tensor_scalar(out=out, in0=in0, scalar1=s1, scalar2=s2, op0=op0, op1=op1),
                       reads, writes)

    def stt(self, out, in0, scalar, in1, op0, op1, reads, writes):
        self.fw.op("vector", lambda e: e.scalar_tensor_tensor(out=out, in0=in0, scalar=scalar, in1=in1,
                                                              op0=op0, op1=op1), reads, writes)

    def copy(self, eng, out, in_, reads, writes):
        if eng == "scalar":
            self.fw.op("scalar", lambda e: e.activation(out=out, in_=in_, func=AF.Copy), reads, writes)
        else:
            self.fw.op(eng, lambda e: e.tensor_copy(out=out, in_=in_), reads, writes)

    def memset(self, eng, ap, val, writes):
        self.fw.op(eng, lambda e: e.memset(ap, val), (), writes)

    def dma(self, q, out, in_, reads=(), writes=(), dyn=0, **kw):
        self.fw.dma(q, out, in_, reads, writes, dyn=dyn, **kw)

    def finish(self):
        self.fw.wait_all("sync")
        self.fw.emit()
        self.st.close()


def dram(nc, name, shape, dtype, kind):
    return nc.dram_tensor(name, list(shape), dtype, kind=kind).ap()


def make_consts(cx):
    c = {}
    c["ones_f"] = cx.sb([128, 128], F32, "ones_f")
    c["ones_b"] = cx.sb([128, 128], BF16, "ones_b")
    c["id_f"] = cx.sb([128, 128], F32, "id_f")
    c["id_b"] = cx.sb([128, 128], BF16, "id_b")
    cx.memset("gpsimd", c["ones_f"].t[:], 1.0, [c["ones_f"].b])
    cx.memset("gpsimd", c["ones_b"].t[:], 1.0, [c["ones_b"].b])
    cx.memset("gpsimd", c["id_f"].t[:], 0.0, [c["id_f"].b])
    idf = c["id_f"]
    cx.fw.op("gpsimd", lambda e: e.affine_select(out=idf.t[:], in_=idf.t[:], compare_op=ALU.not_equal,
                                                 fill=1.0, base=0, pattern=[[-1, 128]], channel_multiplier=1),
             [idf.b], [idf.b])
    cx.copy("gpsimd", c["id_b"].t[:], idf.t[:], [idf.b], [c["id_b"].b])
    return c


def rmsnorm_stats(cx, c, xt, xb, ncols, psum, sqpool, tmp_pool, nfeat_chunks=8, nfeat=1024.0):
    ps = psum.get()
    for kc in range(nfeat_chunks):
        sq = sqpool.get()
        cx.tt("gpsimd", sq.t[:, :ncols], xt(kc), xt(kc), ALU.mult, [xb], [sq.b])
        cx.mm(ps.t[:, :ncols], c["ones_f"].t[:, :], sq.t[:, :ncols], kc == 0, kc == nfeat_chunks - 1,
              [sq.b, c["ones_f"].b], [ps.b])
    ln = tmp_pool.get()
    cx.act(ln.t[:, :ncols], ps.t[:, :ncols], AF.Ln, [ps.b], [ln.b], bias=c["eps"].t[:, 0:1], scale=1.0 / nfeat)
    rstd = tmp_pool.get()
    cx.act(rstd.t[:, :ncols], ln.t[:, :ncols], AF.Exp, [ln.b], [rstd.b], scale=-0.5)
    return rstd


def add_eps(cx, c):
    c["eps"] = cx.sb([128, 1], F32, "eps")
    cx.memset("gpsimd", c["eps"].t[:], EPS, [c["eps"].b])


FM_BASES = [0, 512, 1024, 2048, 2568, 3080]


def emit_phase_a(cx, c, Tc, xT, g_in, w_in, PFM, GATES, VTOK, GTOK, FFM, psum, xch=None, mid_gates=None):
    NB = Tc // 512
    xTv = xT.rearrange("(kc p) t -> p kc t", p=128)
    wv = w_in.rearrange("(kc p) n -> p kc n", p=128)
    with contextlib.ExitStack() as st:
        old, cx.st = cx.st, st
        gt = cx.sb([128, 8], F32, "a_g")
        cx.dma("sync", gt.t[:], g_in[:, :], (), [gt.b])
        hT = cx.sb([128, 8, Tc], BF16, "a_hT")
        bh = [Buf(f"hT{b}") for b in range(NB)]
        xpool = cx.sbpool(2, [128, 8, 512], F32, "a_x")
        sqpool = cx.sbpool(3, [128, 512], F32, "a_sq")
        tmp = cx.sbpool(4, [128, 512], F32, "a_tmp")
        wpool = cx.sbpool(3, [128, 8, 512], BF16, "a_w")
        stpool = cx.sbpool(3, [128, Tc], BF16, "a_st")
        for blk in range(NB):
            x = xpool.get()
            cx.dma("sync", x.t[:], xTv[:, :, blk * 512:(blk + 1) * 512], (), [x.b])
            rstd = rmsnorm_stats(cx, c, lambda kc: x.t[:, kc, :], x.b, 512, psum, sqpool, tmp)
            for kc in range(8):
                cx.stt(hT.t[:, kc, blk * 512:(blk + 1) * 512], x.t[:, kc, :], gt.t[:, kc:kc + 1], rstd.t[:, :],
                       ALU.mult, ALU.mult, [x.b, gt.b, rstd.b], [bh[blk]])
        jobs = []
        for g in range(2):
            for j, base in enumerate(FM_BASES):
                jobs.append((base + g * 256, 256, "pfm", (g, j * 256)))
        gate_jobs = [(4112 + n * 512, 512, "gate", n * 512) for n in range(6)]
        ev = 0

        def flush_all():
            if xch:
                for x_ in xch.values():
                    x_.flush()

        def run_jobs(jobs):
          nonlocal ev
          for (c0, ncols, kind, dest) in jobs:
            w = wpool.get()
            cx.dma("gpsimd", w.t[:, :, :ncols], wv[:, :, c0:c0 + ncols], (), [w.b])
            flush_all()
            for sub in range(ncols // 128):
                stg = stpool.get()
                for blk in range(NB):
                    ps = psum.get()
                    for kc in range(8):
                        cx.mm(ps.t[:, :], w.t[:, kc, sub * 128:(sub + 1) * 128], hT.t[:, kc, blk * 512:(blk + 1) * 512],
                              kc == 0, kc == 7, [w.b, bh[blk]], [ps.b])
                    o = stg.t[:, blk * 512:(blk + 1) * 512]
                    if kind == "gate":
                        cx.act(o, ps.t[:, :], AF.Sigmoid, [ps.b], [stg.b])
                    else:
                        cx.copy("vector" if ev % 2 == 0 else "scalar", o, ps.t[:, :], [ps.b], [stg.b])
                        ev += 1
                if kind == "gate":
                    dst = GATES[dest + sub * 128:dest + (sub + 1) * 128, :]
                    cx.dma("sync", dst, stg.t[:, :], [stg.b], ())
                else:
                    dst = PFM[dest[0], dest[1] + sub * 128:dest[1] + (sub + 1) * 128, :]
                    db_ = Buf("pfmrow")
                    cx.dma("sync", dst, stg.t[:, :], [stg.b], [db_])
                    if xch:
                        r0_ = dest[0] * 1536 + dest[1] + sub * 128
                        xch["pfm"].wrote(r0_, r0_ + 128, Tc, db_)
        run_jobs(jobs)
        vst = cx.sbpool(2, [128, 4, 512], BF16, "a_vst")
        for g in range(2):
            w = wpool.get()
            cx.dma("gpsimd", w.t[:, :, 0:256], wv[:, :, 1536 + g * 256:1536 + (g + 1) * 256], (), [w.b])
            cx.dma("gpsimd", w.t[:, :, 256:512], wv[:, :, 3592 + g * 256:3592 + (g + 1) * 256], (), [w.b])
            vv = VTOK[g].rearrange("(n p) c -> p n c", p=128)
            for t4 in range(Tc // 512):
                stg = vst.get()
                for ti in range(4):
                    tt_ = t4 * 4 + ti
                    ps = psum.get()
                    for kc in range(8):
                        cx.mm(ps.t[:, :], hT.t[:, kc, tt_ * 128:(tt_ + 1) * 128], w.t[:, kc, :], kc == 0, kc == 7,
                              [w.b, bh[tt_ // 4]], [ps.b])
                    cx.copy("vector" if ti % 2 == 0 else "scalar", stg.t[:, ti, :], ps.t[:, :], [ps.b], [stg.b])
                db_ = Buf("vtokrow")
                cx.dma("sync", vv[:, t4 * 4:(t4 + 1) * 4, :], stg.t[:, :, :], [stg.b], [db_])
                if xch:
                    xch["vtok"].wrote(g * Tc + t4 * 512, g * Tc + (t4 + 1) * 512, 512, db_)
                    flush_all()
        w8 = cx.sb([128, 8, 16], BF16, "a_w8")
        for g in range(2):
            cx.dma("gpsimd", w8.t[:, :, g * 4:g * 4 + 2], wv[:, :, 2560 + 2 * g:2560 + 2 * g + 2], (), [w8.b])
            cx.dma("gpsimd", w8.t[:, :, g * 4 + 2:g * 4 + 4], wv[:, :, 2564 + 2 * g:2564 + 2 * g + 2], (), [w8.b])
        cx.dma("gpsimd", w8.t[:, :, 8:16], wv[:, :, 4104:4112], (), [w8.b])
        NT = Tc // 128
        gacc = cx.sb([128, NT, 8], F32, "a_gacc")
        ps = psum.get()
        for tt_ in range(NT):
            for kc in range(8):
                cx.mm(ps.t[:, tt_ * 8:(tt_ + 1) * 8], hT.t[:, kc, tt_ * 128:(tt_ + 1) * 128], w8.t[:, kc, 0:8],
                      kc == 0, kc == 7, [w8.b, bh[tt_ // 4]], [ps.b])
        cx.copy("vector", gacc.t[:, :, :], ps.t[:, 0:NT * 8].rearrange("p (n c) -> p n c", c=8), [ps.b], [gacc.b])
        for g in range(2):
            db_ = Buf("gtokrow")
            cx.dma("sync", GTOK[g], gacc.t[:, :, g * 4:(g + 1) * 4], [gacc.b], [db_])
            if xch:
                xch["gtok"].wrote(g * 128, (g + 1) * 128, (Tc // 128) * 4, db_)
        fst = cx.sb([8, Tc], F32, "a_fst")
        for blk in range(NB):
            ps = psum.get()
            for kc in range(8):
                cx.mm(ps.t[0:8, :], w8.t[:, kc, 8:16], hT.t[:, kc, blk * 512:(blk + 1) * 512], kc == 0, kc == 7,
                      [w8.b, bh[blk]], [ps.b])
            cx.copy("vector", fst.t[0:8, blk * 512:(blk + 1) * 512], ps.t[0:8, :], [ps.b], [fst.b])
        db_ = Buf("ffmrow")
        cx.dma("sync", FFM.rearrange("g h t -> (g h) t"), fst.t[:, :], [fst.b], [db_])
        if xch:
            xch["ffm"].wrote(0, 8, Tc, db_)
        flush_all()
        run_jobs(gate_jobs[:3])
        if mid_gates is not None:
            mid_gates()
        run_jobs(gate_jobs[3:])
        cx.fw.barrier()
        cx.st = old


def build_phase_a(Tc):
    nc = bass.Bass("TRN2", target_bir_lowering=False)
    xT = dram(nc, "xT", [D, Tc], F32, "ExternalInput")
    g_in = dram(nc, "norm_g", [128, 8], F32, "ExternalInput")
    w_in = dram(nc, "w_in", [D, IN_W], F32, "ExternalInput")
    PFM = dram(nc, "pfm", [2, 1536, Tc], BF16, "ExternalOutput")
    GATES = dram(nc, "gates", [3072, Tc], BF16, "ExternalOutput")
    VTOK = dram(nc, "vtok", [2, Tc, 512], BF16, "ExternalOutput")
    GTOK = dram(nc, "gtok", [2, 128, Tc // 128, 4], F32, "ExternalOutput")
    FFM = dram(nc, "ffm", [2, 4, Tc], F32, "ExternalOutput")
    cx = Cx(nc)
    c = make_consts(cx)
    add_eps(cx, c)
    psum = cx.psum_banks(8)
    emit_phase_a(cx, c, Tc, xT, g_in, w_in, PFM, GATES, VTOK, GTOK, FFM, psum)
    cx.finish()
    return nc


TS = 1024


DBG = None


def emit_phase_c(cx, c, Tc, xT, MIN, GATES, w_br, w_out, ffn_g, ffw, outT, psum, moe=False, final_g=None, dyn_min=0,
                 wq="gpsimd", wbuf16=None):
    wrd = [wbuf16] if wbuf16 is not None else []
    F = D_FFE if moe else D_FF
    NFC = F // 128
    NSB = Tc // TS
    NBK = TS // 512
    xTv = xT.rearrange("(kc p) t -> p kc t", p=128)
    oTv = outT.rearrange("(kc p) t -> p kc t", p=128)
    gav = GATES.rearrange("(br oc p) t -> p br oc t", br=3, oc=8, p=128)
    with contextlib.ExitStack() as st:
        old, cx.st = cx.st, st
        gn = cx.sb([128, 8], F32, "c_gn")
        cx.dma("sync", gn.t[:], ffn_g[:, :], (), [gn.b])
        if final_g is not None:
            gf = cx.sb([128, 8], F32, "c_gf")
            cx.dma("sync", gf.t[:], final_g[:, :], (), [gf.b])
        xt = cx.sb([128, 8, TS], F32, "c_x")
        h2 = cx.sb([128, 8, TS], BF16, "c_h2")
        R = cx.sb([128, 28, TS], BF16, "c_R")
        bR = [Buf("R_mt"), Buf("R_mg"), Buf("R_rest")]

        def rbuf(ch):
            return bR[0] if ch < 12 else (bR[1] if ch < 20 else bR[2])
        W = cx.sb([128, 3 * 8192], BF16, "c_W")
        bW = [Buf("W0"), Buf("W1"), Buf("W2")]
        wbr = W.t[:, 0:12288].rearrange("p (q c) -> p q c", c=1024)
        wout = W.t[:, 12288:20480].rearrange("p (q c) -> p q c", c=1024)
        wrot = Rot([(W.t[:, i * 8192:(i + 1) * 8192], bW[i]) for i in range(3)])
        gpool = cx.sbpool(2, [128, 3, 512], BF16, "c_gt")
        sqpool = cx.sbpool(3, [128, 512], F32, "c_sq")
        tmp = cx.sbpool(5, [128, 512], F32, "c_tmp")
        if moe:
            w_router, b_router = ffw[0], ffw[1]
            wr = cx.sb([128, 8, 8], F32, "c_wr")
            cx.dma("sync", wr.t[:], w_router.rearrange("(kc p) e -> p kc e", p=128), (), [wr.b])
            for kc in range(8):
                cx.ts("vector", wr.t[:, kc, :], wr.t[:, kc, :], gn.t[:, kc:kc + 1], ALU.mult, [wr.b, gn.b], [wr.b])
            br_ = cx.sb([8, 1], F32, "c_br")
            cx.dma("sync", br_.t[:], b_router[:, :], (), [br_.b])
            SEL = cx.sb([8, 8, 128], F32, "c_sel")
            cx.memset("gpsimd", SEL.t[:], 0.0, [SEL.b])
            cx.fw.op("gpsimd", lambda e: e.affine_select(out=SEL.t[:], in_=SEL.t[:], compare_op=ALU.not_equal,
                                                         fill=1.0, base=0, pattern=[[-1, 8], [0, 128]],
                                                         channel_multiplier=1), [SEL.b], [SEL.b])
            LT = cx.sb([8, TS], F32, "c_LT")
            GT = cx.sb([8, TS], F32, "c_GT")
            gbpool = cx.sbpool(2, [128, TS], F32, "c_gb")
            small = cx.sbpool(12, [128, 8], F32, "c_small")
            CAP = int(os.environ.get("MOE_CAP", "384"))
            NST = CAP // 128
            CUM = cx.sb([8, TS], F32, "c_cum")
            posb = cx.sb([128, TS], F32, "c_posb")
            PM = cx.sb([128, 8, 16], F32, "c_pm")
            IOTA1 = cx.sb([128, CAP], F32, "c_iota")
            SLOT = cx.sb([128, NST], F32, "c_slot")
            cx.fw.op("gpsimd", lambda e: e.iota(IOTA1.t[:, :], pattern=[[1, CAP]], base=1, channel_multiplier=0,
                                                allow_small_or_imprecise_dtypes=True), (), [IOTA1.b])
            cx.fw.op("gpsimd", lambda e: e.iota(SLOT.t[:, :], pattern=[[128, NST]], base=1, channel_multiplier=1,
                                                allow_small_or_imprecise_dtypes=True), (), [SLOT.b])
            flg = cx.sb([1, 8], F32, "c_flg")
            flgi = cx.sb([1, 1], mybir.dt.int32, "c_flgi")
            cx.n += 1
            pTb = cx.st.enter_context(cx.nc.psum_tensor(f"c_psT{cx.n}", [128, 1024], BF16))
            bpT = Buf("c_pT")
            Rf = R.t[:, :, :].rearrange("p a b -> p (a b)")
            ACTE = Rf[:, 0:28 * CAP].rearrange("p (a b) -> p a b", b=CAP)
            H2T = Rf[:, 10752:10752 + 8192].rearrange("p (a b) -> p a b", b=1024)
            SELt = Rf[:, 18944:18944 + 8 * CAP].rearrange("p (a b) -> p a b", b=CAP)
            SELT = Rf[:, 22016:22016 + NST * 1024].rearrange("p (a b) -> p a b", b=1024)
            Xf = h2.t[:, :, :].rearrange("p a b -> p (a b)")
            H2E = Xf[:, 0:8 * CAP].rearrange("p (a b) -> p a b", b=CAP)
            YE = Xf[:, 3072:3072 + NST * 1024].rearrange("p (a b) -> p a b", b=1024)
        for sb in range(NSB):
            t0 = sb * TS
            cx.dma("sync", xt.t[:], xTv[:, :, t0:t0 + TS], (), [xt.b])
            for g_ in range(2):
                cx.dma("sync", R.t[:, g_ * 6:(g_ + 1) * 6, :], MIN[g_].rearrange("(j p) t -> p j t", p=128)[:, :, t0:t0 + TS],
                       (), [bR[0]], dyn=dyn_min)
            for br in range(3):
                cx.dma("gpsimd", wbr[:, br * 4:(br + 1) * 4, :], w_br[br].rearrange("(n p) c -> p n c", p=128), (),
                       [bW[0], bW[1]])
            cx.dma("gpsimd", wout, w_out.rearrange("(kc p) c -> p kc c", p=128), (), [bW[1], bW[2]])
            for oc in range(8):
                for blk in range(NBK):
                    bs = slice(blk * 512, (blk + 1) * 512)
                    gt = gpool.get()
                    cx.dma("sync", gt.t[:], gav[:, :, oc, t0 + blk * 512:t0 + (blk + 1) * 512], (), [gt.b])
                    pp = []
                    for br in range(3):
                        ps = psum.get()
                        for n in range(4):
                            ch = (n // 2) * 6 + 2 * br + (n % 2)
                            cx.mm(ps.t[:, :], wbr[:, br * 4 + n, oc * 128:(oc + 1) * 128], R.t[:, ch, bs],
                                  n == 0, n == 3, [bW[0], bW[1], bR[0]], [ps.b])
                        pp.append(ps)
                    t1, t2, t3 = tmp.get(), tmp.get(), tmp.get()
                    cx.tt("vector", t1.t[:, :], pp[0].t[:, :], gt.t[:, 0, :], ALU.mult, [pp[0].b, gt.b], [t1.b])
                    cx.tt("vector", t2.t[:, :], pp[1].t[:, :], gt.t[:, 1, :], ALU.mult, [pp[1].b, gt.b], [t2.b])
                    cx.tt("vector", t3.t[:, :], pp[2].t[:, :], gt.t[:, 2, :], ALU.mult, [pp[2].b, gt.b], [t3.b])
                    cx.tt("gpsimd", t1.t[:, :], t1.t[:, :], t2.t[:, :], ALU.add, [t1.b, t2.b], [t1.b])
                    cx.tt("gpsimd", R.t[:, 12 + oc, bs], t1.t[:, :], t3.t[:, :], ALU.add, [t1.b, t3.b], [bR[1]])
            for oc in range(8):
                for blk in range(NBK):
                    bs = slice(blk * 512, (blk + 1) * 512)
                    ps = psum.get()
                    for kc in range(8):
                        cx.mm(ps.t[:, :], wout[:, kc, oc * 128:(oc + 1) * 128], R.t[:, 12 + kc, bs], kc == 0, kc == 7,
                              [bW[1], bW[2], bR[1]], [ps.b])
                    cx.tt("vector", xt.t[:, oc, bs], xt.t[:, oc, bs], ps.t[:, :], ALU.add, [ps.b, xt.b], [xt.b])
            for blk in range(NBK):
                bs = slice(blk * 512, (blk + 1) * 512)
                rstd = rmsnorm_stats(cx, c, lambda kc: xt.t[:, kc, bs], xt.b, 512, psum, sqpool, tmp)
                for kc in range(8):
                    cx.stt(h2.t[:, kc, bs], xt.t[:, kc, bs], gn.t[:, kc:kc + 1], rstd.t[:, :], ALU.mult, ALU.mult,
                           [xt.b, gn.b, rstd.b], [h2.b])
                if moe:
                    ps = psum.get()
                    for kc in range(8):
                        cx.mm(ps.t[0:8, :], wr.t[:, kc, :], xt.t[:, kc, bs], kc == 0, kc == 7, [wr.b, xt.b], [ps.b])
                    cx.tt("vector", LT.t[0:8, bs], ps.t[0:8, :], rstd.t[0:8, :], ALU.mult, [ps.b, rstd.b], [LT.b])
                    cx.ts("vector", LT.t[0:8, bs], LT.t[0:8, bs], br_.t[0:8, 0:1], ALU.add, [LT.b, br_.b], [LT.b])
                    pg = psum.get()
                    for ti in range(4):
                        cs = slice(blk * 512 + ti * 128, blk * 512 + (ti + 1) * 128)
                        pl = psum.get()
                        cx.transpose(pl.t[:, 0:8], LT.t[0:8, cs], c["id_f"].t[0:8, 0:8], [LT.b, c["id_f"].b], [pl.b])
                        lg, m1, eq, lg2, m2, sel, nm1, ex, w_, den, G = [small.get() for _ in range(11)]
                        cx.copy("vector", lg.t[:, :], pl.t[:, 0:8], [pl.b], [lg.b])
                        cx.fw.op("vector", lambda e, m1=m1, lg=lg: e.tensor_reduce(out=m1.t[:, 0:1], in_=lg.t[:, :], axis=AX.X, op=ALU.max), [lg.b], [m1.b])
                        cx.ts("vector", eq.t[:, :], lg.t[:, :], m1.t[:, 0:1], ALU.is_equal, [lg.b, m1.b], [eq.b])
                        cx.stt(lg2.t[:, :], eq.t[:, :], -1e30, lg.t[:, :], ALU.mult, ALU.add, [eq.b, lg.b], [lg2.b])
                        cx.fw.op("vector", lambda e, m2=m2, lg2=lg2: e.tensor_reduce(out=m2.t[:, 0:1], in_=lg2.t[:, :], axis=AX.X, op=ALU.max), [lg2.b], [m2.b])
                        cx.ts("vector", sel.t[:, :], lg.t[:, :], m2.t[:, 0:1], ALU.is_ge, [lg.b, m2.b], [sel.b])
                        cx.ts("vector", nm1.t[:, 0:1], m1.t[:, 0:1], -1.0, ALU.mult, [m1.b], [nm1.b])
                        cx.act(ex.t[:, :], lg.t[:, :], AF.Exp, [lg.b, nm1.b], [ex.b], bias=nm1.t[:, 0:1])
                        cx.tt("vector", w_.t[:, :], ex.t[:, :], sel.t[:, :], ALU.mult, [ex.b, sel.b], [w_.b])
                        cx.fw.op("vector", lambda e, den=den, w_=w_: e.tensor_reduce(out=den.t[:, 0:1], in_=w_.t[:, :], axis=AX.X, op=ALU.add), [w_.b], [den.b])
                        cx.fw.op("vector", lambda e, den=den: e.reciprocal(out=den.t[:, 0:1], in_=den.t[:, 0:1]), [den.b], [den.b])
                        cx.ts("vector", G.t[:, :], w_.t[:, :], den.t[:, 0:1], ALU.mult, [w_.b, den.b], [G.b])
                        cx.transpose(pg.t[0:8, ti * 128:(ti + 1) * 128], G.t[:, 0:8], c["id_f"].t[:, :], [G.b, c["id_f"].b], [pg.b])
                    cx.copy("vector", GT.t[0:8, bs], pg.t[0:8, :], [pg.b], [GT.b])
                if moe and DBG is not None:
                    cx.dma("sync", DBG[0:8, t0:t0 + TS], GT.t[0:8, :], [GT.b], ())
                    cx.dma("sync", DBG[8:16, t0:t0 + TS], LT.t[0:8, :], [LT.b], ())
            def dense_ffn():
                for e_ in range(NEXP if moe else 1):
                    if moe:
                        wg_v = ffw[2][e_].rearrange("(kc p) n -> p kc n", p=128)
                        wu_v = ffw[3][e_].rearrange("(kc p) n -> p kc n", p=128)
                        wd_v = ffw[4][e_].rearrange("(kc p) c -> p kc c", p=128)
                        gb = gbpool.get()
                        for blk in range(NBK):
                            bs = slice(blk * 512, (blk + 1) * 512)
                            ps = psum.get()
                            cx.mm(ps.t[:, :], SEL.t[0:8, e_, :], GT.t[0:8, bs], True, True, [SEL.b, GT.b], [ps.b])
                            cx.copy("vector", gb.t[:, bs], ps.t[:, :], [ps.b], [gb.b])
                    else:
                        wg_v = ffw[0].rearrange("(kc p) n -> p kc n", p=128)
                        wu_v = ffw[1].rearrange("(kc p) n -> p kc n", p=128)
                        wd_v = ffw[2].rearrange("(kc p) c -> p kc c", p=128)
                    for s0 in range(0, F, 512):
                        ncol = min(512, F - s0)
                        wt, wb = wrot.get()
                        wgt = wt[:, 0:4096].rearrange("p (k c) -> p k c", c=512)
                        wut = wt[:, 4096:8192].rearrange("p (k c) -> p k c", c=512)
                        cx.dma(wq, wgt[:, :, :ncol], wg_v[:, :, s0:s0 + ncol], wrd, [wb])
                        cx.dma(wq, wut[:, :, :ncol], wu_v[:, :, s0:s0 + ncol], wrd, [wb])
                        for sub in range(ncol // 128):
                            ch = s0 // 128 + sub
                            for blk in range(NBK):
                                bs = slice(blk * 512, (blk + 1) * 512)
                                pg_, pu_ = psum.get(), psum.get()
                                for kc in range(8):
                                    cx.mm(pg_.t[:, :], wgt[:, kc, sub * 128:(sub + 1) * 128], h2.t[:, kc, bs], kc == 0, kc == 7,
                                          [wb, h2.b], [pg_.b])
                                for kc in range(8):
                                    cx.mm(pu_.t[:, :], wut[:, kc, sub * 128:(sub + 1) * 128], h2.t[:, kc, bs], kc == 0, kc == 7,
                                          [wb, h2.b], [pu_.b])
                                sg = tmp.get()
                                cx.act(sg.t[:, :], pg_.t[:, :], AF.Silu, [pg_.b], [sg.b])
                                cx.tt("vector", R.t[:, ch, bs], pu_.t[:, :], sg.t[:, :], ALU.mult, [pu_.b, sg.b], [rbuf(ch)])
                    KGS = 7 if NFC % 7 == 0 else 11
                    NG = NFC // KGS
                    for blk in range(NBK):
                        bs = slice(blk * 512, (blk + 1) * 512)
                        for och in range(2):
                            accs = [psum.get() for _ in range(4)]
                            for kg in range(NG):
                                wt, wb = wrot.get()
                                wdt = wt[:, 0:KGS * 512].rearrange("p (k c) -> p k c", c=512)
                                cx.dma(wq, wdt, wd_v[:, kg * KGS:(kg + 1) * KGS, och * 512:(och + 1) * 512], wrd, [wb])
                                for o4 in range(4):
                                    for k in range(KGS):
                                        kc = kg * KGS + k
                                        cx.mm(accs[o4].t[:, :], wdt[:, k, o4 * 128:(o4 + 1) * 128], R.t[:, kc, bs], kc == 0,
                                              kc == NFC - 1, [wb, rbuf(kc)], [accs[o4].b])
                            for o4 in range(4):
                                oc = och * 4 + o4
                                ps = accs[o4]
                                if moe:
                                    tm = tmp.get()
                                    cx.tt("vector", tm.t[:, :], ps.t[:, :], gb.t[:, bs], ALU.mult, [ps.b, gb.b], [tm.b])
                                    cx.tt("vector", xt.t[:, oc, bs], xt.t[:, oc, bs], tm.t[:, :], ALU.add, [tm.b, xt.b], [xt.b])
                                else:
                                    cx.tt("vector", xt.t[:, oc, bs], xt.t[:, oc, bs], ps.t[:, :], ALU.add, [ps.b, xt.b], [xt.b])

            def routed_ffn():
                bh2t, bpm, bsel, bselt, bh2e, bacte, bye = [Buf(n_) for n_ in "h2t pm sel selt h2e acte ye".split()]
                for tile in range(8):
                    for kc in range(8):
                        cx.transpose(pTb[:, kc * 128:(kc + 1) * 128], h2.t[:, kc, tile * 128:(tile + 1) * 128], c["id_b"].t[:, :],
                                     [h2.b, c["id_b"].b], [bpT])
                    cx.copy("vector" if tile % 2 == 0 else "scalar", H2T[:, tile, :], pTb[:, :], [bpT], [bh2t])
                for tile in range(8):
                    ps = psum.get()
                    cx.transpose(ps.t[:, 0:8], CUM.t[0:8, tile * 128:(tile + 1) * 128], c["id_f"].t[0:8, 0:8], [CUM.b, c["id_f"].b], [ps.b])
                    cx.transpose(ps.t[:, 8:16], LT.t[0:8, tile * 128:(tile + 1) * 128], c["id_f"].t[0:8, 0:8], [LT.b, c["id_f"].b], [ps.b])
                    cx.copy("vector", PM.t[:, tile, :], ps.t[:, 0:16], [ps.b], [PM.b])
                cx.fw.barrier()
                for e_ in range(NEXP):
                    wg_v = ffw[2][e_].rearrange("(kc p) n -> p kc n", p=128)
                    wu_v = ffw[3][e_].rearrange("(kc p) n -> p kc n", p=128)
                    wd_v = ffw[4][e_].rearrange("(kc p) c -> p kc c", p=128)
                    gb = gbpool.get()
                    for blk in range(NBK):
                        bs = slice(blk * 512, (blk + 1) * 512)
                        ps = psum.get()
                        cx.mm(ps.t[:, :], SEL.t[0:8, e_, :], GT.t[0:8, bs], True, True, [SEL.b, GT.b], [ps.b])
                        cx.copy("scalar", gb.t[:, bs], ps.t[:, :], [ps.b], [gb.b])
                        ps = psum.get()
                        cx.mm(ps.t[:, :], SEL.t[0:8, e_, :], CUM.t[0:8, bs], True, True, [SEL.b, CUM.b], [ps.b])
                        cx.copy("scalar", posb.t[:, bs], ps.t[:, :], [ps.b], [posb.b])
                    for tile in range(8):
                        cx.ts("vector", SELt[:, tile, :], IOTA1.t[:, :], PM.t[:, tile, e_:e_ + 1], ALU.is_equal,
                              [IOTA1.b, PM.b], [bsel], s2=PM.t[:, tile, 8 + e_:9 + e_], op1=ALU.mult)
                    for st_ in range(NST):
                        cx.stt(SELT[:, st_, :], posb.t[:, :], SLOT.t[:, st_:st_ + 1], gb.t[:, :], ALU.is_equal, ALU.mult,
                               [posb.b, SLOT.b, gb.b], [bselt])
                    for kc in range(8):
                        ps = psum.get()
                        for tile in range(8):
                            cx.mm(ps.t[:, 0:CAP], H2T[:, tile, kc * 128:(kc + 1) * 128], SELt[:, tile, :], tile == 0, tile == 7,
                                  [bh2t, bsel], [ps.b])
                        cx.copy("vector" if kc % 2 == 0 else "scalar", H2E[:, kc, :], ps.t[:, 0:CAP], [ps.b], [bh2e])
                    for s0 in range(0, F, 512):
                        wt, wb = wrot.get()
                        wgt = wt[:, 0:4096].rearrange("p (k c) -> p k c", c=512)
                        wut = wt[:, 4096:8192].rearrange("p (k c) -> p k c", c=512)
                        cx.dma(wq, wgt, wg_v[:, :, s0:s0 + 512], wrd, [wb])
                        cx.dma(wq, wut, wu_v[:, :, s0:s0 + 512], wrd, [wb])
                        for sub in range(4):
                            ch = s0 // 128 + sub
                            pg_, pu_ = psum.get(), psum.get()
                            for kc in range(8):
                                cx.mm(pg_.t[:, 0:CAP], wgt[:, kc, sub * 128:(sub + 1) * 128], H2E[:, kc, :], kc == 0, kc == 7,
                                      [wb, bh2e], [pg_.b])
                            for kc in range(8):
                                cx.mm(pu_.t[:, 0:CAP], wut[:, kc, sub * 128:(sub + 1) * 128], H2E[:, kc, :], kc == 0, kc == 7,
                                      [wb, bh2e], [pu_.b])
                            sg = tmp.get()
                            cx.act(sg.t[:, 0:CAP], pg_.t[:, 0:CAP], AF.Silu, [pg_.b], [sg.b])
                            cx.tt("vector", ACTE[:, ch, :], pu_.t[:, 0:CAP], sg.t[:, 0:CAP], ALU.mult, [pu_.b, sg.b], [bacte])
                    for fh in range(2):
                        accs = [psum.get() for _ in range(NST)]
                        for kg in range(4):
                            wt, wb = wrot.get()
                            wdt = wt[:, 0:7 * 512].rearrange("p (k c) -> p k c", c=512)
                            cx.dma(wq, wdt, wd_v[:, kg * 7:(kg + 1) * 7, fh * 512:(fh + 1) * 512], wrd, [wb])
                            for st_ in range(NST):
                                for k in range(7):
                                    kc = kg * 7 + k
                                    cx.mm(accs[st_].t[:, :], ACTE[:, kc, st_ * 128:(st_ + 1) * 128], wdt[:, k, :], kc == 0, kc == 27,
                                          [wb, bacte], [accs[st_].b])
                        for st_ in range(NST):
                            cx.copy("scalar" if st_ % 2 == 0 else "vector", YE[:, st_, fh * 512:(fh + 1) * 512], accs[st_].t[:, :],
                                    [accs[st_].b], [bye])
                    for fc in range(8):
                        for blk in range(NBK):
                            bs = slice(blk * 512, (blk + 1) * 512)
                            ps = psum.get()
                            for st_ in range(NST):
                                cx.mm(ps.t[:, :], YE[:, st_, fc * 128:(fc + 1) * 128], SELT[:, st_, bs], st_ == 0, st_ == NST - 1,
                                      [bye, bselt], [ps.b])
                            cx.tt("vector", xt.t[:, fc, bs], xt.t[:, fc, bs], ps.t[:, :], ALU.add, [ps.b, xt.b], [xt.b])

            if moe and os.environ.get("MOE_DENSE") is None:
                cx.ts("vector", LT.t[0:8, :], GT.t[0:8, :], 0.0, ALU.is_gt, [GT.b], [LT.b])
                cx.fw.op("vector", lambda e: e.tensor_tensor_scan(out=CUM.t[0:8, :], data0=LT.t[0:8, :], data1=LT.t[0:8, :],
                                                                  initial=0.0, op0=ALU.add, op1=ALU.max), [LT.b], [CUM.b])
                ps = psum.get()
                cx.transpose(ps.t[0:1, 0:8], CUM.t[0:8, TS - 1:TS], c["id_f"].t[0:8, 0:8], [CUM.b, c["id_f"].b], [ps.b])
                cx.copy("vector", flg.t[0:1, 0:8], ps.t[0:1, 0:8], [ps.b], [flg.b])
                cx.fw.op("vector", lambda e: e.tensor_reduce(out=flg.t[0:1, 0:1], in_=flg.t[0:1, 0:8], axis=AX.X, op=ALU.max),
                         [flg.b], [flg.b])
                cx.ts("vector", flg.t[0:1, 0:1], flg.t[0:1, 0:1], float(CAP) + 0.5, ALU.is_lt, [flg.b], [flg.b])
                cx.copy("vector", flgi.t[0:1, 0:1], flg.t[0:1, 0:1], [flg.b], [flgi.b])
                cx.fw.begin_if(flgi.t[0:1, 0:1])
                routed_ffn()
                cx.fw.begin_else()
                dense_ffn()
                cx.fw.end_if()
            else:
                dense_ffn()
            if final_g is not None:
                for blk in range(NBK):
                    bs = slice(blk * 512, (blk + 1) * 512)
                    rstd = rmsnorm_stats(cx, c, lambda kc: xt.t[:, kc, bs], xt.b, 512, psum, sqpool, tmp)
                    for kc in range(8):
                        cx.stt(xt.t[:, kc, bs], xt.t[:, kc, bs], gf.t[:, kc:kc + 1], rstd.t[:, :], ALU.mult, ALU.mult,
                               [xt.b, gf.b, rstd.b], [xt.b])
            cx.dma("sync", oTv[:, :, t0:t0 + TS], xt.t[:], [xt.b], ())
        cx.fw.barrier()
        cx.st = old


def precast_jobs(cx, pairs, buf):
    jobs = []
    for dst, src in pairs:
        E_, R_, C_ = src.shape
        for e in range(E_):
            for h in range(2):
                jobs.append(lambda dst=dst, src=src, e=e, h=h, R_=R_: cx.dma(
                    "gpsimd", dst[e, h * (R_ // 2):(h + 1) * (R_ // 2), :], src[e, h * (R_ // 2):(h + 1) * (R_ // 2), :], (), [buf]))
    return jobs


def emit_precast(cx, pairs, buf):
    for j in precast_jobs(cx, pairs, buf):
        j()


def build_phase_c(Tc, moe, final):
    nc = bass.Bass("TRN2", target_bir_lowering=False)
    F = D_FFE if moe else D_FF
    xT = dram(nc, "xT", [D, Tc], F32, "ExternalInput")
    MIN = dram(nc, "min", [2, 768, Tc], BF16, "ExternalInput")
    GATES = dram(nc, "gates", [3072, Tc], BF16, "ExternalInput")
    w_br = dram(nc, "w_br", [3, 512, D], F32, "ExternalInput")
    w_out = dram(nc, "w_out", [D, D], F32, "ExternalInput")
    ffn_g = dram(nc, "ffn_g", [128, 8], F32, "ExternalInput")
    if moe:
        ffw = (dram(nc, "w_router", [D, NEXP], F32, "ExternalInput"),
               dram(nc, "b_router", [NEXP, 1], F32, "ExternalInput"),
               dram(nc, "w_gate", [NEXP, D, F], F32, "ExternalInput"),
               dram(nc, "w_up", [NEXP, D, F], F32, "ExternalInput"),
               dram(nc, "w_down", [NEXP, F, D], F32, "ExternalInput"))
    else:
        ffw = (dram(nc, "w_gate", [D, F], F32, "ExternalInput"),
               dram(nc, "w_up", [D, F], F32, "ExternalInput"),
               dram(nc, "w_down", [F, D], F32, "ExternalInput"))
    final_g = dram(nc, "final_g", [128, 8], F32, "ExternalInput") if final else None
    outT = dram(nc, "outT", [D, Tc], F32, "ExternalOutput")
    cx = Cx(nc)
    c = make_consts(cx)
    add_eps(cx, c)
    psum = cx.psum_banks(7)
    if moe:
        N = lambda name, shape: nc.dram_tensor(name, list(shape), BF16).ap()
        g16, u16, d16 = N("w16g", [NEXP, D, F]), N("w16u", [NEXP, D, F]), N("w16d", [NEXP, F, D])
        b16 = Buf("w16")
        emit_precast(cx, [(g16, ffw[2]), (u16, ffw[3]), (d16, ffw[4])], b16)
        ffw = (ffw[0], ffw[1], g16, u16, d16)
        emit_phase_c(cx, c, Tc, xT, MIN, GATES, w_br, w_out, ffn_g, ffw, outT, psum, moe=moe, final_g=final_g, wq="sync", wbuf16=b16)
    else:
        emit_phase_c(cx, c, Tc, xT, MIN, GATES, w_br, w_out, ffn_g, ffw, outT, psum, moe=moe, final_g=final_g)
    cx.finish()
    return nc


def emit_phase_b(cx, c, Tc, PFM, VTOK, GTOK, FFM, pool_w, pool_scale, pool_coef, pool_rc, conv_w, ml_b, ml_ng,
                 fox_b, MOUT, psum, dyn=None, xch=None, pre_fox=None):
    dyn = dyn or {"pfm": 0, "vtok": 0, "gtok": 0, "ffm": 0}
    S = 2 * Tc
    NCH = S // 128
    nc = cx.nc
    banks = psum.items

    def loc(t):
        return t // Tc, t % Tc

    with contextlib.ExitStack() as st:
        old, cx.st = cx.st, st
        PB = min(1024, Tc)
        L = PB + 16
        pw_f = cx.sb([128, 2, 128], F32, "p_wf")
        pw = cx.sb([128, 2, 128], BF16, "p_w")
        cx.dma("gpsimd", pw.t[:], pool_w[:, :, :], (), [pw.b])
        psc = cx.sb([128, 2], F32, "p_sc")
        cx.dma("sync", psc.t[:], pool_scale[:, :], (), [psc.b])
        pco = cx.sb([128, 2, 4], F32, "p_co")
        cx.dma("sync", pco.t[:], pool_coef[:, :, :], (), [pco.b])
        prc = cx.sb([128, 2, 4, 16], F32, "p_rc")
        cx.dma("sync", prc.t[:], pool_rc[:, :, :, :], (), [prc.b])
        upool = cx.sbpool(3, [128, L], BF16, "p_u")
        wtsets = Rot([[cx.sb([128, L], F32, f"p_w{j}_{i}") for i in range(4)] for j in range(3)])
        dpool = cx.sbpool(3, [128, L], F32, "p_d")
        dbp = cx.sbpool(3, [128, PB], BF16, "p_db")
        opool = cx.sbpool(3, [128, PB], BF16, "p_o")
        t16 = cx.sbpool(4, [128, 16], F32, "p_t16")
        for gi in range(2):
            for t0 in range(0, S, PB):
                hf, tl = loc(t0)
                U = upool.get()
                wt = wtsets.get()
                cx.dma("sync", U.t[:, 16:L], PFM[hf, gi * 128:(gi + 1) * 128, tl:tl + PB], (), [U.b], dyn=dyn["pfm"])
                if t0 == 0:
                    cx.memset("gpsimd", U.t[:, 0:16], 0.0, [U.b])
                else:
                    hp, tp = loc(t0 - 16)
                    cx.dma("sync", U.t[:, 0:16], PFM[hp, gi * 128:(gi + 1) * 128, tp:tp + 16], (), [U.b], dyn=dyn["pfm"])
                src = U
                for k, sh in enumerate((1, 2, 4, 8)):
                    lo = 2 * sh - 1
                    cx.tt("gpsimd", wt[k].t[:, lo:L], src.t[:, lo:L], src.t[:, lo - sh:L - sh],
                          ALU.add, [src.b], [wt[k].b])
                    src = wt[k]
                d = dpool.get()
                cx.stt(d.t[:, 16:L], wt[0].t[:, 16:L], pco.t[:, gi, 0:1], U.t[:, 16:L], ALU.mult, ALU.subtract,
                       [wt[0].b, pco.b, U.b], [d.b])
                for k in range(1, 4):
                    cx.stt(d.t[:, 16:L], wt[k].t[:, 16:L], pco.t[:, gi, k:k + 1], d.t[:, 16:L], ALU.mult, ALU.add,
                           [wt[k].b, pco.b, d.b], [d.b])
                if t0 == 0:
                    a0 = t16.get()
                    cx.tt("vector", a0.t[:, :], wt[0].t[:, 16:32], prc.t[:, gi, 0, :], ALU.mult, [wt[0].b, prc.b], [a0.b])
                    for k in range(1, 4):
                        a1 = t16.get()
                        cx.tt("vector", a1.t[:, :], wt[k].t[:, 16:32], prc.t[:, gi, k, :], ALU.mult, [wt[k].b, prc.b], [a1.b])
                        cx.tt("vector", a0.t[:, :], a0.t[:, :], a1.t[:, :], ALU.add, [a0.b, a1.b], [a0.b])
                    cx.tt("vector", d.t[:, 16:32], a0.t[:, :], U.t[:, 16:32], ALU.subtract, [a0.b, U.b], [d.b])
                db = dbp.get()
                cx.copy("scalar", db.t[:, :], d.t[:, 16:L], [d.b], [db.b])
                o = opool.get()
                for blk in range(PB // 512):
                    ps = psum.get()
                    cx.mm(ps.t[:, :], pw.t[:, gi, :], db.t[:, blk * 512:(blk + 1) * 512], True, True, [pw.b, db.b], [ps.b])
                    cx.act(o.t[:, blk * 512:(blk + 1) * 512], ps.t[:, :], AF.Copy, [ps.b, psc.b], [o.b],
                           scale=psc.t[:, gi:gi + 1])
                db_ = Buf("moutrow")
                cx.dma("scalar", MOUT[hf, gi * 128:(gi + 1) * 128, tl:tl + PB], o.t[:, :], [o.b], [db_])
                if xch:
                    xch.wrote(hf * 768 + gi * 128, hf * 768 + (gi + 1) * 128, PB, db_)
        if xch:
            xch.flush()
        cx.fw.barrier()
        cx.st = old

    with contextlib.ExitStack() as st:
        old, cx.st = cx.st, st
        TRI = cx.sb([128, 128], F32, "m_tri")
        cx.memset("gpsimd", TRI.t[:], 1.0, [TRI.b])
        cx.fw.op("gpsimd", lambda e: e.affine_select(out=TRI.t[:], in_=TRI.t[:], compare_op=ALU.is_ge, fill=0.0, base=0,
                                                     pattern=[[1, 128]], channel_multiplier=-1), [TRI.b], [TRI.b])
        NTRI = cx.sb([128, 128], F32, "m_ntri")
        cx.memset("gpsimd", NTRI.t[:], 0.0, [NTRI.b])
        cx.fw.op("gpsimd", lambda e: e.affine_select(out=NTRI.t[:], in_=NTRI.t[:], compare_op=ALU.is_ge, fill=-30000.0, base=0,
                                                     pattern=[[1, 128]], channel_multiplier=-1), [NTRI.b], [NTRI.b])
        cw = cx.sb([128, 2, 2, 4], F32, "m_cw")
        cx.dma("sync", cw.t[:], conv_w[:, :, :, :], (), [cw.b])
        mlb = cx.sb([128, 4], F32, "m_b")
        cx.dma("sync", mlb.t[:], ml_b[:, :], (), [mlb.b])
        mng = cx.sb([128, 2], F32, "m_ng")
        cx.dma("sync", mng.t[:], ml_ng[:, :], (), [mng.b])
        QK = [[cx.sb([128, S], BF16, f"m_qk{a}{h}") for h in range(2)] for a in range(2)]
        V = cx.sb([128, NCH, 256], BF16, "m_v")
        for hf in range(2):
            cx.dma("sync", V.t[:, hf * (Tc // 128):(hf + 1) * (Tc // 128), :],
                   VTOK[hf].rearrange("(n p) c -> p n c", p=128)[:, :, 0:256], (), [V.b], dyn=dyn["vtok"])
        GT = cx.sb([128, NCH, 4], F32, "m_gt")
        for hf in range(2):
            cx.dma("sync", GT.t[:, hf * (Tc // 128):(hf + 1) * (Tc // 128), :], GTOK[hf], (), [GT.b], dyn=dyn["gtok"])
        for col in range(4):
            cx.ts("vector", GT.t[:, :, col:col + 1], GT.t[:, :, col:col + 1], mlb.t[:, col:col + 1], ALU.add,
                  [GT.b, mlb.b], [GT.b])
        LF = cx.sb([128, NCH, 2], F32, "m_lf")
        cx.act(LF.t[:, :, :], GT.t[:, :, 2:4], AF.Exp, [GT.b], [LF.b], scale=-1.0)
        cx.act(LF.t[:, :, :], LF.t[:, :, :], AF.Ln, [LF.b, c["ones_f"].b], [LF.b], bias=c["ones_f"].t[:, 0:1])
        cx.ts("vector", LF.t[:, :, :], LF.t[:, :, :], -1.0, ALU.mult, [LF.b], [LF.b])
        CB = min(2048, Tc)
        with contextlib.ExitStack() as st2:
            cx.st = st2
            rpool = cx.sbpool(2, [128, CB + 3], BF16, "m_raw")
            apool = cx.sbpool(2, [128, CB], F32, "m_acc")
            for a in range(2):
                for h in range(2):
                    for t0 in range(0, S, CB):
                        hf, tl = loc(t0)
                        r0 = 256 * (1 + a) + h * 128
                        raw = rpool.get()
                        cx.dma("sync", raw.t[:, 3:CB + 3], PFM[hf, r0:r0 + 128, tl:tl + CB], (), [raw.b], dyn=dyn["pfm"])
                        if t0 == 0:
                            cx.memset("gpsimd", raw.t[:, 0:3], 0.0, [raw.b])
                        else:
                            hp, tp = loc(t0 - 3)
                            cx.dma("sync", raw.t[:, 0:3], PFM[hp, r0:r0 + 128, tp:tp + 3], (), [raw.b], dyn=dyn["pfm"])
                        acc = apool.get()
                        cx.ts("vector", acc.t[:, :], raw.t[:, 3:CB + 3], cw.t[:, a, h, 3:4], ALU.mult, [raw.b, cw.b], [acc.b])
                        for tap in range(3):
                            cx.stt(acc.t[:, :], raw.t[:, tap:CB + tap], cw.t[:, a, h, tap:tap + 1], acc.t[:, :],
                                   ALU.mult, ALU.add, [raw.b, cw.b, acc.b], [acc.b])
                        if a == 0:
                            cx.act(QK[a][h].t[:, t0:t0 + CB], acc.t[:, :], AF.Silu, [acc.b], [QK[a][h].b])
                        else:
                            cx.act(acc.t[:, :], acc.t[:, :], AF.Silu, [acc.b], [acc.b])
                            cx.ts("gpsimd", QK[a][h].t[:, t0:t0 + CB], acc.t[:, :], 128.0 ** -0.5, ALU.mult, [acc.b],
                                  [QK[a][h].b])
            cx.fw.barrier()
            cx.st = st
        CT = [cx.sb([128, 256], F32, f"m_ct{h}") for h in range(2)]
        CTb = [cx.sb([128, 256], BF16, f"m_ctb{h}") for h in range(2)]
        for h in range(2):
            cx.memset("gpsimd", CT[h].t[:], 0.0, [CT[h].b])
            cx.memset("gpsimd", CTb[h].t[:], 0.0, [CTb[h].b])
        H = [cx.sbpool(2, [128, 512], F32, f"m_H{h}") for h in range(2)]
        sm = cx.sbpool(16, [128, 2], F32, "m_sm")
        f128 = cx.sbpool(18, [128, 128], F32, "m_f128")
        b128 = cx.sbpool(16, [128, 128], BF16, "m_b128")
        f512 = cx.sbpool(6, [128, 512], F32, "m_f512")
        sqp = cx.sbpool(2, [128, 512], F32, "m_sq")
        opl = cx.sbpool(3, [128, 512], BF16, "m_o")
        cx.n += 1
        pT = cx.st.enter_context(nc.psum_tensor(f"m_psT{cx.n}", [128, 1024], BF16))
        prot = Rot(banks[0:7])
        pTb = [Buf("pT0"), Buf("pT1")]
        Hcur = [None, None]

        def stage_a(ch):
            cs = slice(ch * 128, (ch + 1) * 128)
            pss = prot.get()
            cx.mm(pss.t[:, 0:2], TRI.t[:, :], LF.t[:, ch, :], True, True, [TRI.b, LF.b], [pss.b])
            cx.mm(pss.t[:, 2:4], c["ones_f"].t[:, :], LF.t[:, ch, :], True, True, [c["ones_f"].b, LF.b], [pss.b])
            a_ = sm.get()
            cx.tt("vector", a_.t[:, :], GT.t[:, ch, 0:2], pss.t[:, 0:2], ALU.subtract, [GT.b, pss.b], [a_.b])
            ab = sm.get()
            cx.tt("vector", ab.t[:, :], a_.t[:, :], pss.t[:, 2:4], ALU.add, [a_.b, pss.b], [ab.b])
            wk = sm.get()
            cx.act(wk.t[:, :], ab.t[:, :], AF.Exp, [ab.b], [wk.b])
            dec = sm.get()
            cx.act(dec.t[:, :], pss.t[:, 2:4], AF.Exp, [pss.b], [dec.b])
            out = {"dec": dec, "h": []}
            for h in range(2):
                qT, kT = QK[0][h], QK[1][h]
                lfb = f128.get()
                cx.ts("gpsimd", lfb.t[:, :], c["ones_f"].t[:, :], LF.t[:, ch, h:h + 1], ALU.mult, [c["ones_f"].b, LF.b], [lfb.b])
                pb = prot.get()
                cx.mm(pb.t[:, 0:128], lfb.t[:, :], TRI.t[:, :], True, True, [lfb.b, TRI.b], [pb.b])
                cx.mm(pb.t[:, 128:256], kT.t[:, cs], qT.t[:, cs], True, True, [kT.b, qT.b], [pb.b])
                DT = f128.get()
                cx.act(DT.t[:, :], pb.t[:, 0:128], AF.Exp, [pb.b, a_.b], [DT.b], bias=a_.t[:, h:h + 1])
                cx.tt("gpsimd", DT.t[:, :], DT.t[:, :], TRI.t[:, :], ALU.mult, [DT.b, TRI.b], [DT.b])
                EB = f128.get()
                cx.act(EB.t[:, :], pb.t[:, 0:128], AF.Exp, [pb.b], [EB.b])
                PT = b128.get()
                cx.tt("vector", PT.t[:, :], DT.t[:, :], pb.t[:, 128:256], ALU.mult, [DT.b, pb.b], [PT.b])
                qs = b128.get()
                cx.tt("gpsimd", qs.t[:, :], qT.t[:, cs], EB.t[:, :], ALU.mult, [qT.b, EB.b], [qs.b])
                tb = pTb[h]
                cx.transpose(pT[:, h * 128:(h + 1) * 128], kT.t[:, cs], c["id_b"].t[:, :], [kT.b, c["id_b"].b], [tb])
                Kw = b128.get()
                cx.act(Kw.t[:, :], pT[:, h * 128:(h + 1) * 128], AF.Copy, [tb, wk.b], [Kw.b], scale=wk.t[:, h:h + 1])
                out["h"].append((PT, qs, Kw))
            return out

        def stage_b(ch, sa):
            dec = sa["dec"]
            for h in range(2):
                PT, qs, Kw = sa["h"][h]
                if ch % 4 == 0:
                    Hcur[h] = H[h].get()
                pn = prot.get()
                cx.mm(pn.t[:, 0:128], CTb[h].t[:, 0:128], qs.t[:, :], True, False, [CTb[h].b, qs.b], [pn.b])
                cx.mm(pn.t[:, 0:128], V.t[:, ch, h * 128:(h + 1) * 128], PT.t[:, :], False, True, [V.b, PT.b], [pn.b])
                cx.mm(pn.t[:, 128:256], CTb[h].t[:, 128:256], qs.t[:, :], True, False, [CTb[h].b, qs.b], [pn.b])
                cx.mm(pn.t[:, 128:256], c["ones_b"].t[:, :], PT.t[:, :], False, True, [c["ones_b"].b, PT.b], [pn.b])
                pu = prot.get()
                cx.mm(pu.t[:, 0:128], Kw.t[:, :], V.t[:, ch, h * 128:(h + 1) * 128], True, True, [Kw.b, V.b], [pu.b])
                cx.mm(pu.t[:, 128:256], Kw.t[:, :], c["ones_b"].t[:, :], True, True, [Kw.b, c["ones_b"].b], [pu.b])
                cx.stt(CT[h].t[:, :], CT[h].t[:, :], dec.t[:, h:h + 1], pu.t[:, 0:256], ALU.mult, ALU.add,
                       [CT[h].b, dec.b, pu.b], [CT[h].b])
                cx.copy("gpsimd", CTb[h].t[:, :], CT[h].t[:, :], [CT[h].b], [CTb[h].b])
                r = f128.get()
                cx.ts("vector", r.t[:, :], pn.t[:, 128:256], 1.0, ALU.max, [pn.b], [r.b])
                cx.stt(r.t[:, :], pn.t[:, 128:256], -1.0, r.t[:, :], ALU.mult, ALU.max, [pn.b, r.b], [r.b])
                cx.fw.op("vector", lambda e, r=r: e.reciprocal(out=r.t[:, :], in_=r.t[:, :]), [r.b], [r.b])
                Hc = Hcur[h]
                cx.tt("vector", Hc.t[:, (ch % 4) * 128:(ch % 4 + 1) * 128], pn.t[:, 0:128], r.t[:, :], ALU.mult,
                      [pn.b, r.b], [Hc.b])
                if ch % 4 == 3:
                    t0 = (ch - 3) * 128
                    hf, tl = loc(t0)
                    sq = sqp.get()
                    cx.tt("gpsimd", sq.t[:, :], Hc.t[:, :], Hc.t[:, :], ALU.mult, [Hc.b], [sq.b])
                    ps = prot.get()
                    cx.mm(ps.t[:, :], c["ones_f"].t[:, :], sq.t[:, :], True, True, [c["ones_f"].b, sq.b], [ps.b])
                    ln = f512.get()
                    cx.act(ln.t[:, :], ps.t[:, :], AF.Ln, [ps.b, c["eps"].b], [ln.b], bias=c["eps"].t[:, 0:1], scale=1.0 / 128)
                    rstd = f512.get()
                    cx.act(rstd.t[:, :], ln.t[:, :], AF.Exp, [ln.b], [rstd.b], scale=-0.5)
                    hn = f512.get()
                    cx.stt(hn.t[:, :], Hc.t[:, :], mng.t[:, h:h + 1], rstd.t[:, :], ALU.mult, ALU.mult,
                           [Hc.b, mng.b, rstd.b], [hn.b])
                    og = opl.get()
                    cx.dma("sync", og.t[:, :], PFM[hf, 768 + h * 128:768 + (h + 1) * 128, tl:tl + 512], (), [og.b], dyn=dyn["pfm"])
                    eo = f512.get()
                    cx.act(eo.t[:, :], og.t[:, :], AF.Exp, [og.b], [eo.b], scale=-1.0)
                    cx.ts("gpsimd", eo.t[:, :], eo.t[:, :], 1.0, ALU.add, [eo.b], [eo.b])
                    cx.fw.op("vector", lambda e, eo=eo: e.reciprocal(out=eo.t[:, :], in_=eo.t[:, :]), [eo.b], [eo.b])
                    ob = opl.get()
                    cx.tt("vector", ob.t[:, :], hn.t[:, :], eo.t[:, :], ALU.mult, [hn.b, eo.b], [ob.b])
                    db_ = Buf("moutrow")
                    cx.dma("sync", MOUT[hf, 256 + h * 128:256 + (h + 1) * 128, tl:tl + 512], ob.t[:, :], [ob.b], [db_])
                    if xch:
                        xch.wrote(hf * 768 + 256 + h * 128, hf * 768 + 256 + (h + 1) * 128, 512, db_)

        pend = stage_a(0)
        for ch in range(NCH):
            nxt_a = stage_a(ch + 1) if ch + 1 < NCH else None
            stage_b(ch, pend)
            pend = nxt_a
        if xch:
            xch.flush()
        cx.fw.barrier()
        cx.st = old

    with contextlib.ExitStack() as st:
        old, cx.st = cx.st, st
        pre_fox = list(pre_fox or [])
        fb = cx.sb([4, 1], F32, "f_b")
        cx.dma("sync", fb.t[:], fox_b[:, :], (), [fb.b])
        cx.n += 1
        SCR = nc.dram_tensor(f"f_scr{cx.n}", [4, 6, S], BF16).ap()
        bscr = Buf("scr")
        SEG = min(2048, Tc)
        with contextlib.ExitStack() as st2:
            cx.st = st2
            onesr = cx.sb([4, SEG], F32, "f_ones")
            cx.memset("gpsimd", onesr.t[:], 1.0, [onesr.b])
            fcp = cx.sbpool(2, [4, SEG], F32, "f_F")
            f2p = cx.sbpool(2, [4, SEG], F32, "f_F2")
            p3p = cx.sbpool(2, [4, 6, SEG], BF16, "f_P3")
            prev = None
            for t0 in range(0, S, SEG):
                hf, tl = loc(t0)
                Fc, F2, P3 = fcp.get(), f2p.get(), p3p.get()
                cx.dma("sync", Fc.t[:, :], FFM[hf, :, tl:tl + SEG], (), [Fc.b], dyn=dyn["ffm"])
                cx.ts("vector", Fc.t[:, :], Fc.t[:, :], fb.t[:, 0:1], ALU.add, [Fc.b, fb.b], [Fc.b])
                cx.act(Fc.t[:, :], Fc.t[:, :], AF.Exp, [Fc.b], [Fc.b], scale=-1.0)
                cx.act(Fc.t[:, :], Fc.t[:, :], AF.Ln, [Fc.b, c["ones_f"].b], [Fc.b], bias=c["ones_f"].t[0:4, 0:1])
                cx.ts("vector", Fc.t[:, :], Fc.t[:, :], -1.0, ALU.mult, [Fc.b], [Fc.b])
                if prev is None:
                    cx.fw.op("vector", lambda e, F2=F2, Fc=Fc: e.tensor_tensor_scan(
                        out=F2.t[:, :], data0=onesr.t[:, :], data1=Fc.t[:, :], initial=0.0, op0=ALU.mult, op1=ALU.add),
                        [onesr.b, Fc.b], [F2.b])
                else:
                    cx.fw.op("vector", lambda e, F2=F2, Fc=Fc, prev=prev: e.tensor_tensor_scan(
                        out=F2.t[:, :], data0=onesr.t[:, :], data1=Fc.t[:, :], initial=prev.t[:, SEG - 1:SEG],
                        op0=ALU.mult, op1=ALU.add), [onesr.b, Fc.b, prev.b], [F2.b])
                prev = F2
                cx.ts("vector", P3.t[:, 0, :], F2.t[:, :], 8.0, ALU.mult, [F2.b], [P3.b])
                cx.stt(Fc.t[:, :], F2.t[:, :], 8.0, P3.t[:, 0, :], ALU.mult, ALU.subtract, [F2.b, P3.b], [Fc.b])
                cx.copy("vector", P3.t[:, 1, :], Fc.t[:, :], [Fc.b], [P3.b])
                cx.tt("vector", Fc.t[:, :], Fc.t[:, :], P3.t[:, 1, :], ALU.subtract, [Fc.b, P3.b], [Fc.b])
                cx.copy("vector", P3.t[:, 2, :], Fc.t[:, :], [Fc.b], [P3.b])
                cx.ts("vector", P3.t[:, 3:6, :], P3.t[:, 0:3, :], -1.0, ALU.mult, [P3.b], [P3.b])
                cx.dma("sync", SCR[:, :, t0:t0 + SEG], P3.t[:, :, :], [P3.b], [bscr])
            cx.fw.barrier()
            cx.st = st
        MASK = []
        for r in range(4):
            m = cx.sb([128, 512], BF16, f"f_mask{r}")
            cx.memset("gpsimd", m.t[:], 0.0, [m.b])
            cx.fw.op("gpsimd", lambda e, m=m, r=r: e.affine_select(out=m.t[:], in_=m.t[:], compare_op=ALU.is_ge, fill=-30000.0,
                                                                    base=-128 * r, pattern=[[1, 512]], channel_multiplier=-1),
                     [m.b], [m.b])
            MASK.append(m)
        QA = cx.sbpool(2, [128, S], BF16, "f_qa")
        KA = cx.sbpool(2, [128, S], BF16, "f_ka")
        VA = cx.sbpool(2, [128, NCH, 128], BF16, "f_va")
        ptp = cx.sbpool(6, [128, 512], BF16, "f_pt")
        rp = cx.sbpool(2, [128, 512], F32, "f_r")
        r0p = cx.sbpool(2, [128, 512], F32, "f_r0")
        ost = cx.sbpool(3, [64, 512], BF16, "f_ost")
        srot = Rot(banks[0:5])
        orot = Rot(banks[5:8])
        def load_head(h):
            qa, ka, va = QA.get(), KA.get(), VA.get()
            cx.memset("gpsimd", qa.t[64:70, :], 1.0, [qa.b])
            cx.memset("gpsimd", ka.t[64:70, :], 1.0, [ka.b])
            cx.memset("gpsimd", va.t[:, :, :], 1.0, [va.b])
            for hf in range(2):
                cx.dma("sync", qa.t[0:64, hf * Tc:(hf + 1) * Tc], PFM[hf, 1024 + h * 64:1024 + (h + 1) * 64, :], (), [qa.b], dyn=dyn["pfm"])
                cx.dma("sync", ka.t[0:64, hf * Tc:(hf + 1) * Tc], PFM[hf, 1280 + h * 64:1280 + (h + 1) * 64, :], (), [ka.b], dyn=dyn["pfm"])
                cx.dma("sync", va.t[:, hf * (Tc // 128):(hf + 1) * (Tc // 128), 0:64],
                       VTOK[hf].rearrange("(n p) c -> p n c", p=128)[:, :, 256 + h * 64:256 + (h + 1) * 64], (), [va.b], dyn=dyn["vtok"])
            cx.dma("sync", qa.t[64:67, :], SCR[h, 0:3, :], [bscr], [qa.b])
            cx.dma("sync", ka.t[67:70, :], SCR[h, 3:6, :], [bscr], [ka.b])
            return qa, ka, va

        nxt = load_head(0)
        for h in range(4):
            qa, ka, va = nxt
            if h + 1 < 4:
                nxt = load_head(h + 1)
            for qb in range(S // 512):
                qs_ = slice(qb * 512, (qb + 1) * 512)
                nk = 4 * qb + 4
                if pre_fox and qb % 2 == 1:
                    pre_fox.pop(0)()
                po = orot.get()
                pend = []

                def issue_s(kt):
                    ps = srot.get()
                    diag = kt >= 4 * qb
                    cx.mm(ps.t[:, :], ka.t[0:70, kt * 128:(kt + 1) * 128], qa.t[0:70, qs_], True, not diag, [ka.b, qa.b], [ps.b])
                    if diag:
                        mk = MASK[kt - 4 * qb]
                        cx.mm(ps.t[:, :], c["id_b"].t[:, :], mk.t[:, :], False, True, [c["id_b"].b, mk.b], [ps.b])
                    return ps
                SKEW = 2
                for kt in range(min(SKEW, nk)):
                    pend.append(issue_s(kt))
                for kt in range(nk):
                    if kt + SKEW < nk:
                        pend.append(issue_s(kt + SKEW))
                    ps = pend.pop(0)
                    pt = ptp.get()
                    cx.act(pt.t[:, :], ps.t[:, :], AF.Exp, [ps.b], [pt.b], scale=0.125)
                    cx.mm(po.t[:, :], va.t[:, kt, :], pt.t[:, :], kt == 0, kt == nk - 1, [va.b, pt.b], [po.b])
                rr = rp.get()
                cx.fw.op("vector", lambda e, rr=rr, po=po: e.reciprocal(out=rr.t[64:128, :], in_=po.t[64:128, :]), [po.b], [rr.b])
                r0 = r0p.get()
                cx.copy("vector", r0.t[0:64, :], rr.t[64:128, :], [rr.b], [r0.b])
                os_ = ost.get()
                cx.tt("vector", os_.t[0:64, :], po.t[0:64, :], r0.t[0:64, :], ALU.mult, [po.b, r0.b], [os_.b])
                hf, tl = loc(qb * 512)
                db_ = Buf("moutrow")
                cx.dma("sync", MOUT[hf, 512 + h * 64:512 + (h + 1) * 64, tl:tl + 512], os_.t[0:64, :], [os_.b], [db_])
                if xch:
                    xch.wrote(hf * 768 + 512 + h * 64, hf * 768 + 512 + (h + 1) * 64, 512, db_)
        while pre_fox:
            pre_fox.pop(0)()
        cx.fw.barrier()
        cx.st = old
    cx.fw.wait_all("sync")


def build_phase_b(Tc):
    nc = bass.Bass("TRN2", target_bir_lowering=False)
    PFM = dram(nc, "pfm", [2, 1536, Tc], BF16, "ExternalInput")
    VTOK = dram(nc, "vtok", [2, Tc, 512], BF16, "ExternalInput")
    GTOK = dram(nc, "gtok", [2, 128, Tc // 128, 4], F32, "ExternalInput")
    FFM = dram(nc, "ffm", [2, 4, Tc], F32, "ExternalInput")
    pool_w = dram(nc, "pool_w", [128, 2, 128], F32, "ExternalInput")
    pool_scale = dram(nc, "pool_scale", [128, 2], F32, "ExternalInput")
    pool_coef = dram(nc, "pool_coef", [128, 2, 4], F32, "ExternalInput")
    pool_rc = dram(nc, "pool_rc", [128, 2, 4, 16], F32, "ExternalInput")
    conv_w = dram(nc, "conv_w", [128, 2, 2, 4], F32, "ExternalInput")
    ml_b = dram(nc, "ml_b", [128, 4], F32, "ExternalInput")
    ml_ng = dram(nc, "ml_ng", [128, 2], F32, "ExternalInput")
    fox_b = dram(nc, "fox_b", [4, 1], F32, "ExternalInput")
    MOUT = dram(nc, "mout", [2, 768, Tc], BF16, "ExternalOutput")
    cx = Cx(nc)
    c = make_consts(cx)
    add_eps(cx, c)
    psum = cx.psum_banks(7)
    emit_phase_b(cx, c, Tc, PFM, VTOK, GTOK, FFM, pool_w, pool_scale, pool_coef, pool_rc, conv_w, ml_b, ml_ng, fox_b,
                 MOUT, psum)
    cx.finish()
    return nc


POOL_WINDOWS = (2, 4, 8, 16)


def lay128(v):
    v = np.asarray(v, np.float32)
    return np.ascontiguousarray(v.reshape(-1, 128).T)


def small_b_inputs(g, pool_w_grp, pool_scale, ml_conv_w, ml_b_i, ml_b_f, ml_norm_g, fox_b_f):
    d = {}
    d["pool_w"] = np.ascontiguousarray(np.stack([pool_w_grp[2 * g + gi] for gi in range(2)], axis=1)).astype(np.float32)
    d["pool_scale"] = lay128(pool_scale[g * 256:(g + 1) * 256])
    coef = np.zeros((128, 2, 4), np.float32)
    rc = np.zeros((128, 2, 4, 16), np.float32)
    for gi in range(2):
        w = POOL_WINDOWS[2 * g + gi]
        k = POOL_WINDOWS.index(w)
        coef[:, gi, k] = 1.0 / w
        rc[:, gi, k, :] = 1.0 / np.minimum(np.arange(16) + 1, w)
    d["pool_coef"] = coef
    d["pool_rc"] = rc
    cw = np.zeros((128, 2, 2, 4), np.float32)
    for a in range(2):
        for h in range(2):
            c0 = a * 512 + (2 * g + h) * 128
            cw[:, a, h, :] = ml_conv_w[:, c0:c0 + 128].T
    d["conv_w"] = cw
    mb = np.array([ml_b_i[2 * g], ml_b_i[2 * g + 1], ml_b_f[2 * g], ml_b_f[2 * g + 1]], np.float32)
    d["ml_b"] = np.ascontiguousarray(np.broadcast_to(mb, (128, 4)))
    d["ml_ng"] = lay128(ml_norm_g[g * 256:(g + 1) * 256])
    d["fox_b"] = np.asarray(fox_b_f[4 * g:4 * g + 4], np.float32).reshape(4, 1).copy()
    return d


GROUPS = [[0, 1], [2, 3], [4, 5], [6, 7]]
B_SMALL = (("pool_w", [128, 2, 128]), ("pool_scale", [128, 2]), ("pool_coef", [128, 2, 4]), ("pool_rc", [128, 2, 4, 16]),
           ("conv_w", [128, 2, 2, 4]), ("ml_b", [128, 4]), ("ml_ng", [128, 2]), ("fox_b", [4, 1]))


CC_MAX_BYTES = 2 * 1024 * 1024


class Xch:
    def __init__(self, cx, name, src2d, elem_bytes):
        self.cx = cx
        self.src = src2d
        rows, cols = src2d.shape
        self.rows, self.cols = rows, cols
        half = rows // 2
        R = 1
        for r_ in range(1, half + 1):
            if half % r_ == 0 and r_ * cols * elem_bytes <= CC_MAX_BYTES:
                R = r_
        self.R = R
        self.nch = rows // R
        self.GA = cx.nc.dram_tensor(name, [self.nch, 2, R, cols], src2d.dtype).ap()
        self.area = [0] * self.nch
        self.bufs = [[] for _ in range(self.nch)]
        self.issued = [False] * self.nch

    def wrote(self, row0, row1, ncols, buf):
        R = self.R
        for k in range(row0 // R, (row1 - 1) // R + 1):
            lo, hi = max(row0, k * R), min(row1, (k + 1) * R)
            self.area[k] += (hi - lo) * ncols
            self.bufs[k].append(buf)

    def flush(self):
        R = self.R
        for k in range(self.nch):
            if not self.issued[k] and self.area[k] >= R * self.cols:
                assert self.area[k] == R * self.cols, (k, self.area[k])
                self.issued[k] = True
                src, GA = self.src, self.GA
                self.cx.fw.cc_op(lambda e, k=k: e.collective_compute(
                    "AllGather", ALU.bypass, replica_groups=GROUPS, ins=[src[k * R:(k + 1) * R, :]],
                    outs=[GA[k].rearrange("r i c -> (r i) c")]), self.bufs[k])

    def select(self, dst3):
        assert all(self.issued), self.issued
        hk = self.nch // 2
        mult = hk * 2 * self.R * self.cols
        for r_ in range(2):
            self.cx.dma("sync", dst3[r_].rearrange("(kk i) c -> kk i c", i=self.R), self.GA[0:hk, r_], (), (), dyn=mult)


def build_fused(Tc, depth=2):
    nc = bass.Bass("TRN2", target_bir_lowering=False)
    I = lambda name, shape, dt=F32: dram(nc, name, shape, dt, "ExternalInput")
    xT = I("xT", [D, Tc])
    mix_g = I("mix_g", [depth, 128, 8])
    w_in = I("w_in", [depth, D, IN_W])
    bsm = [{k: I(f"{k}{l}", shp) for k, shp in B_SMALL} for l in range(depth)]
    w_br = I("w_br", [depth, 3, 512, D])
    w_out = I("w_out", [depth, D, D])
    ffn_g = I("ffn_g", [depth, 128, 8])
    ff_g = I("ff_w_gate", [(depth + 1) // 2, D, D_FF])
    ff_u = I("ff_w_up", [(depth + 1) // 2, D, D_FF])
    ff_d = I("ff_w_down", [(depth + 1) // 2, D_FF, D])
    m_r = I("moe_w_router", [depth // 2, D, NEXP])
    m_b = I("moe_b_router", [depth // 2, NEXP, 1])
    m_g = I("moe_w_gate", [depth // 2, NEXP, D, D_FFE])
    m_u = I("moe_w_up", [depth // 2, NEXP, D, D_FFE])
    m_d = I("moe_w_down", [depth // 2, NEXP, D_FFE, D])
    final_g = I("final_g", [128, 8])
    outT = dram(nc, "outT", [D, Tc], F32, "ExternalOutput")
    N = lambda name, shape, dt: nc.dram_tensor(name, list(shape), dt).ap()
    PFM = N("s_pfm", [2, 1536, Tc], BF16)
    GATES = N("s_gates", [3072, Tc], BF16)
    VTOK = N("s_vtok", [2, Tc, 512], BF16)
    GTOK = N("s_gtok", [2, 128, Tc // 128, 4], F32)
    FFM = N("s_ffm", [2, 4, Tc], F32)
    MOUT = N("s_mout", [2, 768, Tc], BF16)
    XN = N("s_xn", [D, Tc], F32)
    PFMS = N("s_pfms", [2, 1536, Tc], BF16)
    VTOKS = N("s_vtoks", [2, Tc, 512], BF16)
    GTOKS = N("s_gtoks", [2, 128, Tc // 128, 4], F32)
    FFMS = N("s_ffms", [2, 4, Tc], F32)
    MINS = N("s_mins", [2, 768, Tc], BF16)
    cx = Cx(nc)
    c = make_consts(cx)
    add_eps(cx, c)
    psum = cx.psum_banks(7)
    xcur = xT
    NM = depth // 2
    W16 = [(N(f"w16g{i}", [NEXP, D, D_FFE], BF16), N(f"w16u{i}", [NEXP, D, D_FFE], BF16), N(f"w16d{i}", [NEXP, D_FFE, D], BF16))
           for i in range(NM)]
    b16 = Buf("w16")

    pc_jobs = []
    for i in range(NM):
        pc_jobs += precast_jobs(cx, [(W16[i][0], m_g[i]), (W16[i][1], m_u[i]), (W16[i][2], m_d[i])], b16)
    for l in range(depth):
        xa = {"pfm": Xch(cx, f"ga_pfm{l}", PFM.rearrange("a b c -> (a b) c"), 2),
              "vtok": Xch(cx, f"ga_vtok{l}", VTOK.rearrange("a b c -> (a b) c"), 2),
              "gtok": Xch(cx, f"ga_gtok{l}", GTOK.rearrange("a p n c -> (a p) (n c)"), 4),
              "ffm": Xch(cx, f"ga_ffm{l}", FFM.rearrange("a h t -> (a h) t"), 4)}
        def early_select(xa=xa):
            for x_ in xa.values():
                x_.flush()
            cx.fw.cc_wait(only="sync")
            xa["pfm"].select(PFMS)
            xa["vtok"].select(VTOKS)
            xa["gtok"].select(GTOKS.rearrange("r p n c -> r p (n c)"))
            xa["ffm"].select(FFMS)
        emit_phase_a(cx, c, Tc, xcur, mix_g[l], w_in[l], PFM, GATES, VTOK, GTOK, FFM, psum, xch=xa, mid_gates=early_select)
        cx.fw.cc_wait()
        cx.fw.barrier()
        b = bsm[l]
        xm = Xch(cx, f"ga_mout{l}", MOUT.rearrange("a b c -> (a b) c"), 2)
        emit_phase_b(cx, c, Tc, PFMS, VTOKS, GTOKS, FFMS, b["pool_w"], b["pool_scale"], b["pool_coef"],
                     b["pool_rc"], b["conv_w"], b["ml_b"], b["ml_ng"], b["fox_b"], MOUT, psum, xch=xm,
                     pre_fox=pc_jobs[:len(pc_jobs) * 2 // 3] if l == 0 else pc_jobs[len(pc_jobs) * 2 // 3:])
        xm.flush()
        cx.fw.cc_wait()
        xm.select(MINS)
        cx.fw.barrier()
        moe = (l % 2 == 1)
        final = (l == depth - 1)
        if moe:
            ffw = (m_r[l // 2], m_b[l // 2]) + W16[l // 2]
        else:
            ffw = (ff_g[l // 2], ff_u[l // 2], ff_d[l // 2])
        dst = outT if final else XN
        emit_phase_c(cx, c, Tc, xcur, MINS, GATES, w_br[l], w_out[l], ffn_g[l], ffw, dst, psum, moe=moe,
                     final_g=final_g if final else None, wq="sync" if moe else "gpsimd", wbuf16=b16 if moe else None)
        xcur = XN
    cx.finish()
    return nc


_PROGS = {}


def kernel(x, mix_norm_g, w_in, pool_w_grp, pool_scale, ml_conv_w, ml_b_i, ml_b_f, ml_norm_g, fox_b_f,
           w_br_pool, w_br_ml, w_br_fox, w_out, ffn_norm_g, ff_w_gate, ff_w_up, ff_w_down,
           moe_w_router, moe_b_router, moe_w_gate, moe_w_up, moe_w_down, final_norm_g):
    f32 = lambda a: np.ascontiguousarray(np.asarray(a, dtype=np.float32))
    x = f32(x)
    Tc = SEQ // 2
    depth = int(np.asarray(w_in).shape[0])
    if "fused" not in _PROGS:
        _PROGS["fused"] = build_fused(Tc, depth)
    nc = _PROGS["fused"]
    shared = {
        "mix_g": np.stack([lay128(mix_norm_g[l]) for l in range(depth)]),
        "w_in": f32(w_in),
        "w_br": np.ascontiguousarray(np.stack([np.stack([f32(w_br_pool[l]), f32(w_br_ml[l]), f32(w_br_fox[l])]) for l in range(depth)])),
        "w_out": f32(w_out),
        "ffn_g": np.stack([lay128(ffn_norm_g[l]) for l in range(depth)]),
        "ff_w_gate": f32(ff_w_gate), "ff_w_up": f32(ff_w_up), "ff_w_down": f32(ff_w_down),
        "moe_w_router": f32(moe_w_router), "moe_b_router": f32(moe_b_router).reshape(-1, NEXP, 1),
        "moe_w_gate": f32(moe_w_gate), "moe_w_up": f32(moe_w_up), "moe_w_down": f32(moe_w_down),
        "final_g": lay128(final_norm_g),
    }
    smalls = [[small_b_inputs(g, f32(pool_w_grp[l]), f32(pool_scale[l]), f32(ml_conv_w[l]), f32(ml_b_i[l]), f32(ml_b_f[l]),
                              f32(ml_norm_g[l]), f32(fox_b_f[l])) for g in range(2)] for l in range(depth)]
    in_maps = []
    for c in range(NCORES):
        d = dict(shared)
        d["xT"] = np.ascontiguousarray(x[c // 2, (c % 2) * Tc:(c % 2 + 1) * Tc, :].T)
        for l in range(depth):
            for k, v in smalls[l][c % 2].items():
                d[f"{k}{l}"] = v
        in_maps.append(d)
    res = run_bass_kernel_spmd(nc, in_maps, core_ids=list(range(NCORES)))
    out = np.empty((BATCH, SEQ, D), np.float32)
    for c in range(NCORES):
        out[c // 2, (c % 2) * Tc:(c % 2 + 1) * Tc, :] = np.asarray(res.results[c]["outT"]).T
    return out
```
